# Optimizing a Trainium2 kernel written in Bass

```python
import math
import jax, jax.numpy as jnp
from jax import lax
import numpy as np

D_MODEL = 2048
BATCH = 2
SEQ = 8192
DEPTH = 2

QBLK = 128
ROPE_THETA = 500000.0
EPS = 1e-6
TINY = 1e-30
BIG = 1e9

SB_HEADS = 8
SB_DIM = 128
MLA_HEADS = 8
MLA_Q_RANK = 512
MLA_KV_RANK = 256
MLA_NOPE = 128
MLA_ROPE = 64
MLA_V = 128
NSA_HEADS = 16
NSA_GROUPS = 2
NSA_DIM = 128
ROT_DIM = NSA_DIM // 4
CMP_BLK = 32
CMP_STRIDE = 16
CMP_HIDDEN = 256
SLC_BLK = 64
SLC_TOPK = 16
WINDOW = 512
PEER_HEADS = 8
PEER_KEYS = 128
PEER_DKEY = 256
PEER_TOPK = 16
N_EXPERTS = PEER_KEYS * PEER_KEYS
PEER_CHUNK = 64

N_EVEN = (DEPTH + 1) // 2
N_ODD = DEPTH // 2
SBMLA_IN = 3 * SB_HEADS * SB_DIM + MLA_Q_RANK + MLA_KV_RANK + MLA_ROPE
SBMLA_OUT = SB_HEADS * SB_DIM + MLA_HEADS * MLA_V
NSA_IN = NSA_HEADS * NSA_DIM + 6 * NSA_GROUPS * NSA_DIM + 3 * NSA_HEADS

kernel_name = "hybrid_sb_mla_nsa_peer_adaln"


def rmsnorm(x, g):
    xf = x.astype(jnp.float32)
    y = xf * lax.rsqrt(jnp.mean(xf * xf, axis=-1, keepdims=True) + EPS)
    return (y * g.astype(jnp.float32)).astype(x.dtype)


def rope(x, pos, rot_dim):
    half = rot_dim // 2
    inv = ROPE_THETA ** (-jnp.arange(half, dtype=jnp.float32) / half)
    ang = pos.astype(jnp.float32)[..., None] * inv
    cos = jnp.cos(ang)[:, :, None, :]
    sin = jnp.sin(ang)[:, :, None, :]
    x1 = x[..., :half].astype(jnp.float32)
    x2 = x[..., half:rot_dim].astype(jnp.float32)
    r1 = (x1 * cos - x2 * sin).astype(x.dtype)
    r2 = (x2 * cos + x1 * sin).astype(x.dtype)
    return jnp.concatenate([r1, r2, x[..., rot_dim:]], axis=-1)


def masked_softmax(z, mask):
    z = jnp.where(mask, z, -jnp.inf)
    m = jnp.max(z, axis=-1, keepdims=True)
    m = jnp.where(jnp.isfinite(m), m, 0.0)
    e = jnp.exp(z - m)
    return e / jnp.maximum(jnp.sum(e, axis=-1, keepdims=True), TINY)


def modulation(c, w, b):
    m = jax.nn.silu(c) @ w + b
    shift, scale, gate = jnp.split(m, 3, axis=-1)
    return shift[:, None, :], scale[:, None, :], gate[:, None, :]


def sweep(fn, block, *arrs):
    n = arrs[0].shape[1] // block
    def body(s0):
        return fn(s0, *[lax.dynamic_slice_in_dim(a, s0, block, axis=1) for a in arrs])
    out = lax.map(body, jnp.arange(n, dtype=jnp.int32) * block)
    out = jnp.moveaxis(out, 0, 1)
    return out.reshape((out.shape[0], n * block) + out.shape[3:])


def stick_breaking(q, k, v):
    S = q.shape[1]
    scale = 1.0 / math.sqrt(SB_DIM)
    kpos = jnp.arange(S)
    def blk(q0, qb):
        z = jnp.einsum('bqhd,bkhd->bhqk', qb, k).astype(jnp.float32) * scale
        t = q0 + jnp.arange(QBLK)
        strict = kpos[None, :] < t[:, None]
        log_1m = jnp.where(strict, jax.nn.log_sigmoid(-z), 0.0)
        cs = jnp.cumsum(log_1m, axis=-1)
        suffix = cs[..., -1:] - cs
        a = jnp.where(strict, jnp.exp(jax.nn.log_sigmoid(z) + suffix), 0.0)
        return jnp.einsum('bhqk,bkhd->bqhd', a.astype(v.dtype), v)
    return sweep(blk, QBLK, q)


def mla(c_q, c_kv, k_r, pos, q_norm, w_uq, kv_norm, w_ukv):
    B, S, _ = c_q.shape
    q = (rmsnorm(c_q, q_norm) @ w_uq).reshape(B, S, MLA_HEADS, MLA_NOPE + MLA_ROPE)
    q_nope = q[..., :MLA_NOPE]
    q_rope = rope(q[..., MLA_NOPE:], pos, MLA_ROPE)
    kv = (rmsnorm(c_kv, kv_norm) @ w_ukv).reshape(B, S, MLA_HEADS, MLA_NOPE + MLA_V)
    k_nope, v = kv[..., :MLA_NOPE], kv[..., MLA_NOPE:]
    k_rope = rope(k_r[:, :, None, :], pos, MLA_ROPE)[:, :, 0, :]
    scale = 1.0 / math.sqrt(MLA_NOPE + MLA_ROPE)
    kpos = jnp.arange(S)
    def blk(q0, qn, qr):
        t = q0 + jnp.arange(QBLK)
        z = (jnp.einsum('bqhd,bkhd->bhqk', qn, k_nope)
             + jnp.einsum('bqhr,bkr->bhqk', qr, k_rope)).astype(jnp.float32) * scale
        p = masked_softmax(z, kpos[None, :] <= t[:, None])
        return jnp.einsum('bhqk,bkhd->bqhd', p.astype(v.dtype), v)
    return sweep(blk, QBLK, q_nope, q_rope)


def sb_mla_mixer(h, pos, w_in, q_norm, w_uq, kv_norm, w_ukv, w_out):
    B, S, _ = h.shape
    sbw = SB_HEADS * SB_DIM
    splits = np.cumsum([sbw, sbw, sbw, MLA_Q_RANK, MLA_KV_RANK]).tolist()
    q_sb, k_sb, v_sb, c_q, c_kv, k_r = jnp.split(h @ w_in, splits, axis=-1)
    shp = (B, S, SB_HEADS, SB_DIM)
    o_a = stick_breaking(q_sb.reshape(shp), k_sb.reshape(shp), v_sb.reshape(shp))
    o_b = mla(c_q, c_kv, k_r, pos, q_norm, w_uq, kv_norm, w_ukv)
    o = jnp.concatenate([o_a.reshape(B, S, -1), o_b.reshape(B, S, -1)], axis=-1)
    return o @ w_out


def nsa_mixer(h, pos, w_in, pe_k, pe_v, w1_k, w2_k, w1_v, w2_v, w_out):
    B, S, _ = h.shape
    G, R, d = NSA_GROUPS, NSA_HEADS // NSA_GROUPS, NSA_DIM
    kvw = G * d
    splits = np.cumsum([NSA_HEADS * d, kvw, kvw, kvw, kvw, kvw, kvw]).tolist()
    q, kc, vc, ks, vs, kw, vw, gl = jnp.split(h @ w_in, splits, axis=-1)
    q = rope(q.reshape(B, S, NSA_HEADS, d), pos, ROT_DIM).reshape(B, S, G, R, d)
    kc = rope(kc.reshape(B, S, G, d), pos, ROT_DIM)
    ks = rope(ks.reshape(B, S, G, d), pos, ROT_DIM)
    kw = rope(kw.reshape(B, S, G, d), pos, ROT_DIM)
    vc, vs, vw = (a.reshape(B, S, G, d) for a in (vc, vs, vw))
    gates = jax.nn.sigmoid(gl.reshape(B, S, G, R, 3))

    n_cmp = (S - CMP_BLK) // CMP_STRIDE + 1
    cidx = np.arange(n_cmp)[:, None] * CMP_STRIDE + np.arange(CMP_BLK)[None, :]
    def compress(t, pe, w1, w2):
        blk = t[:, cidx] + pe[None, None, :, None, :]
        blk = blk.transpose(0, 1, 3, 2, 4).reshape(B, n_cmp, G, CMP_BLK * d)
        return jax.nn.gelu(blk @ w1) @ w2
    kc_c = compress(kc, pe_k, w1_k, w2_k)
    vc_c = compress(vc, pe_v, w1_v, w2_v)
    cmp_end = jnp.asarray(cidx[:, -1])

    n_slc = S // SLC_BLK
    slc_k = min(SLC_TOPK, n_slc)
    c_s = np.arange(n_cmp) * CMP_STRIDE
    s_s = np.arange(n_slc) * SLC_BLK
    overlap = np.clip(np.minimum(c_s[:, None] + CMP_BLK, s_s[None, :] + SLC_BLK)
                      - np.maximum(c_s[:, None], s_s[None, :]), 0, None)
    overlap = jnp.asarray(overlap.astype(np.float32))
    ks_b = ks.reshape(B, n_slc, SLC_BLK, G, d).transpose(0, 3, 1, 2, 4)
    vs_b = vs.reshape(B, n_slc, SLC_BLK, G, d).transpose(0, 3, 1, 2, 4)
    blk_id = jnp.arange(n_slc)
    bi = jnp.arange(B)[:, None, None, None]
    gi = jnp.arange(G)[None, :, None, None]

    kw_p = jnp.pad(kw, ((0, 0), (WINDOW, 0), (0, 0), (0, 0)))
    vw_p = jnp.pad(vw, ((0, 0), (WINDOW, 0), (0, 0), (0, 0)))
    scale = 1.0 / math.sqrt(d)

    def blk(q0, qb, gb):
        t = q0 + jnp.arange(QBLK)
        zc = jnp.einsum('bqgrd,bngd->bgrqn', qb, kc_c).astype(jnp.float32) * scale
        pc = masked_softmax(zc, cmp_end[None, :] <= t[:, None])
        oc = jnp.einsum('bgrqn,bngd->bqgrd', pc.astype(vc_c.dtype), vc_c)
        imp = jnp.einsum('bgrqn,nj->bgqj', pc.astype(jnp.float32), overlap)
        cur = t // SLC_BLK
        forced = (blk_id[None, :] == 0) | (blk_id[None, :] == cur[:, None]) | (blk_id[None, :] == cur[:, None] - 1)
        valid = blk_id[None, :] * SLC_BLK <= t[:, None]
        score = jnp.where(forced, BIG, jnp.where(valid, imp, -BIG))
        _, sel = lax.top_k(score, slc_k)
        kg = ks_b[bi, gi, sel]
        vg = vs_b[bi, gi, sel]
        zs = jnp.einsum('bqgrd,bgqkld->bgrqkl', qb, kg).astype(jnp.float32) * scale
        kpos = sel[..., None] * SLC_BLK + jnp.arange(SLC_BLK)
        smask = (kpos <= t[:, None, None])[:, :, None].reshape(B, G, 1, QBLK, slc_k * SLC_BLK)
        ps = masked_softmax(zs.reshape(B, G, R, QBLK, slc_k * SLC_BLK), smask)
        ps = ps.reshape(B, G, R, QBLK, slc_k, SLC_BLK)
        osl = jnp.einsum('bgrqkl,bgqkld->bqgrd', ps.astype(vg.dtype), vg)
        kwb = lax.dynamic_slice_in_dim(kw_p, q0, WINDOW + QBLK, axis=1)
        vwb = lax.dynamic_slice_in_dim(vw_p, q0, WINDOW + QBLK, axis=1)
        wpos = q0 - WINDOW + jnp.arange(WINDOW + QBLK)
        wmask = (wpos[None, :] <= t[:, None]) & (wpos[None, :] > t[:, None] - WINDOW) & (wpos[None, :] >= 0)
        zw = jnp.einsum('bqgrd,bkgd->bgrqk', qb, kwb).astype(jnp.float32) * scale
        pw = masked_softmax(zw, wmask)
        ow = jnp.einsum('bgrqk,bkgd->bqgrd', pw.astype(vwb.dtype), vwb)
        return gb[..., 0:1] * oc + gb[..., 1:2] * osl + gb[..., 2:3] * ow

    o = sweep(blk, QBLK, q, gates)
    return o.reshape(B, S, NSA_HEADS * d) @ w_out


def peer(h, w_q, k1, k2, u, v):
    B, S, D = h.shape
    half = PEER_DKEY // 2
    q = (h @ w_q).reshape(B, S, PEER_HEADS, 2, half).astype(jnp.float32)
    s1 = jnp.einsum('bshd,nd->bshn', q[..., 0, :], k1.astype(jnp.float32))
    s2 = jnp.einsum('bshd,nd->bshn', q[..., 1, :], k2.astype(jnp.float32))
    v1, i1 = lax.top_k(s1, PEER_TOPK)
    v2, i2 = lax.top_k(s2, PEER_TOPK)
    cand = (v1[..., :, None] + v2[..., None, :]).reshape(B, S, PEER_HEADS, PEER_TOPK * PEER_TOPK)
    cidx = (i1[..., :, None] * PEER_KEYS + i2[..., None, :]).reshape(B, S, PEER_HEADS, PEER_TOPK * PEER_TOPK)
    top, pos = lax.top_k(cand, PEER_TOPK)
    experts = jnp.take_along_axis(cidx, pos, axis=-1)
    g = jax.nn.softmax(top, axis=-1).astype(h.dtype)
    def chunk(s0, hb, eb, gb):
        act = jax.nn.gelu(jnp.einsum('bqd,bqhkd->bqhk', hb, u[eb]))
        return jnp.einsum('bqhk,bqhkd->bqd', gb * act, v[eb])
    return sweep(chunk, PEER_CHUNK, h, experts, g)


def setup_inputs(seed: int = 0) -> dict:
    key = jax.random.key(seed)
    ks = iter(jax.random.split(key, 40))
    def nrm(shape, std):
        return jax.random.normal(next(ks), shape, jnp.float32) * std
    def gain(shape):
        return 1.0 + nrm(shape, 0.05)
    D = D_MODEL
    x = nrm((BATCH, SEQ, D), 1.0)
    c = nrm((BATCH, D), 1.0)
    offset = jax.random.randint(next(ks), (BATCH, 1), 0, 1024, dtype=jnp.int32)
    positions = offset + jnp.arange(SEQ, dtype=jnp.int32)[None, :]
    return {
        "x": x,
        "c": c,
        "positions": positions,
        "norm_mix": gain((DEPTH, D)),
        "ada_mix_w": nrm((DEPTH, D, 3 * D), 0.5 * D ** -0.5),
        "ada_mix_b": nrm((DEPTH, 3 * D), 0.02),
        "sbmla_w_in": nrm((N_EVEN, D, SBMLA_IN), D ** -0.5),
        "mla_q_norm": gain((N_EVEN, MLA_Q_RANK)),
        "mla_w_uq": nrm((N_EVEN, MLA_Q_RANK, MLA_HEADS * (MLA_NOPE + MLA_ROPE)), MLA_Q_RANK ** -0.5),
        "mla_kv_norm": gain((N_EVEN, MLA_KV_RANK)),
        "mla_w_ukv": nrm((N_EVEN, MLA_KV_RANK, MLA_HEADS * (MLA_NOPE + MLA_V)), MLA_KV_RANK ** -0.5),
        "sbmla_w_out": nrm((N_EVEN, SBMLA_OUT, D), SBMLA_OUT ** -0.5),
        "nsa_w_in": nrm((N_ODD, D, NSA_IN), D ** -0.5),
        "nsa_pe_k": nrm((N_ODD, CMP_BLK, NSA_DIM), 0.1),
        "nsa_pe_v": nrm((N_ODD, CMP_BLK, NSA_DIM), 0.1),
        "nsa_w1_k": nrm((N_ODD, CMP_BLK * NSA_DIM, CMP_HIDDEN), (CMP_BLK * NSA_DIM) ** -0.5),
        "nsa_w2_k": nrm((N_ODD, CMP_HIDDEN, NSA_DIM), CMP_HIDDEN ** -0.5),
        "nsa_w1_v": nrm((N_ODD, CMP_BLK * NSA_DIM, CMP_HIDDEN), (CMP_BLK * NSA_DIM) ** -0.5),
        "nsa_w2_v": nrm((N_ODD, CMP_HIDDEN, NSA_DIM), CMP_HIDDEN ** -0.5),
        "nsa_w_out": nrm((N_ODD, NSA_HEADS * NSA_DIM, D), (NSA_HEADS * NSA_DIM) ** -0.5),
        "norm_ffn": gain((DEPTH, D)),
        "ada_ffn_w": nrm((DEPTH, D, 3 * D), 0.5 * D ** -0.5),
        "ada_ffn_b": nrm((DEPTH, 3 * D), 0.02),
        "peer_w_q": nrm((DEPTH, D, PEER_HEADS * PEER_DKEY), D ** -0.5),
        "peer_k1": nrm((DEPTH, PEER_KEYS, PEER_DKEY // 2), (PEER_DKEY // 2) ** -0.5),
        "peer_k2": nrm((DEPTH, PEER_KEYS, PEER_DKEY // 2), (PEER_DKEY // 2) ** -0.5),
        "peer_u": nrm((DEPTH, N_EXPERTS, D), D ** -0.5),
        "peer_v": nrm((DEPTH, N_EXPERTS, D), PEER_HEADS ** -0.5),
        "final_norm": gain((D,)),
    }


def reference(x, c, positions, norm_mix, ada_mix_w, ada_mix_b, sbmla_w_in, mla_q_norm, mla_w_uq,
              mla_kv_norm, mla_w_ukv, sbmla_w_out, nsa_w_in, nsa_pe_k, nsa_pe_v, nsa_w1_k, nsa_w2_k,
              nsa_w1_v, nsa_w2_v, nsa_w_out, norm_ffn, ada_ffn_w, ada_ffn_b, peer_w_q, peer_k1, peer_k2,
              peer_u, peer_v, final_norm):
    for layer in range(DEPTH):
        i = layer // 2
        shift, scale, gate = modulation(c, ada_mix_w[layer], ada_mix_b[layer])
        h = rmsnorm(x, norm_mix[layer]) * (1.0 + scale) + shift
        if layer % 2 == 0:
            y = sb_mla_mixer(h, positions, sbmla_w_in[i], mla_q_norm[i], mla_w_uq[i],
                             mla_kv_norm[i], mla_w_ukv[i], sbmla_w_out[i])
        else:
            y = nsa_mixer(h, positions, nsa_w_in[i], nsa_pe_k[i], nsa_pe_v[i], nsa_w1_k[i],
                          nsa_w2_k[i], nsa_w1_v[i], nsa_w2_v[i], nsa_w_out[i])
        x = x + gate * y
        shift, scale, gate = modulation(c, ada_ffn_w[layer], ada_ffn_b[layer])
        h = rmsnorm(x, norm_ffn[layer]) * (1.0 + scale) + shift
        x = x + gate * peer(h, peer_w_q[layer], peer_k1[layer], peer_k2[layer], peer_u[layer], peer_v[layer])
    return rmsnorm(x, final_norm)
```

```python
import math
import numpy as np
import concourse.bass as bass
import concourse.mybir as mybir
from concourse.alu_op_type import AluOpType as ALU
from concourse.bass_utils import run_bass_kernel_spmd

AF = mybir.ActivationFunctionType
F32 = mybir.dt.float32
BF16 = mybir.dt.bfloat16
I32 = mybir.dt.int32
U32 = mybir.dt.uint32
AX = mybir.AxisListType


class Res:
    __slots__ = ("name", "lw", "rd", "dsem", "dcnt", "dlast", "dkey")

    def __init__(self, name=""):
        self.name = name
        self.lw = None
        self.rd = {}
        self.dsem = None
        self.dcnt = 0
        self.dlast = None
        self.dkey = None


class Prog:
    ENGS = ("pe", "dve", "act", "pool", "sp")

    def __init__(self, nc):
        self.nc = nc
        self.ops = {e: [] for e in self.ENGS}
        self.cnt = {e: 0 for e in self.ENGS}
        self.seen = {e: {} for e in self.ENGS}
        self.esem = {e: nc.alloc_semaphore(name="es_" + e) for e in self.ENGS}
        self.dsems = {}
        self.all_dma = []
        self.nres = 0
        self.prefix = ""
        self.free_dsems = []
        self.nsem = 0

    def sb(self, name, shape, dt=F32):
        return self.nc.alloc_sbuf_tensor("s_" + self.prefix + name, list(shape), dt)

    def ps(self, name, shape=(128, 512), dt=F32):
        return self.nc.alloc_psum_tensor("p_" + self.prefix + name, list(shape), dt)

    def res(self, name=""):
        self.nres += 1
        return Res(name or f"r{self.nres}")

    def _deps(self, reads, writes):
        deps = {}

        def add(tok):
            if tok is None:
                return
            k, v = tok
            if deps.get(k, 0) < v:
                deps[k] = v
        for r in reads:
            add(r.lw)
        for w in writes:
            add(w.lw)
            for k, v in w.rd.items():
                add((k, v))
        return deps

    def _filter(self, eng, deps):
        waits = []
        seen = self.seen[eng]
        for k, v in deps.items():
            if k == ("e", eng) and eng == "pe":
                continue
            if seen.get(k, 0) >= v:
                continue
            seen[k] = v
            waits.append((k, v))
        return waits

    def _commit(self, tok, reads, writes):
        k, v = tok
        for r in reads:
            if r.rd.get(k, 0) < v:
                r.rd[k] = v
        for w in writes:
            w.lw = tok
            w.rd = {}

    def op(self, eng, fn, reads=(), writes=()):
        deps = self._deps(reads, writes)
        waits = self._filter(eng, deps)
        self.cnt[eng] += 1
        tok = (("e", eng), self.cnt[eng])
        self.ops[eng].append((waits, fn, None, 0))
        self._commit(tok, reads, writes)
        return tok

    def dma(self, eng, fns, semres, reads=(), writes=()):
        if not isinstance(fns, (list, tuple)):
            fns = [fns]
        if semres.dsem is None:
            if self.free_dsems:
                semres.dsem, semres.dkey, semres.dcnt = self.free_dsems.pop()
            else:
                self.nsem += 1
                semres.dsem = self.nc.alloc_semaphore(name=f"ds{self.nsem}")
                semres.dkey = ("d", self.nsem)
                semres.dcnt = 0
            self.all_dma.append(semres)
        deps = self._deps(reads, writes)
        if semres.dlast is not None:
            k, v = semres.dlast
            if deps.get(k, 0) < v:
                deps[k] = v
        waits = self._filter(eng, deps)
        key = semres.dkey
        self.dsems[key] = semres.dsem
        tok = None
        for i, fn in enumerate(fns):
            semres.dcnt += 16
            tok = (key, semres.dcnt)
            self.ops[eng].append((waits if i == 0 else [], fn, semres.dsem, 16))
        semres.dlast = tok
        self._commit(tok, reads, writes)
        return tok

    def final_wait(self, eng="sp"):
        deps = {}
        for e in self.ENGS:
            if self.cnt[e] > 0:
                deps[("e", e)] = self.cnt[e]
        for r in self.all_dma:
            deps[r.dkey] = r.dcnt
        waits = self._filter(eng, deps)
        self.ops[eng].append((waits, None, None, 0))

    def barrier(self):
        for e in self.ENGS:
            self.final_wait(e)
        for r in self.all_dma:
            self.free_dsems.append((r.dsem, r.dkey, r.dcnt))
            r.dsem = None
        self.all_dma = []

    def _sem(self, k):
        return self.esem[k[1]] if k[0] == "e" else self.dsems[k]

    def _emit_eng(self, ename, e):
        for waits, fn, dsem, inc in self.ops[ename]:
            for k, v in waits:
                e.wait_ge(self._sem(k), v)
            if fn is None:
                continue
            ins = fn(e)
            if dsem is not None:
                ins.then_inc(dsem, 16)
            else:
                ins.then_inc(self.esem[ename], 1)

    def emit(self):
        for e in self.ENGS:
            self.final_wait(e)
        with self.nc.Block() as block:
            @block.sync
            def _(e):
                self._emit_eng("sp", e)

            @block.tensor
            def _(e):
                self._emit_eng("pe", e)

            @block.vector
            def _(e):
                self._emit_eng("dve", e)

            @block.scalar
            def _(e):
                self._emit_eng("act", e)

            @block.gpsimd
            def _(e):
                self._emit_eng("pool", e)

    def stats(self):
        return {e: len(self.ops[e]) for e in self.ENGS}


class T:
    def __init__(self, P, name, shape, dt=F32, psum=False):
        self.t = P.ps(name, shape, dt) if psum else P.sb(name, shape, dt)
        self.r = P.res(name)

    def __getitem__(self, k):
        return self.t[k]


class Pool_:
    def __init__(self, P, name, shape, n, dt=F32, psum=False):
        self.tiles = [T(P, f"{name}{i}", shape, dt, psum) for i in range(n)]
        self.i = 0

    def next(self):
        t = self.tiles[self.i % len(self.tiles)]
        self.i += 1
        return t


def dram_in(nc, name, shape, dt=F32):
    return nc.dram_tensor(name, list(shape), dt, kind="ExternalInput").ap()


def dram_out(nc, name, shape, dt=F32):
    return nc.dram_tensor(name, list(shape), dt, kind="ExternalOutput").ap()


class Phase:
    def __init__(self, P, name):
        self.P, self.name = P, name

    def __enter__(self):
        nc = self.P.nc
        self.save = (nc.psum_base, nc.psum_top, nc.sbuf_base, nc.sbuf_top)
        self.P.prefix = self.name + "_"
        return self

    def __exit__(self, *a):
        nc = self.P.nc
        self.P.barrier()
        nc.psum_base, nc.psum_top, nc.sbuf_base, nc.sbuf_top = self.save
        self.P.prefix = ""
        return False


def dram_tmp(nc, name, shape, dt=F32):
    return nc.dram_tensor(name, list(shape), dt, kind="Internal").ap()


import math
ADT = BF16

D_MODEL = 2048
NT = 16
EPS = 1e-6
TWO_PI = 2 * math.pi


def ld(P, dst, src, res, eng="sp", reads=()):
    return P.dma(eng, lambda e: e.dma_start(out=dst, in_=src), res, reads=list(reads), writes=[res])


def xload_direct(x_ap):
    def f(P, xt, ti):
        ld(P, xt[:], x_ap[ti * 128:(ti + 1) * 128, :], xt.r)
    return f


def xload_gather(P, x_ap, idx_ap, nt):
    it = T(P, "xidx", [128, nt], I32)
    ld(P, it[:], idx_ap, it.r)
    def f(P, xt, ti):
        P.dma("pool", lambda e: e.indirect_dma_start(out=xt[:], out_offset=None, in_=x_ap, in_offset=bass.IndirectOffsetOnAxis(ap=it[:, ti:ti + 1], axis=0)), xt.r, reads=[it.r], writes=[xt.r])
    return f


class SlabLoader:
    def __init__(self, P, name, lowp, shape=(128, 16, 256)):
        self.P, self.lowp = P, lowp
        self.f = Pool_(P, name, list(shape), 2)
        self.b = Pool_(P, name + "b", list(shape), 2, dt=BF16) if lowp else None
        self.i = 0

    def load(self, src_ap, ncol=256):
        P = self.P
        sl = self.f.next()
        ld(P, sl[:, :, 0:ncol], src_ap, sl.r)
        if not self.lowp:
            return sl
        sb_ = self.b.next()
        eng = ("act", "dve", "pool")[self.i % 3]
        self.i += 1
        if eng == "act":
            P.op("act", lambda e: e.copy(out=sb_[:, :, 0:ncol], in_=sl[:, :, 0:ncol]), reads=[sl.r], writes=[sb_.r])
        else:
            P.op(eng, lambda e: e.tensor_copy(out=sb_[:, :, 0:ncol], in_=sl[:, :, 0:ncol]), reads=[sl.r], writes=[sb_.r])
        return sb_


def st(P, dst, src, res, eng="pool", dram=None):
    return P.dma(eng, lambda e: e.dma_start(out=dst, in_=src), res, reads=[res], writes=[dram] if dram else [])


def build_M():
    nc = bass.Bass("TRN2", target_bir_lowering=False)
    P = Prog(nc)
    c2 = dram_in(nc, "c2", [128, 16])
    Ws = [dram_in(nc, f"adaw{m}", [2048, 6144]) for m in range(4)]
    bs = [dram_in(nc, f"adab{m}", [1, 6144]) for m in range(4)]
    gs = [dram_in(nc, f"g{m}", [1, 2048]) for m in range(4)]
    mods = dram_out(nc, "mods", [4, 3, 2048])
    emit_M(P, c2, Ws, bs, gs, mods)
    P.emit()
    return nc


def emit_M(P, c2, Ws, bs, gs, mods):
    c2t = T(P, "c2t", [128, 16]); sc = T(P, "sc", [128, 16])
    ld(P, c2t[:], c2, c2t.r)
    P.op("act", lambda e: e.activation(out=sc[:], in_=c2t[:], func=AF.Silu), reads=[c2t.r], writes=[sc.r])
    slabs = Pool_(P, "mslab", [128, 16, 256], 2)
    pbank = Pool_(P, "mps", [128, 512], 2, psum=True)
    brow = T(P, "brow", [1, 6144]); grow = T(P, "grow", [1, 2048])
    mrow = T(P, "mrow", [1, 6144]); arow = T(P, "arow", [1, 2048])
    for m in range(4):
        ld(P, brow[:], bs[m], brow.r); ld(P, grow[:], gs[m], grow.r)
        Wv = Ws[m].rearrange("(k p) c -> p k c", p=128)
        for j in range(24):
            sl = slabs.next()
            ld(P, sl[:], Wv[:, :, j * 256:(j + 1) * 256], sl.r)
            pb = pbank.next()
            for k in range(16):
                P.op("pe", lambda e, pb=pb, sl=sl, k=k: e.matmul(pb[0:1, 0:256], lhsT=sc[:, k:k + 1], rhs=sl[:, k, :], start=(k == 0), stop=(k == 15)),
                     reads=[sc.r, sl.r], writes=[pb.r])
            P.op("dve", lambda e, pb=pb, j=j, mrow=mrow, brow=brow: e.tensor_tensor(out=mrow[0:1, j * 256:(j + 1) * 256], in0=pb[0:1, 0:256], in1=brow[0:1, j * 256:(j + 1) * 256], op=ALU.add),
                 reads=[pb.r, brow.r], writes=[mrow.r])
        P.op("dve", lambda e, mrow=mrow, grow=grow, arow=arow: e.scalar_tensor_tensor(out=arow[:], in0=mrow[0:1, 2048:4096], scalar=1.0, in1=grow[:], op0=ALU.add, op1=ALU.mult),
             reads=[mrow.r, grow.r], writes=[arow.r])
        st(P, mods[m, 0:1, :], arow[:], arow.r)
        st(P, mods[m, 1:2, :], mrow[0:1, 0:2048], mrow.r)
        st(P, mods[m, 2:3, :], mrow[0:1, 4096:6144], mrow.r)


class HCtx:
    def __init__(self, P, mods, mi, ident, need_gate=False):
        self.P = P
        self.A = T(P, "Abc", [128, 2048]); self.B = T(P, "Bbc", [128, 2048])
        ld(P, self.A[:], mods[mi, 0:1, :].broadcast_to([128, 2048]), self.A.r)
        ld(P, self.B[:], mods[mi, 1:2, :].broadcast_to([128, 2048]), self.B.r)
        if need_gate:
            self.G = T(P, "Gbc", [128, 2048])
            ld(P, self.G[:], mods[mi, 2:3, :].broadcast_to([128, 2048]), self.G.r)
        self.id = T(P, "ident", [128, 128])
        ld(P, self.id[:], ident, self.id.r)
        self.ss = Pool_(P, "ss", [128, 1], 2)
        self.tp = Pool_(P, "tps", [128, 512], 2, psum=True)
        self.evi = 0

    def norm_mod(self, xt, ht):
        P = self.P
        ss = self.ss.next()
        P.op("act", lambda e: e.activation(out=ht[:], in_=xt[:], func=AF.Square, accum_out=ss[:]), reads=[xt.r], writes=[ht.r, ss.r])
        P.op("dve", lambda e: e.tensor_scalar(out=ss[:], in0=ss[:], scalar1=1.0 / D_MODEL, scalar2=EPS, op0=ALU.mult, op1=ALU.add), reads=[ss.r], writes=[ss.r])
        P.op("act", lambda e: e.activation(out=ss[:], in_=ss[:], func=AF.Sqrt), reads=[ss.r], writes=[ss.r])
        P.op("dve", lambda e: e.reciprocal(out=ss[:], in_=ss[:]), reads=[ss.r], writes=[ss.r])
        P.op("dve", lambda e: e.scalar_tensor_tensor(out=ht[:], in0=xt[:], scalar=ss[:, 0:1], in1=self.A[:], op0=ALU.mult, op1=ALU.mult),
             reads=[xt.r, ss.r, self.A.r], writes=[ht.r])
        P.op("pool", lambda e: e.tensor_tensor(out=ht[:], in0=ht[:], in1=self.B[:], op=ALU.add), reads=[ht.r, self.B.r], writes=[ht.r])

    def evac(self, dst_ap, src_ap, reads, writes, scale=None):
        P = self.P
        self.evi += 1
        if self.evi % 2 == 0:
            if scale is None:
                P.op("act", lambda e: e.copy(out=dst_ap, in_=src_ap), reads=reads, writes=writes)
            else:
                P.op("act", lambda e: e.activation(out=dst_ap, in_=src_ap, func=AF.Copy, scale=scale), reads=reads, writes=writes)
        else:
            if scale is None:
                P.op("dve", lambda e: e.tensor_copy(out=dst_ap, in_=src_ap), reads=reads, writes=writes)
            else:
                P.op("dve", lambda e: e.tensor_scalar(out=dst_ap, in0=src_ap, scalar1=scale, scalar2=None, op0=ALU.mult), reads=reads, writes=writes)

    def transpose_to(self, src, ncols, dst_fn, dst_res):
        P = self.P
        nk = (ncols + 127) // 128
        for k0 in range(0, nk, 4):
            pb = self.tp.next()
            kk = list(range(k0, min(nk, k0 + 4)))
            for k in kk:
                w = min(128, ncols - k * 128)
                P.op("pe", lambda e, pb=pb, k=k, k0=k0, w=w: e.transpose(pb[0:w, (k - k0) * 128:(k - k0 + 1) * 128], src[:, k * 128:k * 128 + w], self.id[:]),
                     reads=[src.r, self.id.r], writes=[pb.r])
            for k in kk:
                w = min(128, ncols - k * 128)
                self.evac(dst_fn(k), pb[0:w, (k - k0) * 128:(k - k0 + 1) * 128], [pb.r], [dst_res])


class Rope:
    def __init__(self, P, invf_ap, half):
        self.P, self.half = P, half
        self.posi = T(P, "posi", [128, 4], I32); self.posf = T(P, "posf", [128, 4])
        self.inv = T(P, "invf", [128, half])
        ld(P, self.inv[:], invf_ap.broadcast_to([128, half]), self.inv.r)
        self.y = T(P, "ropey", [128, 4, half]); self.ki = T(P, "ropeki", [128, 4, half], I32); self.kf = T(P, "ropekf", [128, 4, half])
        self.yp = [T(P, "ypsin", [128, 4, half]), T(P, "ypcos", [128, 4, half])]
        self.tab = [T(P, "tabsin", [128, 4, half]), T(P, "tabcos", [128, 4, half])]
        self.nb = T(P, "negpi", [128, 1])
        P.op("pool", lambda e: e.memset(self.nb[:], -math.pi * 0.999999), writes=[self.nb.r])

    def compute(self, pos_ap):
        P, half = self.P, self.half
        posi, posf, inv, y, ki, kf, nb = self.posi, self.posf, self.inv, self.y, self.ki, self.kf, self.nb
        ld(P, posi[:], pos_ap, posi.r)
        P.op("dve", lambda e: e.tensor_copy(out=posf[:], in_=posi[:]), reads=[posi.r], writes=[posf.r])
        P.op("dve", lambda e: e.tensor_tensor(out=y[:], in0=posf[:].unsqueeze(2).broadcast_to([128, 4, half]), in1=inv[:].unsqueeze(1).broadcast_to([128, 4, half]), op=ALU.mult),
             reads=[posf.r, inv.r], writes=[y.r])
        for yp, tab, off in ((self.yp[0], self.tab[0], 0.5), (self.yp[1], self.tab[1], 0.75)):
            P.op("dve", lambda e, yp=yp, off=off: e.tensor_scalar(out=yp[:], in0=y[:], scalar1=off, scalar2=None, op0=ALU.add), reads=[y.r], writes=[yp.r])
            P.op("dve", lambda e, yp=yp: e.tensor_copy(out=ki[:], in_=yp[:]), reads=[yp.r], writes=[ki.r])
            P.op("dve", lambda e: e.tensor_copy(out=kf[:], in_=ki[:]), reads=[ki.r], writes=[kf.r])
            P.op("dve", lambda e, yp=yp: e.tensor_tensor(out=yp[:], in0=yp[:], in1=kf[:], op=ALU.subtract), reads=[yp.r, kf.r], writes=[yp.r])
            P.op("dve", lambda e, yp=yp: e.tensor_single_scalar(out=kf[:], in_=yp[:], scalar=0.0, op=ALU.is_lt), reads=[yp.r], writes=[kf.r])
            P.op("dve", lambda e, yp=yp: e.tensor_tensor(out=yp[:], in0=yp[:], in1=kf[:], op=ALU.add), reads=[yp.r, kf.r], writes=[yp.r])
            P.op("dve", lambda e, yp=yp: e.tensor_scalar(out=yp[:], in0=yp[:], scalar1=0.0, scalar2=0.999999, op0=ALU.max, op1=ALU.min), reads=[yp.r], writes=[yp.r])
            P.op("act", lambda e, tab=tab, yp=yp: e.activation(out=tab[:], in_=yp[:], func=AF.Sin, bias=nb[:, 0:1], scale=TWO_PI * 0.999999), reads=[yp.r, nb.r], writes=[tab.r])
        return self.tab[0], self.tab[1]


def rope_apply(P, dst, src, sin, cos, ti, nh, half, tmp):
    x1, x2, r1, r2, reads, writes = src
    cb = cos[:, ti, :].unsqueeze(1).broadcast_to([128, nh, half])
    sb_ = sin[:, ti, :].unsqueeze(1).broadcast_to([128, nh, half])
    t1, t2 = tmp
    rd = list(reads) + [sin.r, cos.r]
    P.op("dve", lambda e: e.tensor_tensor(out=t1[:, 0:nh, :], in0=x1, in1=cb, op=ALU.mult), reads=rd, writes=[t1.r])
    P.op("dve", lambda e: e.tensor_tensor(out=t2[:, 0:nh, :], in0=x2, in1=sb_, op=ALU.mult), reads=rd, writes=[t2.r])
    P.op("dve", lambda e: e.tensor_tensor(out=r1, in0=t1[:, 0:nh, :], in1=t2[:, 0:nh, :], op=ALU.subtract), reads=[t1.r, t2.r], writes=writes)
    P.op("dve", lambda e: e.tensor_tensor(out=t1[:, 0:nh, :], in0=x2, in1=cb, op=ALU.mult), reads=rd, writes=[t1.r])
    P.op("dve", lambda e: e.tensor_tensor(out=t2[:, 0:nh, :], in0=x1, in1=sb_, op=ALU.mult), reads=rd, writes=[t2.r])
    P.op("dve", lambda e: e.tensor_tensor(out=r2, in0=t1[:, 0:nh, :], in1=t2[:, 0:nh, :], op=ALU.add), reads=[t1.r, t2.r], writes=writes)


def build_A():
    nc = bass.Bass("TRN2", target_bir_lowering=False)
    P = Prog(nc)
    i_ = dict(
        x=dram_in(nc, "x", [2048, 2048]), pos=dram_in(nc, "pos", [128, NT], I32), mods=dram_in(nc, "mods", [4, 3, 2048]),
        w_in=dram_in(nc, "w_in", [2048, 3904]), qn_g=dram_in(nc, "qn_g", [128, 4]), w_uq=dram_in(nc, "w_uq", [512, 1536]),
        kvn_g=dram_in(nc, "kvn_g", [128, 2]), w_ukv=dram_in(nc, "w_ukv", [256, 2048]), ident=dram_in(nc, "ident", [128, 128]),
        ones=dram_in(nc, "ones", [128, 128]), invf=dram_in(nc, "invf", [1, 32]))
    o_ = dict(
        qT_sb=dram_out(nc, "qT_sb", [8, 128, 2048]), kT_sb=dram_out(nc, "kT_sb", [8, 128, 2048]), v_sb=dram_out(nc, "v_sb", [2048, 1024]),
        qT_mn=dram_out(nc, "qT_mn", [8, 128, 2048]), qT_mr=dram_out(nc, "qT_mr", [4, 128, 2048]), kT_mn=dram_out(nc, "kT_mn", [8, 128, 2048]),
        kT_r=dram_out(nc, "kT_r", [64, 2048]), v_m=dram_out(nc, "v_m", [2048, 1024]))
    emit_A(P, i_, o_)
    P.emit()
    return nc


def emit_A(P, i_, o_, nt=NT):
    H = HCtx(P, i_["mods"], 0, i_["ident"])
    ones = T(P, "ones", [128, 128]); ld(P, ones[:], i_["ones"], ones.r)
    qng = T(P, "qng", [128, 4]); ld(P, qng[:], i_["qn_g"], qng.r)
    kvg = T(P, "kvg", [128, 2]); ld(P, kvg[:], i_["kvn_g"], kvg.r)
    wuq = T(P, "wuq", [128, 4, 1536]); ld(P, wuq[:], i_["w_uq"].rearrange("(k p) c -> p k c", p=128), wuq.r)
    wukv = T(P, "wukv", [128, 2, 2048]); ld(P, wukv[:], i_["w_ukv"].rearrange("(k p) c -> p k c", p=128), wukv.r)
    rope = Rope(P, i_["invf"], 32)
    xs = Pool_(P, "xt", [128, 2048], 2)
    hts = Pool_(P, "ht", [128, 2048], 2)
    hT = T(P, "hT", [128, 16, 512], BF16)
    slabs = SlabLoader(P, "wslab", True)
    acc = Pool_(P, "accps", [128, 512], 4, psum=True)
    stg = Pool_(P, "stg", [128, 512], 3, dt=ADT)
    cqT = T(P, "cqT", [128, 4, 512]); ckvT = T(P, "ckvT", [128, 2, 512])
    sq = T(P, "sqT", [128, 4, 512])
    rstd = T(P, "rstdbc", [128, 512])
    rt1 = T(P, "rt1", [128, 8, 32]); rt2 = T(P, "rt2", [128, 8, 32])
    qr = T(P, "qr", [128, 8, 64]); qrr = T(P, "qrr", [128, 512])
    krt = T(P, "krt", [128, 64]); krr = T(P, "krr", [128, 64])
    Wv = i_["w_in"].rearrange("(k p) c -> p k c", p=128)
    s_sb = 1.0 / math.sqrt(128.0); s_mla = 1.0 / math.sqrt(192.0)
    for g in range(nt // 4):
        gc = slice(g * 512, (g + 1) * 512)
        sin, cos = rope.compute(i_["pos"][:, g * 4:(g + 1) * 4])
        for tl in range(4):
            ti = g * 4 + tl
            xt = xs.next(); ht = hts.next()
            i_["xload"](P, xt, ti)
            H.norm_mod(xt, ht)
            H.transpose_to(ht, 2048, lambda k, tl=tl: hT[:, k, tl * 128:(tl + 1) * 128], hT.r)
        for s in range(16):
            c0 = s * 256
            nc_ = min(256, 3904 - c0)
            sl = slabs.load(Wv[:, :, c0:c0 + nc_], nc_)
            if s < 8 or 12 <= s < 15:
                for half in range(2):
                    pb = acc.next()
                    for k in range(16):
                        P.op("pe", lambda e, pb=pb, sl=sl, k=k, half=half: e.matmul(pb[:, :], lhsT=sl[:, k, half * 128:(half + 1) * 128], rhs=hT[:, k, :], start=(k == 0), stop=(k == 15)),
                             reads=[sl.r, hT.r], writes=[pb.r])
                    if s < 8:
                        hh = (s % 4) * 2 + half
                        sg = stg.next()
                        H.evac(sg[:], pb[:, :], [pb.r], [sg.r], scale=(s_sb if s < 4 else None))
                        dst = o_["qT_sb"] if s < 4 else o_["kT_sb"]
                        st(P, dst[hh, :, gc], sg[:], sg.r)
                    elif s < 14:
                        kc = (s - 12) * 2 + half
                        H.evac(cqT[:, kc, :], pb[:, :], [pb.r], [cqT.r])
                    else:
                        H.evac(ckvT[:, half, :], pb[:, :], [pb.r], [ckvT.r])
            elif s < 12:
                for tl in range(4):
                    ti = g * 4 + tl
                    pb = acc.next()
                    for k in range(16):
                        P.op("pe", lambda e, pb=pb, sl=sl, k=k, tl=tl: e.matmul(pb[:, 0:256], lhsT=hT[:, k, tl * 128:(tl + 1) * 128], rhs=sl[:, k, :], start=(k == 0), stop=(k == 15)),
                             reads=[sl.r, hT.r], writes=[pb.r])
                    sg = stg.next()
                    H.evac(sg[:, 0:256], pb[:, 0:256], [pb.r], [sg.r])
                    st(P, o_["v_sb"][ti * 128:(ti + 1) * 128, (s - 8) * 256:(s - 7) * 256], sg[:, 0:256], sg.r)
            else:
                for tl in range(4):
                    ti = g * 4 + tl
                    pb = acc.next()
                    for k in range(16):
                        P.op("pe", lambda e, pb=pb, sl=sl, k=k, tl=tl: e.matmul(pb[:, 0:64], lhsT=hT[:, k, tl * 128:(tl + 1) * 128], rhs=sl[:, k, 0:64], start=(k == 0), stop=(k == 15)),
                             reads=[sl.r, hT.r], writes=[pb.r])
                    H.evac(krt[:], pb[:, 0:64], [pb.r], [krt.r])
                    rope_apply(P, None, (krt[:, 0:32].unsqueeze(1), krt[:, 32:64].unsqueeze(1), krr[:, 0:32].unsqueeze(1), krr[:, 32:64].unsqueeze(1), [krt.r], [krr.r]),
                               sin, cos, tl, 1, 32, (rt1, rt2))
                    sg = stg.next()
                    pt = H.tp.next()
                    P.op("pe", lambda e, pt=pt: e.transpose(pt[0:64, 0:128], krr[:, 0:64], H.id[:]), reads=[krr.r, H.id.r], writes=[pt.r])
                    H.evac(sg[0:64, 0:128], pt[0:64, 0:128], [pt.r], [sg.r])
                    st(P, o_["kT_r"][:, ti * 128:(ti + 1) * 128], sg[0:64, 0:128], sg.r)
        for (src, nk, gcol, width) in ((cqT, 4, qng, 512.0), (ckvT, 2, kvg, 256.0)):
            P.op("act", lambda e, src=src, nk=nk: e.activation(out=sq[:, 0:nk, :], in_=src[:, 0:nk, :], func=AF.Square), reads=[src.r], writes=[sq.r])
            pb = acc.next()
            for k in range(nk):
                P.op("pe", lambda e, pb=pb, k=k, nk=nk: e.matmul(pb[:, :], lhsT=ones[:], rhs=sq[:, k, :], start=(k == 0), stop=(k == nk - 1)), reads=[ones.r, sq.r], writes=[pb.r])
            P.op("dve", lambda e, pb=pb, width=width: e.tensor_scalar(out=rstd[:], in0=pb[:, :], scalar1=1.0 / width, scalar2=EPS, op0=ALU.mult, op1=ALU.add), reads=[pb.r], writes=[rstd.r])
            P.op("act", lambda e: e.activation(out=rstd[:], in_=rstd[:], func=AF.Sqrt), reads=[rstd.r], writes=[rstd.r])
            P.op("dve", lambda e: e.reciprocal(out=rstd[:], in_=rstd[:]), reads=[rstd.r], writes=[rstd.r])
            for k in range(nk):
                P.op("dve", lambda e, src=src, k=k, gcol=gcol: e.scalar_tensor_tensor(out=src[:, k, :], in0=src[:, k, :], scalar=gcol[:, k:k + 1], in1=rstd[:], op0=ALU.mult, op1=ALU.mult),
                     reads=[src.r, gcol.r, rstd.r], writes=[src.r])
        for hh in range(8):
            pb = acc.next()
            for k in range(4):
                P.op("pe", lambda e, pb=pb, k=k, hh=hh: e.matmul(pb[:, :], lhsT=wuq[:, k, hh * 192:hh * 192 + 128], rhs=cqT[:, k, :], start=(k == 0), stop=(k == 3)),
                     reads=[wuq.r, cqT.r], writes=[pb.r])
            sg = stg.next()
            H.evac(sg[:], pb[:, :], [pb.r], [sg.r], scale=s_mla)
            st(P, o_["qT_mn"][hh, :, gc], sg[:], sg.r)
        for hh in range(8):
            pb = acc.next()
            for k in range(2):
                P.op("pe", lambda e, pb=pb, k=k, hh=hh: e.matmul(pb[:, :], lhsT=wukv[:, k, hh * 256:hh * 256 + 128], rhs=ckvT[:, k, :], start=(k == 0), stop=(k == 1)),
                     reads=[wukv.r, ckvT.r], writes=[pb.r])
            sg = stg.next()
            H.evac(sg[:], pb[:, :], [pb.r], [sg.r])
            st(P, o_["kT_mn"][hh, :, gc], sg[:], sg.r)
        wuq_r = wuq[:].rearrange("p k (h c) -> p k h c", c=192)
        wukv_v = wukv[:].rearrange("p k (h c) -> p k h c", c=256)
        for tl in range(4):
            ti = g * 4 + tl
            tc_ = slice(tl * 128, (tl + 1) * 128)
            pb = acc.next()
            for k in range(4):
                P.op("pe", lambda e, pb=pb, k=k, tc_=tc_: e.matmul(pb[:, :].rearrange("p (h c) -> p h c", c=64), lhsT=cqT[:, k, tc_], rhs=wuq_r[:, k, :, 128:192], start=(k == 0), stop=(k == 3)),
                     reads=[wuq.r, cqT.r], writes=[pb.r])
            H.evac(qr[:], pb[:, :].rearrange("p (h c) -> p h c", c=64), [pb.r], [qr.r], scale=s_mla)
            qrr3 = qrr[:].rearrange("p (h c) -> p h c", c=64)
            rope_apply(P, None, (qr[:, :, 0:32], qr[:, :, 32:64], qrr3[:, :, 0:32], qrr3[:, :, 32:64], [qr.r], [qrr.r]), sin, cos, tl, 8, 32, (rt1, rt2))
            sg = stg.next()
            H.transpose_to(qrr, 512, lambda k, sg=sg: sg[:, k * 128:(k + 1) * 128], sg.r)
            st(P, o_["qT_mr"][:, :, ti * 128:(ti + 1) * 128].rearrange("k p t -> p k t"), sg[:].rearrange("p (k t) -> p k t", t=128), sg.r)
            for vh in range(2):
                pb = acc.next()
                for k in range(2):
                    P.op("pe", lambda e, pb=pb, k=k, tc_=tc_, vh=vh: e.matmul(pb[:, :].rearrange("p (h c) -> p h c", c=128), lhsT=ckvT[:, k, tc_], rhs=wukv_v[:, k, vh * 4:(vh + 1) * 4, 128:256], start=(k == 0), stop=(k == 1)),
                         reads=[wukv.r, ckvT.r], writes=[pb.r])
                sg = stg.next()
                H.evac(sg[:], pb[:, :], [pb.r], [sg.r])
                st(P, o_["v_m"][ti * 128:(ti + 1) * 128, vh * 512:(vh + 1) * 512], sg[:], sg.r)


class Masker:
    def __init__(self, P, masks_ap):
        self.P = P; self.masks = masks_ap
        self.pool = Pool_(P, "mask", [128, 512], 4)
        self.cur = None; self.key = None

    def get(self, kbp):
        if self.key != kbp:
            m = self.pool.next()
            ld(self.P, m[:], self.masks[kbp], m.r)
            self.cur, self.key = m, kbp
        return self.cur

    def apply(self, t, kbp):
        m = self.get(kbp)
        self.P.op("pool", lambda e: e.tensor_tensor(out=t[:], in0=t[:], in1=m[:], op=ALU.mult), reads=[t.r, m.r], writes=[t.r])


def build_B1(heads=8):
    nc = bass.Bass("TRN2", target_bir_lowering=False)
    P = Prog(nc)
    i_ = dict(qT=dram_in(nc, "qT", [8, 128, 2048]), kT=dram_in(nc, "kT", [8, 128, 8192]), v=dram_in(nc, "v", [8, 8192, 128]),
              negT1=dram_in(nc, "negT1", [128, 128]), negOnes=dram_in(nc, "negOnes", [128, 128]), masks=dram_in(nc, "masks", [16, 128, 512]))
    o_ = dict(oT=dram_out(nc, "oT", [8, 128, 2048]))
    emit_B1(P, i_, o_, heads)
    P.emit()
    return nc


def emit_B1(P, i_, o_, heads=8, sched=None):
    if sched is None:
        sched = [(16 * g + 15, 16 * g) for g in range(4)]
    MK = Masker(P, i_["masks"])
    nT1 = T(P, "nT1", [128, 128]); ld(P, nT1[:], i_["negT1"], nT1.r)
    nOn = T(P, "nOn", [128, 128]); ld(P, nOn[:], i_["negOnes"], nOn.r)
    nT1b = T(P, "nT1b", [128, 128], BF16); nOnb = T(P, "nOnb", [128, 128], BF16)
    P.op("dve", lambda e: e.tensor_copy(out=nT1b[:], in_=nT1[:]), reads=[nT1.r], writes=[nT1b.r])
    P.op("dve", lambda e: e.tensor_copy(out=nOnb[:], in_=nOn[:]), reads=[nOn.r], writes=[nOnb.r])
    sph = Pool_(P, "sph", [128, 512], 4, dt=BF16); spl = Pool_(P, "spl", [128, 512], 4, dt=BF16)
    sah = Pool_(P, "sah", [128, 512], 3, dt=BF16); sal = Pool_(P, "sal", [128, 512], 3, dt=BF16)
    KT = Pool_(P, "KT", [128, 8192], 2, dt=ADT); V = Pool_(P, "V", [128, 64, 128], 2, dt=ADT); QT = Pool_(P, "QT", [128, 512], 3, dt=ADT)
    zb = Pool_(P, "zb", [128, 512], 3, psum=True); lb = Pool_(P, "lb", [128, 512], 3, psum=True); ob = Pool_(P, "ob", [128, 512], 2, psum=True)
    et = Pool_(P, "et", [128, 512], 3); spt = Pool_(P, "spt", [128, 512], 4); At = Pool_(P, "At", [128, 512], 4, dt=ADT)
    sacc = Pool_(P, "sacc", [128, 512], 3); stg = Pool_(P, "ostg", [128, 512], 2, dt=o_["oT"].dtype)
    for h in range(heads):
        kt = KT.next(); v = V.next()
        P.dma("sp", [lambda e, kt=kt, h=h, c=c: e.dma_start(out=kt[:, c * 2048:(c + 1) * 2048], in_=i_["kT"][h, :, c * 2048:(c + 1) * 2048]) for c in range(4)], kt.r, writes=[kt.r])
        vv = i_["v"][h].rearrange("(k p) d -> p k d", p=128)
        P.dma("sp", [lambda e, v=v, vv=vv, c=c: e.dma_start(out=v[:, c * 16:(c + 1) * 16, :], in_=vv[:, c * 16:(c + 1) * 16, :]) for c in range(4)], v.r, writes=[v.r])
        units = []
        for g, (kb_max, span0) in enumerate(sched):
            for kb in range(kb_max, -1, -1):
                units.append(dict(g=g, kb=kb, kb_max=kb_max, span0=span0))
        gstate = {}

        def S1(u):
            g = u["g"]
            if u["kb"] == u["kb_max"]:
                qt = QT.next()
                ld(P, qt[:], i_["qT"][h, :, g * 512:(g + 1) * 512], qt.r)
                gstate[g] = dict(qt=qt, o=ob.next(), sa=None)
            gs_ = gstate[g]; qt = gs_["qt"]
            kblk = kt[:, u["kb"] * 128:(u["kb"] + 1) * 128]
            z = zb.next(); e_ = et.next(); sp = spt.next()
            u.update(kblk=kblk, sp=sp, qt=qt)
            P.op("pe", lambda e, z=z, kblk=kblk, qt=qt: e.matmul(z[:, :], lhsT=kblk, rhs=qt[:], start=True, stop=True), reads=[kt.r, qt.r], writes=[z.r])
            P.op("act", lambda e, z=z, e_=e_: e.activation(out=e_[:], in_=z[:, :], func=AF.Exp), reads=[z.r], writes=[e_.r])
            if u["kb"] >= u["span0"]:
                MK.apply(e_, u["kb"] - u["span0"])
            P.op("act", lambda e, sp=sp, e_=e_: e.activation(out=sp[:], in_=e_[:], func=AF.Ln, bias=1.0, scale=1.0), reads=[e_.r], writes=[sp.r])
            h_ = sph.next(); l_ = spl.next()
            u.update(sph=h_, spl=l_)
            P.op("dve", lambda e, sp=sp, h_=h_: e.tensor_copy(out=h_[:], in_=sp[:]), reads=[sp.r], writes=[h_.r])
            P.op("pool", lambda e, sp=sp, h_=h_, l_=l_: e.tensor_tensor(out=l_[:], in0=sp[:], in1=h_[:], op=ALU.subtract), reads=[sp.r, h_.r], writes=[l_.r])

        def S2(u):
            gs_ = gstate[u["g"]]; sa = gs_["sa"]; sp = u["sp"]; kblk = u["kblk"]; qt = u["qt"]
            lg = lb.next(); A = At.next()
            u["A"] = A
            P.op("pe", lambda e, lg=lg, kblk=kblk, qt=qt: e.matmul(lg[:, :], lhsT=kblk, rhs=qt[:], start=True, stop=False), reads=[kt.r, qt.r], writes=[lg.r])
            h_ = u["sph"]; l_ = u["spl"]
            P.op("pe", lambda e, lg=lg, h_=h_: e.matmul(lg[:, :], lhsT=nT1b[:], rhs=h_[:], start=False, stop=False), reads=[nT1b.r, h_.r], writes=[lg.r])
            P.op("pe", lambda e, lg=lg, l_=l_, last=(sa is None): e.matmul(lg[:, :], lhsT=nT1b[:], rhs=l_[:], start=False, stop=last), reads=[nT1b.r, l_.r], writes=[lg.r])
            if sa is not None:
                ah = gs_["sah"]; al = gs_["sal"]
                P.op("pe", lambda e, lg=lg, ah=ah: e.matmul(lg[:, :], lhsT=nOnb[:], rhs=ah[:], start=False, stop=False), reads=[nOnb.r, ah.r], writes=[lg.r])
                P.op("pe", lambda e, lg=lg, al=al: e.matmul(lg[:, :], lhsT=nOnb[:], rhs=al[:], start=False, stop=True), reads=[nOnb.r, al.r], writes=[lg.r])
            P.op("act", lambda e, lg=lg, A=A: e.activation(out=A[:], in_=lg[:, :], func=AF.Exp), reads=[lg.r], writes=[A.r])
            if u["kb"] >= u["span0"]:
                MK.apply(A, u["kb"] - u["span0"])
            if u["kb"] > 0:
                sn = sacc.next()
                if sa is None:
                    P.op("dve", lambda e, sn=sn, sp=sp: e.tensor_copy(out=sn[:], in_=sp[:]), reads=[sp.r], writes=[sn.r])
                else:
                    P.op("dve", lambda e, sn=sn, sa=sa, sp=sp: e.tensor_tensor(out=sn[:], in0=sa[:], in1=sp[:], op=ALU.add), reads=[sa.r, sp.r], writes=[sn.r])
                gs_["sa"] = sn
                ah = sah.next(); al = sal.next()
                P.op("dve", lambda e, sn=sn, ah=ah: e.tensor_copy(out=ah[:], in_=sn[:]), reads=[sn.r], writes=[ah.r])
                P.op("pool", lambda e, sn=sn, ah=ah, al=al: e.tensor_tensor(out=al[:], in0=sn[:], in1=ah[:], op=ALU.subtract), reads=[sn.r, ah.r], writes=[al.r])
                gs_["sah"] = ah; gs_["sal"] = al

        def S3(u):
            gs_ = gstate[u["g"]]; o = gs_["o"]; A = u["A"]; kb = u["kb"]; kb_max = u["kb_max"]; g = u["g"]
            P.op("pe", lambda e, o=o, kb=kb, A=A, kb_max=kb_max, v=v: e.matmul(o[:, :], lhsT=v[:, kb, :], rhs=A[:], start=(kb == kb_max), stop=(kb == 0)), reads=[v.r, A.r], writes=[o.r])
            if kb == 0:
                sg = stg.next()
                P.op("dve", lambda e, sg=sg, o=o: e.tensor_copy(out=sg[:], in_=o[:, :]), reads=[o.r], writes=[sg.r])
                st(P, o_["oT"][h, :, g * 512:(g + 1) * 512], sg[:], sg.r)

        n = len(units)
        for t in range(n + 2):
            if t < n:
                S1(units[t])
            if 0 <= t - 1 < n:
                S2(units[t - 1])
            if 0 <= t - 2 < n:
                S3(units[t - 2])


def build_B2(heads=8):
    nc = bass.Bass("TRN2", target_bir_lowering=False)
    P = Prog(nc)
    i_ = dict(qTn=dram_in(nc, "qTn", [8, 128, 2048]), qTr=dram_in(nc, "qTr", [4, 128, 2048]), kTn=dram_in(nc, "kTn", [8, 128, 8192]),
              kTr=dram_in(nc, "kTr", [64, 8192]), v=dram_in(nc, "v", [8, 8192, 128]), ones=dram_in(nc, "ones", [128, 128]), masks=dram_in(nc, "masks", [16, 128, 512]))
    o_ = dict(oT=dram_out(nc, "oT", [8, 128, 2048]))
    emit_B2(P, i_, o_, heads)
    P.emit()
    return nc


def emit_B2(P, i_, o_, heads=8, sched=None):
    if sched is None:
        sched = [(16 * g + 15, 16 * g) for g in range(4)]
    MK = Masker(P, i_["masks"])
    on32 = T(P, "on32", [128, 128]); ld(P, on32[:], i_["ones"], on32.r)
    on = T(P, "on", [128, 128], ADT)
    P.op("dve", lambda e: e.tensor_copy(out=on[:], in_=on32[:]), reads=[on32.r], writes=[on.r])
    ktr = T(P, "ktr", [64, 8192], ADT); ld(P, ktr[:], i_["kTr"], ktr.r)
    KT = Pool_(P, "KT", [128, 8192], 2, dt=ADT); V = Pool_(P, "V", [128, 64, 128], 2, dt=ADT); QT = Pool_(P, "QT", [128, 512], 3, dt=ADT); QR = Pool_(P, "QR", [64, 512], 3, dt=ADT)
    zb = Pool_(P, "zb", [128, 512], 4, psum=True); ob = Pool_(P, "ob", [128, 512], 2, psum=True); db = Pool_(P, "db", [128, 512], 2, psum=True)
    pt = Pool_(P, "pt", [128, 512], 5, dt=ADT); stg = Pool_(P, "ostg", [128, 512], 2, dt=o_["oT"].dtype); rec = Pool_(P, "rec", [128, 512], 2)
    for h in range(heads):
        kt = KT.next(); v = V.next()
        P.dma("sp", [lambda e, kt=kt, h=h, c=c: e.dma_start(out=kt[:, c * 2048:(c + 1) * 2048], in_=i_["kTn"][h, :, c * 2048:(c + 1) * 2048]) for c in range(4)], kt.r, writes=[kt.r])
        vv = i_["v"][h].rearrange("(k p) d -> p k d", p=128)
        P.dma("sp", [lambda e, v=v, vv=vv, c=c: e.dma_start(out=v[:, c * 16:(c + 1) * 16, :], in_=vv[:, c * 16:(c + 1) * 16, :]) for c in range(4)], v.r, writes=[v.r])
        units = []
        for g, (kb_max, span0) in enumerate(sched):
            for kb in range(kb_max, -1, -1):
                units.append(dict(g=g, kb=kb, kb_max=kb_max, span0=span0))
        gstate = {}

        def S1(u):
            g = u["g"]; gs = slice(g * 512, (g + 1) * 512)
            if u["kb"] == u["kb_max"]:
                qt = QT.next(); qr = QR.next()
                ld(P, qt[:], i_["qTn"][h, :, gs], qt.r)
                ld(P, qr[:], i_["qTr"][h // 2, (h % 2) * 64:(h % 2) * 64 + 64, gs], qr.r)
                gstate[g] = dict(qt=qt, qr=qr, o=ob.next(), d=db.next())
            qt = gstate[g]["qt"]; qr = gstate[g]["qr"]
            ks = slice(u["kb"] * 128, (u["kb"] + 1) * 128)
            z = zb.next(); p = pt.next()
            u["p"] = p
            P.op("pe", lambda e, z=z, qt=qt, ks=ks, kt=kt: e.matmul(z[:, :], lhsT=kt[:, ks], rhs=qt[:], start=True, stop=False), reads=[kt.r, qt.r], writes=[z.r])
            P.op("pe", lambda e, z=z, qr=qr, ks=ks: e.matmul(z[:, :], lhsT=ktr[:, ks], rhs=qr[:], start=False, stop=True), reads=[ktr.r, qr.r], writes=[z.r])
            P.op("act", lambda e, z=z, p=p: e.activation(out=p[:], in_=z[:, :], func=AF.Exp), reads=[z.r], writes=[p.r])
            if u["kb"] >= u["span0"]:
                MK.apply(p, u["kb"] - u["span0"])

        def S2(u):
            g = u["g"]; gs = slice(g * 512, (g + 1) * 512)
            o = gstate[g]["o"]; d = gstate[g]["d"]; p = u["p"]; kb = u["kb"]; kb_max = u["kb_max"]
            P.op("pe", lambda e, o=o, kb=kb, p=p, kb_max=kb_max, v=v: e.matmul(o[:, :], lhsT=v[:, kb, :], rhs=p[:], start=(kb == kb_max), stop=(kb == 0)), reads=[v.r, p.r], writes=[o.r])
            P.op("pe", lambda e, d=d, p=p, kb=kb, kb_max=kb_max: e.matmul(d[:, :], lhsT=on[:], rhs=p[:], start=(kb == kb_max), stop=(kb == 0)), reads=[on.r, p.r], writes=[d.r])
            if kb == 0:
                rc = rec.next(); sg = stg.next()
                P.op("dve", lambda e, rc=rc, d=d: e.reciprocal(out=rc[:], in_=d[:, :]), reads=[d.r], writes=[rc.r])
                P.op("dve", lambda e, sg=sg, o=o, rc=rc: e.tensor_tensor(out=sg[:], in0=o[:, :], in1=rc[:], op=ALU.mult), reads=[o.r, rc.r], writes=[sg.r])
                st(P, o_["oT"][h, :, gs], sg[:], sg.r)

        n = len(units)
        for t in range(n + 2):
            if t < n:
                S1(units[t])
            if 0 <= t - 2 < n:
                S2(units[t - 2])


def build_C1(mi=0):
    nc = bass.Bass("TRN2", target_bir_lowering=False)
    P = Prog(nc)
    i_ = dict(oT=dram_in(nc, "oT", [16, 128, 2048]), w_out=dram_in(nc, "w_out", [2048, 2048]), x=dram_in(nc, "x", [2048, 2048]), mods=dram_in(nc, "mods", [4, 3, 2048]))
    o_ = dict(x1=dram_out(nc, "x1", [2048, 2048]))
    emit_C1(P, i_, o_, mi)
    P.emit()
    return nc


def emit_C1(P, i_, o_, mi, nt=NT):
    lowp = i_["oT"].dtype == BF16
    G = T(P, "Gbc", [128, 2048]); ld(P, G[:], i_["mods"][mi, 2:3, :].broadcast_to([128, 2048]), G.r)
    oT = T(P, "oTg", [128, 16, 512], BF16 if lowp else F32)
    xs = Pool_(P, "xc", [128, 2048], 5)
    slabs = SlabLoader(P, "woslab", lowp)
    acc = Pool_(P, "c1ps", [128, 512], 4, psum=True)
    tmp = Pool_(P, "c1tmp", [128, 256], 2)
    Wv = i_["w_out"].rearrange("(k p) c -> p k c", p=128)
    for g in range(nt // 4):
        P.dma("sp", [lambda e, hu=hu, g=g: e.dma_start(out=oT[:, hu, :], in_=i_["oT"][hu, :, g * 512:(g + 1) * 512]) for hu in range(16)], oT.r, writes=[oT.r])
        xt = []
        for tl in range(4):
            x = xs.next(); ti = g * 4 + tl
            i_["xload"](P, x, ti)
            xt.append(x)
        for s in range(8):
            sl = slabs.load(Wv[:, :, s * 256:(s + 1) * 256])
            cs = slice(s * 256, (s + 1) * 256)
            for tl in range(4):
                pb = acc.next(); x = xt[tl]; tm = tmp.next()
                for k in range(16):
                    P.op("pe", lambda e, pb=pb, sl=sl, k=k, tl=tl: e.matmul(pb[:, 0:256], lhsT=oT[:, k, tl * 128:(tl + 1) * 128], rhs=sl[:, k, :], start=(k == 0), stop=(k == 15)),
                         reads=[oT.r, sl.r], writes=[pb.r])
                P.op("dve", lambda e, pb=pb, tm=tm, cs=cs: e.tensor_tensor(out=tm[:], in0=pb[:, 0:256], in1=G[:, cs], op=ALU.mult), reads=[pb.r, G.r], writes=[tm.r])
                P.op("pool", lambda e, x=x, tm=tm, cs=cs: e.tensor_tensor(out=x[:, cs], in0=x[:, cs], in1=tm[:], op=ALU.add), reads=[x.r, tm.r], writes=[x.r])
        for tl in range(4):
            ti = g * 4 + tl
            st(P, o_["x1"][ti * 128:(ti + 1) * 128, :], xt[tl][:], xt[tl].r, eng="act")


def build_C2(mi=1):
    nc = bass.Bass("TRN2", target_bir_lowering=False)
    P = Prog(nc)
    i_ = dict(x=dram_in(nc, "x", [2048, 2048]), mods=dram_in(nc, "mods", [4, 3, 2048]), w_q=dram_in(nc, "w_q", [2048, 2048]),
              k1T=dram_in(nc, "k1T", [128, 128]), k2T=dram_in(nc, "k2T", [128, 128]), ident=dram_in(nc, "ident", [128, 128]), iota16=dram_in(nc, "iota16", [1, 16]))
    o_ = dict(h=dram_out(nc, "h", [2048, 2048]), ids=dram_out(nc, "ids", [2048, 128], I32), gw=dram_out(nc, "gw", [2048, 128]))
    emit_C2(P, i_, o_, mi)
    P.emit()
    return nc


def emit_C2(P, i_, o_, mi, nt=NT):
    H = HCtx(P, i_["mods"], mi, i_["ident"])
    k1T = T(P, "k1T", [128, 128]); ld(P, k1T[:], i_["k1T"], k1T.r)
    k2T = T(P, "k2T", [128, 128]); ld(P, k2T[:], i_["k2T"], k2T.r)
    io = T(P, "iota16", [128, 16]); ld(P, io[:], i_["iota16"].broadcast_to([128, 16]), io.r)
    xs = Pool_(P, "xt", [128, 2048], 1)
    hts = Pool_(P, "ht", [128, 2048], 2)
    hT = T(P, "hT", [128, 16, 512])
    slabs = Pool_(P, "wqslab", [128, 16, 128], 2)
    acc = Pool_(P, "c2acc", [128, 512], 2, psum=True)
    scb = Pool_(P, "c2sc", [128, 512], 2, psum=True)
    qT = Pool_(P, "qTc", [128, 512], 2)
    Sp = Pool_(P, "S", [128, 4, 16, 128], 2)
    Wv = i_["w_q"].rearrange("(k p) c -> p k c", p=128)
    v = T(P, "tv", [128, 16, 16]); ix = T(P, "tix", [128, 16, 16], U32); ixf = T(P, "tixf", [128, 16, 16])
    rep16 = T(P, "trep16", [128, 16, 128]); rep8 = T(P, "trep8", [128, 8, 256]); dmy = T(P, "dmy", [128, 2])
    P.op("dve", lambda e: e.memset(dmy[:], 0.0), writes=[dmy.r])
    vr = [P.res(f"vr{i}") for i in range(16)]; ixr = [P.res(f"ixr{i}") for i in range(16)]; repr_ = [P.res(f"rr{i}") for i in range(16)]
    tr_ = [P.res(f"tr{i}") for i in range(8)]; pr_ = [P.res(f"pr{i}") for i in range(8)]; rr8 = [P.res(f"rr8{i}") for i in range(8)]
    cand = T(P, "cand", [128, 8, 256]); top = T(P, "top", [128, 8, 16]); pos = T(P, "pos", [128, 8, 16], U32)
    au = T(P, "au", [128, 8, 16], U32); bu = T(P, "bu", [128, 8, 16], U32); af = T(P, "af", [128, 8, 16]); bf = T(P, "bf", [128, 8, 16])
    eq = T(P, "eq", [128, 8, 16, 16]); sel1 = T(P, "sel1", [128, 8, 16]); sel2 = T(P, "sel2", [128, 8, 16])
    idf = T(P, "idf", [128, 8, 16]); idi = T(P, "idi", [128, 8, 16], I32)
    gm = T(P, "gm", [128, 8, 16]); gs = T(P, "gs", [128, 8]); gw = T(P, "gw", [128, 8, 16])
    for g in range(nt // 4):
        S = Sp.next()
        for tl in range(4):
            ti = g * 4 + tl
            xt = xs.next(); ht = hts.next()
            ld(P, xt[:], i_["x"][ti * 128:(ti + 1) * 128, :], xt.r)
            H.norm_mod(xt, ht)
            H.transpose_to(ht, 2048, lambda k, tl=tl: hT[:, k, tl * 128:(tl + 1) * 128], hT.r)
            st(P, o_["h"][ti * 128:(ti + 1) * 128, :], ht[:], ht.r)
        for s in range(8):
            for half in range(2):
                sl = slabs.next()
                ld(P, sl[:], Wv[:, :, (s * 2 + half) * 128:(s * 2 + half + 1) * 128], sl.r)
                pb = acc.next(); q = qT.next()
                for k in range(16):
                    P.op("pe", lambda e, pb=pb, sl=sl, k=k, half=half: e.matmul(pb[:, :], lhsT=sl[:, k, :], rhs=hT[:, k, :], start=(k == 0), stop=(k == 15)),
                         reads=[sl.r, hT.r], writes=[pb.r])
                H.evac(q[:], pb[:, :], [pb.r], [q.r])
                sb_ = scb.next(); kT = k1T if half == 0 else k2T
                for tl in range(4):
                    P.op("pe", lambda e, sb_=sb_, q=q, tl=tl, kT=kT: e.matmul(sb_[:, tl * 128:(tl + 1) * 128], lhsT=q[:, tl * 128:(tl + 1) * 128], rhs=kT[:], start=True, stop=True),
                         reads=[q.r, kT.r], writes=[sb_.r])
                H.evac(S[:, :, s * 2 + half, :], sb_[:, :].rearrange("p (t n) -> p t n", n=128), [sb_.r], [S.r])
        for tl in range(4):
            ti = g * 4 + tl
            for cc in range(16):
                P.op("dve", lambda e, cc=cc, tl=tl, S=S: e.max(out=v[:, cc, 0:8], in_=S[:, tl, cc, :]), reads=[S.r], writes=[vr[cc]])
            for cc in range(16):
                P.op("dve", lambda e, cc=cc, tl=tl, S=S: e.max_index(out=ix[:, cc, 0:8], in_max=v[:, cc, 0:8], in_values=S[:, tl, cc, :]), reads=[S.r, vr[cc]], writes=[ixr[cc]])
            for cc in range(16):
                P.op("dve", lambda e, cc=cc, tl=tl, S=S: e.match_replace(out=rep16[:, cc, :], in_to_replace=v[:, cc, 0:8], in_values=S[:, tl, cc, :], imm_value=-1e30), reads=[S.r, vr[cc]], writes=[repr_[cc]])
            for cc in range(16):
                P.op("dve", lambda e, cc=cc: e.max(out=v[:, cc, 8:16], in_=rep16[:, cc, :]), reads=[repr_[cc]], writes=[vr[cc]])
            for cc in range(16):
                P.op("dve", lambda e, cc=cc: e.max_index(out=ix[:, cc, 8:16], in_max=v[:, cc, 8:16], in_values=rep16[:, cc, :]), reads=[repr_[cc], vr[cc]], writes=[ixr[cc]])
            P.op("dve", lambda e: e.engine_nop() if False else e.tensor_copy(out=ixf[:, 0:1, 0:1], in_=ix[:, 0:1, 0:1]), reads=vr + ixr, writes=[v.r, ix.r])
            P.op("dve", lambda e: e.tensor_copy(out=ixf[:], in_=ix[:]), reads=[ix.r], writes=[ixf.r])
            v4 = v[:].rearrange("p (h j) k -> p h j k", j=2); i4 = ixf[:].rearrange("p (h j) k -> p h j k", j=2)
            v1, v2, i1, i2 = v4[:, :, 0, :], v4[:, :, 1, :], i4[:, :, 0, :], i4[:, :, 1, :]
            c4 = cand[:].rearrange("p h (a b) -> p h a b", b=16)
            P.op("dve", lambda e, v1=v1, v2=v2, c4=c4: e.tensor_tensor(out=c4, in0=v1.unsqueeze(3).broadcast_to([128, 8, 16, 16]), in1=v2.unsqueeze(2).broadcast_to([128, 8, 16, 16]), op=ALU.add),
                 reads=[v.r], writes=[cand.r])
            P.op("dve", lambda e: e.tensor_copy(out=dmy[:, 0:1], in_=dmy[:, 1:2]), reads=[v.r, ix.r, dmy.r], writes=vr + ixr + repr_ + [dmy.r])
            for h in range(8):
                P.op("dve", lambda e, h=h: e.max(out=top[:, h, 0:8], in_=cand[:, h, :]), reads=[cand.r], writes=[tr_[h]])
            for h in range(8):
                P.op("dve", lambda e, h=h: e.max_index(out=pos[:, h, 0:8], in_max=top[:, h, 0:8], in_values=cand[:, h, :]), reads=[cand.r, tr_[h]], writes=[pr_[h]])
            for h in range(8):
                P.op("dve", lambda e, h=h: e.match_replace(out=rep8[:, h, :], in_to_replace=top[:, h, 0:8], in_values=cand[:, h, :], imm_value=-1e30), reads=[cand.r, tr_[h]], writes=[rr8[h]])
            for h in range(8):
                P.op("dve", lambda e, h=h: e.max(out=top[:, h, 8:16], in_=rep8[:, h, :]), reads=[rr8[h]], writes=[tr_[h]])
            for h in range(8):
                P.op("dve", lambda e, h=h: e.max_index(out=pos[:, h, 8:16], in_max=top[:, h, 8:16], in_values=rep8[:, h, :]), reads=[rr8[h], tr_[h]], writes=[pr_[h]])
            P.op("dve", lambda e: e.tensor_copy(out=au[:, 0:1, 0:1], in_=pos[:, 0:1, 0:1]), reads=tr_ + pr_, writes=[top.r, pos.r])
            P.op("dve", lambda e: e.tensor_single_scalar(out=au[:], in_=pos[:], scalar=4, op=ALU.logical_shift_right), reads=[pos.r], writes=[au.r])
            P.op("dve", lambda e: e.tensor_single_scalar(out=bu[:], in_=pos[:], scalar=15, op=ALU.bitwise_and), reads=[pos.r], writes=[bu.r])
            P.op("dve", lambda e: e.tensor_copy(out=af[:], in_=au[:]), reads=[au.r], writes=[af.r])
            P.op("dve", lambda e: e.tensor_copy(out=bf[:], in_=bu[:]), reads=[bu.r], writes=[bf.r])
            iob = io[:].unsqueeze(1).unsqueeze(1).broadcast_to([128, 8, 16, 16])
            for (xf, tab, sel) in ((af, i1, sel1), (bf, i2, sel2)):
                P.op("dve", lambda e, xf=xf: e.tensor_tensor(out=eq[:], in0=xf[:].unsqueeze(3).broadcast_to([128, 8, 16, 16]), in1=iob, op=ALU.is_equal), reads=[xf.r, io.r], writes=[eq.r])
                P.op("dve", lambda e, tab=tab: e.tensor_tensor(out=eq[:], in0=eq[:], in1=tab.unsqueeze(2).broadcast_to([128, 8, 16, 16]), op=ALU.mult), reads=[eq.r, ixf.r], writes=[eq.r])
                P.op("dve", lambda e, sel=sel: e.tensor_reduce(out=sel[:], in_=eq[:], axis=AX.X, op=ALU.add), reads=[eq.r], writes=[sel.r])
            P.op("dve", lambda e: e.scalar_tensor_tensor(out=idf[:], in0=sel1[:], scalar=128.0, in1=sel2[:], op0=ALU.mult, op1=ALU.add), reads=[sel1.r, sel2.r], writes=[idf.r])
            P.op("dve", lambda e: e.tensor_copy(out=idi[:], in_=idf[:]), reads=[idf.r], writes=[idi.r])
            st(P, o_["ids"][ti * 128:(ti + 1) * 128, :], idi[:].rearrange("p h k -> p (h k)"), idi.r)
            P.op("dve", lambda e: e.tensor_tensor(out=gm[:], in0=top[:], in1=top[:, :, 0:1].broadcast_to([128, 8, 16]), op=ALU.subtract), reads=[top.r], writes=[gm.r])
            P.op("act", lambda e: e.activation(out=gm[:], in_=gm[:], func=AF.Exp), reads=[gm.r], writes=[gm.r])
            P.op("dve", lambda e: e.tensor_reduce(out=gs[:], in_=gm[:], axis=AX.X, op=ALU.add), reads=[gm.r], writes=[gs.r])
            P.op("dve", lambda e: e.reciprocal(out=gs[:], in_=gs[:]), reads=[gs.r], writes=[gs.r])
            P.op("dve", lambda e: e.tensor_tensor(out=gw[:], in0=gm[:], in1=gs[:].unsqueeze(2).broadcast_to([128, 8, 16]), op=ALU.mult), reads=[gm.r, gs.r], writes=[gw.r])
            st(P, o_["gw"][ti * 128:(ti + 1) * 128, :], gw[:].rearrange("p h k -> p (h k)"), gw.r)
            P.op("dve", lambda e: e.tensor_copy(out=dmy[:, 0:1], in_=dmy[:, 1:2]), reads=[top.r, pos.r, dmy.r], writes=tr_ + pr_ + rr8 + [dmy.r])


def build_C3(mi=1, ntiles=NT, final=False):
    nc = bass.Bass("TRN2", target_bir_lowering=False)
    P = Prog(nc)
    i_ = dict(x=dram_in(nc, "x", [2048, 2048]), h=dram_in(nc, "h", [2048, 2048]), ids=dram_in(nc, "ids", [2048, 128], I32), gw=dram_in(nc, "gw", [2048, 128]),
              mods=dram_in(nc, "mods", [4, 3, 2048]), u=dram_in(nc, "u", [16384, 2048]), v=dram_in(nc, "v", [16384, 2048]))
    if final:
        i_["fin_g"] = dram_in(nc, "fin_g", [1, 2048])
    o_ = dict(x2=dram_out(nc, "x2", [2048, 2048]))
    emit_C3(P, i_, o_, mi, ntiles)
    P.emit()
    return nc


def emit_PC(P, src, dst, half):
    a = Pool_(P, "pcin", [128, 8192], 2); b = Pool_(P, "pcout", [128, 8192], 2, dt=BF16)
    sv = src.rearrange("(n p r) d -> n p (r d)", p=128, r=4)
    dv = dst[:, half * 2048:(half + 1) * 2048].rearrange("(n p r) d -> n p r d", p=128, r=4)
    for n in range(32):
        t = a.next(); o = b.next()
        ld(P, t[:], sv[n], t.r)
        eng = ("act", "dve", "pool")[n % 3]
        if eng == "act":
            P.op("act", lambda e, t=t, o=o: e.copy(out=o[:], in_=t[:]), reads=[t.r], writes=[o.r])
        else:
            P.op(eng, lambda e, t=t, o=o: e.tensor_copy(out=o[:], in_=t[:]), reads=[t.r], writes=[o.r])
        st(P, dv[n], o[:].rearrange("p (r d) -> p r d", r=4), o.r, eng="act")


def emit_C3f(P, i_, o_, mi, ntiles=NT):
    G = T(P, "Gbc", [128, 2048]); ld(P, G[:], i_["mods"][mi, 2:3, :].broadcast_to([128, 2048]), G.r)
    xs = Pool_(P, "x3", [128, 2048], 2); hs = Pool_(P, "h3", [128, 2048], 2)
    idp = Pool_(P, "id3", [128, 128], 2, dt=I32); gwp = Pool_(P, "gw3", [128, 128], 2)
    gb = Pool_(P, "gath", [128, 4096], 6, dt=BF16)
    junk = T(P, "junk3", [128, 2048])
    acol = Pool_(P, "acol", [128, 1], 8); gcol = Pool_(P, "gcol", [128, 1], 8); wcol = Pool_(P, "wcol", [128, 1], 8)
    accp = Pool_(P, "acc3", [128, 2048], 2)
    idb = T(P, "idb3", [128, 128]); ld(P, idb[:], i_["ident"], idb.r)
    dg = Pool_(P, "diag3", [128, 128], 6, dt=BF16)
    pacc = [T(P, f"pacc{k}", [128, 512], psum=True) for k in range(4)]
    final = "fin_g" in i_
    if final:
        gF = T(P, "gF", [128, 2048]); ld(P, gF[:], i_["fin_g"].broadcast_to([128, 2048]), gF.r)
        fss = Pool_(P, "fss", [128, 1], 2)
    for ti in range(ntiles):
        rs = slice(ti * 128, (ti + 1) * 128)
        x = xs.next(); h = hs.next(); idt = idp.next(); gwt = gwp.next(); acc = accp.next()
        ld(P, h[:], i_["h"][rs, :], h.r); ld(P, idt[:], i_["ids"][rs, :], idt.r); ld(P, gwt[:], i_["gw"][rs, :], gwt.r); ld(P, x[:], i_["x"][rs, :], x.r)
        for s in range(128):
            ug = gb.next(); ac = acol.next(); gc_ = gcol.next(); wc = wcol.next(); d_ = dg.next()
            P.dma("pool", lambda e, ug=ug, idt=idt, s=s: e.indirect_dma_start(out=ug[:], out_offset=None, in_=i_["uv"], in_offset=bass.IndirectOffsetOnAxis(ap=idt[:, s:s + 1], axis=0)),
                  ug.r, reads=[idt.r], writes=[ug.r])
            P.op("dve", lambda e, ug=ug, h=h, ac=ac: e.scalar_tensor_tensor(out=junk[:], in0=ug[:, 0:2048], scalar=1.0, in1=h[:], op0=ALU.mult, op1=ALU.mult, accum_out=ac[:, 0:1]),
                 reads=[ug.r, h.r], writes=[junk.r, ac.r])
            P.op("act", lambda e, ac=ac, gc_=gc_: e.activation(out=gc_[:], in_=ac[:], func=AF.Gelu), reads=[ac.r], writes=[gc_.r])
            P.op("dve", lambda e, gc_=gc_, wc=wc, gwt=gwt, s=s: e.tensor_tensor(out=wc[:], in0=gc_[:], in1=gwt[:, s:s + 1], op=ALU.mult), reads=[gc_.r, gwt.r], writes=[wc.r])
            P.op("act", lambda e, d_=d_, wc=wc: e.activation(out=d_[:], in_=idb[:], func=AF.Copy, scale=wc[:, 0:1]), reads=[idb.r, wc.r], writes=[d_.r])
            for k in range(4):
                P.op("pe", lambda e, d_=d_, ug=ug, k=k, s=s: e.matmul(pacc[k][:, :], lhsT=d_[:], rhs=ug[:, 2048 + k * 512:2048 + (k + 1) * 512], start=(s == 0), stop=(s == 127)),
                     reads=[d_.r, ug.r], writes=[pacc[k].r])
        for k in range(4):
            ks = slice(k * 512, (k + 1) * 512)
            P.op("dve", lambda e, acc=acc, k=k, ks=ks: e.tensor_tensor(out=acc[:, ks], in0=pacc[k][:, :], in1=G[:, ks], op=ALU.mult), reads=[pacc[k].r, G.r], writes=[acc.r])
        P.op("pool", lambda e, acc=acc, x=x: e.tensor_tensor(out=acc[:], in0=acc[:], in1=x[:], op=ALU.add), reads=[acc.r, x.r], writes=[acc.r])
        if final:
            ss = fss.next()
            P.op("act", lambda e, acc=acc, ss=ss: e.activation(out=junk[:], in_=acc[:], func=AF.Square, accum_out=ss[:]), reads=[acc.r], writes=[junk.r, ss.r])
            P.op("dve", lambda e, ss=ss: e.tensor_scalar(out=ss[:], in0=ss[:], scalar1=1.0 / 2048.0, scalar2=1e-6, op0=ALU.mult, op1=ALU.add), reads=[ss.r], writes=[ss.r])
            P.op("act", lambda e, ss=ss: e.activation(out=ss[:], in_=ss[:], func=AF.Sqrt), reads=[ss.r], writes=[ss.r])
            P.op("dve", lambda e, ss=ss: e.reciprocal(out=ss[:], in_=ss[:]), reads=[ss.r], writes=[ss.r])
            P.op("dve", lambda e, acc=acc, ss=ss: e.scalar_tensor_tensor(out=acc[:], in0=acc[:], scalar=ss[:, 0:1], in1=gF[:], op0=ALU.mult, op1=ALU.mult), reads=[acc.r, ss.r, gF.r], writes=[acc.r])
        st(P, o_["x2"][rs, :], acc[:], acc.r, eng="act")


def emit_C3(P, i_, o_, mi, ntiles=NT):
    bf = i_["u"].dtype == BF16
    G = T(P, "Gbc", [128, 2048]); ld(P, G[:], i_["mods"][mi, 2:3, :].broadcast_to([128, 2048]), G.r)
    xs = Pool_(P, "x3", [128, 2048], 2); hs = Pool_(P, "h3", [128, 2048], 2)
    idp = Pool_(P, "id3", [128, 128], 2, dt=I32); gwp = Pool_(P, "gw3", [128, 128], 2)
    gb = Pool_(P, "gath", [128, 2048], 8 if bf else 6, dt=(BF16 if bf else F32))
    junk = T(P, "junk3", [128, 2048])
    actp = Pool_(P, "act3", [128, 128], 2); wp = Pool_(P, "w3", [128, 128], 2)
    accp = Pool_(P, "acc3", [128, 2048], 2)
    if bf:
        idb = T(P, "idb3", [128, 128]); ld(P, idb[:], i_["ident"], idb.r)
        dg = Pool_(P, "diag3", [128, 128], 4, dt=BF16)
        pacc = [T(P, f"pacc{k}", [128, 512], psum=True) for k in range(4)]
    final = "fin_g" in i_
    if final:
        gF = T(P, "gF", [128, 2048]); ld(P, gF[:], i_["fin_g"].broadcast_to([128, 2048]), gF.r)
        fss = Pool_(P, "fss", [128, 1], 2)
    for ti in range(ntiles):
        rs = slice(ti * 128, (ti + 1) * 128)
        x = xs.next(); h = hs.next(); idt = idp.next(); gwt = gwp.next(); act = actp.next(); w = wp.next(); acc = accp.next()
        ld(P, h[:], i_["h"][rs, :], h.r); ld(P, idt[:], i_["ids"][rs, :], idt.r); ld(P, gwt[:], i_["gw"][rs, :], gwt.r); ld(P, x[:], i_["x"][rs, :], x.r)
        for s in range(128):
            ug = gb.next()
            P.dma("pool", lambda e, ug=ug, idt=idt, s=s: e.indirect_dma_start(out=ug[:], out_offset=None, in_=i_["u"], in_offset=bass.IndirectOffsetOnAxis(ap=idt[:, s:s + 1], axis=0)),
                  ug.r, reads=[idt.r], writes=[ug.r])
            P.op("dve", lambda e, ug=ug, h=h, act=act, s=s: e.scalar_tensor_tensor(out=junk[:], in0=ug[:], scalar=1.0, in1=h[:], op0=ALU.mult, op1=ALU.mult, accum_out=act[:, s:s + 1]),
                 reads=[ug.r, h.r], writes=[junk.r, act.r])
        P.op("act", lambda e, act=act: e.activation(out=act[:], in_=act[:], func=AF.Gelu), reads=[act.r], writes=[act.r])
        P.op("dve", lambda e, act=act, w=w, gwt=gwt: e.tensor_tensor(out=w[:], in0=act[:], in1=gwt[:], op=ALU.mult), reads=[act.r, gwt.r], writes=[w.r])
        for s in range(128):
            vg = gb.next()
            P.dma("pool", lambda e, vg=vg, idt=idt, s=s: e.indirect_dma_start(out=vg[:], out_offset=None, in_=i_["v"], in_offset=bass.IndirectOffsetOnAxis(ap=idt[:, s:s + 1], axis=0)),
                  vg.r, reads=[idt.r], writes=[vg.r])
            if bf:
                d_ = dg.next()
                P.op("act", lambda e, d_=d_, w=w, s=s: e.activation(out=d_[:], in_=idb[:], func=AF.Copy, scale=w[:, s:s + 1]), reads=[idb.r, w.r], writes=[d_.r])
                for k in range(4):
                    P.op("pe", lambda e, d_=d_, vg=vg, k=k, s=s: e.matmul(pacc[k][:, :], lhsT=d_[:], rhs=vg[:, k * 512:(k + 1) * 512], start=(s == 0), stop=(s == 127)),
                         reads=[d_.r, vg.r], writes=[pacc[k].r])
            elif s == 0:
                P.op("dve", lambda e, vg=vg, w=w, acc=acc: e.tensor_scalar(out=acc[:], in0=vg[:], scalar1=w[:, 0:1], scalar2=None, op0=ALU.mult), reads=[vg.r, w.r], writes=[acc.r])
            else:
                P.op("dve", lambda e, vg=vg, w=w, acc=acc, s=s: e.scalar_tensor_tensor(out=acc[:], in0=vg[:], scalar=w[:, s:s + 1], in1=acc[:], op0=ALU.mult, op1=ALU.add),
                     reads=[vg.r, w.r, acc.r], writes=[acc.r])
        if bf:
            for k in range(4):
                ks = slice(k * 512, (k + 1) * 512)
                P.op("dve", lambda e, acc=acc, k=k, ks=ks: e.tensor_tensor(out=acc[:, ks], in0=pacc[k][:, :], in1=G[:, ks], op=ALU.mult), reads=[pacc[k].r, G.r], writes=[acc.r])
        else:
            P.op("dve", lambda e, acc=acc: e.tensor_tensor(out=acc[:], in0=acc[:], in1=G[:], op=ALU.mult), reads=[acc.r, G.r], writes=[acc.r])
        P.op("pool", lambda e, acc=acc, x=x: e.tensor_tensor(out=acc[:], in0=acc[:], in1=x[:], op=ALU.add), reads=[acc.r, x.r], writes=[acc.r])
        if final:
            ss = fss.next()
            P.op("act", lambda e, acc=acc, ss=ss: e.activation(out=junk[:], in_=acc[:], func=AF.Square, accum_out=ss[:]), reads=[acc.r], writes=[junk.r, ss.r])
            P.op("dve", lambda e, ss=ss: e.tensor_scalar(out=ss[:], in0=ss[:], scalar1=1.0 / 2048.0, scalar2=1e-6, op0=ALU.mult, op1=ALU.add), reads=[ss.r], writes=[ss.r])
            P.op("act", lambda e, ss=ss: e.activation(out=ss[:], in_=ss[:], func=AF.Sqrt), reads=[ss.r], writes=[ss.r])
            P.op("dve", lambda e, ss=ss: e.reciprocal(out=ss[:], in_=ss[:]), reads=[ss.r], writes=[ss.r])
            P.op("dve", lambda e, acc=acc, ss=ss: e.scalar_tensor_tensor(out=acc[:], in0=acc[:], scalar=ss[:, 0:1], in1=gF[:], op0=ALU.mult, op1=ALU.mult), reads=[acc.r, ss.r, gF.r], writes=[acc.r])
        st(P, o_["x2"][rs, :], acc[:], acc.r, eng="act")


def build_D1():
    nc = bass.Bass("TRN2", target_bir_lowering=False)
    P = Prog(nc)
    i_ = dict(x=dram_in(nc, "x", [2048, 2048]), pos=dram_in(nc, "pos", [128, NT], I32), mods=dram_in(nc, "mods", [4, 3, 2048]),
              w_in=dram_in(nc, "w_in", [2048, 3632]), ident=dram_in(nc, "ident", [128, 128]), invf=dram_in(nc, "invf", [1, 16]))
    o_ = dict(qT=dram_out(nc, "qT", [16, 128, 2048]), kcT=dram_out(nc, "kcT", [2, 128, 2048]), ksT=dram_out(nc, "ksT", [2, 128, 2048]), kwT=dram_out(nc, "kwT", [2, 128, 2048]),
              vc=dram_out(nc, "vc", [2048, 256]), vs=dram_out(nc, "vs", [2048, 256]), vw=dram_out(nc, "vw", [2048, 256]), gT=dram_out(nc, "gT", [48, 2048]))
    emit_D1(P, i_, o_, 2)
    P.emit()
    return nc


def emit_D1(P, i_, o_, mi, nt, part):
    H = HCtx(P, i_["mods"], mi, i_["ident"])
    rope = Rope(P, i_["invf"], 16)
    xs = Pool_(P, "xt", [128, 2048], 2)
    hts = Pool_(P, "ht", [128, 2048], 2)
    lowp = (part == "k")
    hT = T(P, "hT", [128, 16, 512], BF16 if lowp else F32)
    slabs = SlabLoader(P, "wslab", lowp)
    acc = Pool_(P, "accps", [128, 512], 4, psum=True)
    tm = Pool_(P, "tm", [128, 2, 128], 3)
    rr = T(P, "rr", [128, 2, 32]); rt1 = T(P, "rt1", [128, 2, 16]); rt2 = T(P, "rt2", [128, 2, 16])
    stg = Pool_(P, "stg", [128, 2, 512], 2)
    stgb = Pool_(P, "stgb", [128, 2, 512], 2, dt=ADT)
    vst = Pool_(P, "vst", [128, 256], 3, dt=ADT)
    tmb = Pool_(P, "tmb", [128, 2, 128], 2, dt=ADT)
    gst = T(P, "gst", [128, 48]); gTs = T(P, "gTs", [48, 512])
    Wv = i_["w_in"].rearrange("(k p) c -> p k c", p=128)
    s_nsa = 1.0 / math.sqrt(128.0)
    allkinds = ["q"] * 8 + ["kc", "vc", "ks", "vs", "kw", "vw", "gl"]
    sel = [(s_, k) for s_, k in enumerate(allkinds) if (k in ("q", "gl")) == (part == "q")]
    if part == "k":
        zt = T(P, "zpad", [128, 128], ADT)
        P.op("pool", lambda e: e.memset(zt[:], 0.0), writes=[zt.r])
        for nm in ("kw", "vw"):
            for g2 in range(2):
                st(P, o_[nm][g2][nt * 128:nt * 128 + 128, :], zt[:], zt.r)
    for g in range(nt // 4):
        gc = slice(g * 512, (g + 1) * 512)
        sin, cos = rope.compute(i_["pos"][:, g * 4:(g + 1) * 4])
        for tl in range(4):
            ti = g * 4 + tl
            xt = xs.next(); ht = hts.next()
            i_["xload"](P, xt, ti)
            H.norm_mod(xt, ht)
            H.transpose_to(ht, 2048, lambda k, tl=tl: hT[:, k, tl * 128:(tl + 1) * 128], hT.r)
        for s, kind in sel:
            c0 = s * 256
            ncol = min(256, 3632 - c0)
            sl = slabs.load(Wv[:, :, c0:c0 + ncol], ncol)
            sg = (stgb.next() if kind == "ks" else stg.next()) if kind in ("q", "kc", "ks", "vc") else None
            for tl in range(4):
                ti = g * 4 + tl
                pb = acc.next()
                for k in range(16):
                    P.op("pe", lambda e, pb=pb, sl=sl, k=k, tl=tl, ncol=ncol: e.matmul(pb[:, 0:ncol], lhsT=hT[:, k, tl * 128:(tl + 1) * 128], rhs=sl[:, k, 0:ncol], start=(k == 0), stop=(k == 15)),
                         reads=[sl.r, hT.r], writes=[pb.r])
                if kind in ("q", "kc", "ks", "kw", "vc"):
                    t_ = tm.next()
                    H.evac(t_[:], pb[:, 0:256].rearrange("p (h d) -> p h d", d=128), [pb.r], [t_.r], scale=(s_nsa if kind == "q" else None))
                    if kind != "vc":
                        rope_apply(P, None, (t_[:, :, 0:16], t_[:, :, 16:32], rr[:, :, 0:16], rr[:, :, 16:32], [t_.r], [rr.r]), sin, cos, tl, 2, 16, (rt1, rt2))
                        P.op("pool", lambda e, t_=t_: e.tensor_copy(out=t_[:, :, 0:32], in_=rr[:]), reads=[rr.r], writes=[t_.r])
                    if kind == "kw":
                        tb_ = tmb.next()
                        P.op("act", lambda e, tb_=tb_, t_=t_: e.copy(out=tb_[:], in_=t_[:]), reads=[t_.r], writes=[tb_.r])
                        for g2 in range(2):
                            st(P, o_["kw"][g2][ti * 128:(ti + 1) * 128, :], tb_[:, g2, :], tb_.r)
                    else:
                        pt = H.tp.next()
                        for hh in range(2):
                            P.op("pe", lambda e, pt=pt, t_=t_, hh=hh: e.transpose(pt[:, hh * 128:(hh + 1) * 128], t_[:, hh, :], H.id[:]), reads=[t_.r, H.id.r], writes=[pt.r])
                        H.evac(sg[:, :, tl * 128:(tl + 1) * 128], pt[:, 0:256].rearrange("p (h t) -> p h t", t=128), [pt.r], [sg.r])
                elif kind == "gl":
                    P.op("act", lambda e, pb=pb: e.activation(out=gst[:], in_=pb[:, 0:48], func=AF.Sigmoid), reads=[pb.r], writes=[gst.r])
                    pt = H.tp.next()
                    P.op("pe", lambda e, pt=pt: e.transpose(pt[0:48, 0:128], gst[:, 0:48], H.id[:]), reads=[gst.r, H.id.r], writes=[pt.r])
                    H.evac(gTs[:, tl * 128:(tl + 1) * 128], pt[0:48, 0:128], [pt.r], [gTs.r])
                elif kind == "vs":
                    vt = vst.next()
                    H.evac(vt[:], pb[:, 0:256], [pb.r], [vt.r])
                    st(P, o_["vs"][ti * 128:(ti + 1) * 128, :], vt[:], vt.r)
                else:
                    vt = vst.next()
                    H.evac(vt[:], pb[:, 0:256], [pb.r], [vt.r])
                    for g2 in range(2):
                        st(P, o_["vw"][g2][ti * 128:(ti + 1) * 128, :], vt[:, g2 * 128:(g2 + 1) * 128], vt.r)
            if kind == "q":
                st(P, o_["qT"][2 * s:2 * s + 2, :, gc].rearrange("h p t -> p h t"), sg[:], sg.r)
                sgb = stgb.next()
                P.op("pool", lambda e, sgb=sgb, sg=sg: e.tensor_copy(out=sgb[:], in_=sg[:]), reads=[sg.r], writes=[sgb.r])
                st(P, o_["qTb"][2 * s:2 * s + 2, :, gc].rearrange("h p t -> p h t"), sgb[:], sgb.r)
            elif kind in ("kc", "ks", "vc"):
                st(P, o_[kind + "T"][:, :, gc].rearrange("h p t -> p h t"), sg[:], sg.r)
            elif kind == "gl":
                st(P, o_["gT"][:, gc], gTs[:], gTs.r)


def build_D2():
    nc = bass.Bass("TRN2", target_bir_lowering=False)
    P = Prog(nc)
    i_ = dict(kcT=dram_in(nc, "kcT", [2, 128, 8192]), vcT=dram_in(nc, "vcT", [2, 128, 8192]), pekT=dram_in(nc, "pekT", [128, 32]), pevT=dram_in(nc, "pevT", [128, 32]),
              w1k=dram_in(nc, "w1k", [4096, 256]), w2k=dram_in(nc, "w2k", [256, 128]), w1v=dram_in(nc, "w1v", [4096, 256]), w2v=dram_in(nc, "w2v", [256, 128]))
    o_ = dict(kccT=dram_out(nc, "kccT", [2, 128, 512]), vcc=dram_out(nc, "vcc", [2, 512, 128]))
    emit_D2(P, i_, o_)
    P.emit()
    return nc


def emit_D2(P, i_, o_):
    w1 = T(P, "w1", [128, 32, 256]); w2 = T(P, "w2", [128, 2, 128]); pe = T(P, "pe", [128, 32])
    src = Pool_(P, "csrc", [128, 8192], 2)
    hps = Pool_(P, "hps", [128, 512], 2, psum=True); bps = T(P, "bps", [128, 512], psum=True); ops_ = Pool_(P, "ops", [128, 512], 2, psum=True)
    bias = T(P, "cbias", [128, 2]); hid = T(P, "hid", [128, 2, 512]); og = Pool_(P, "og", [128, 512], 2)
    for kv in range(2):
        nm = "k" if kv == 0 else "v"
        ld(P, w1[:], i_["w1" + nm].rearrange("(l p) c -> p l c", p=128), w1.r)
        ld(P, w2[:], i_["w2" + nm].rearrange("(k p) d -> p k d", p=128), w2.r)
        ld(P, pe[:], i_["pe" + nm + "T"], pe.r)
        for cc in range(2):
            for l in range(32):
                P.op("pe", lambda e, cc=cc, l=l: e.matmul(bps[:, cc:cc + 1], lhsT=w1[:, l, cc * 128:(cc + 1) * 128], rhs=pe[:, l:l + 1], start=(l == 0), stop=(l == 31)),
                     reads=[w1.r, pe.r], writes=[bps.r])
        P.op("dve", lambda e: e.tensor_copy(out=bias[:], in_=bps[:, 0:2]), reads=[bps.r], writes=[bias.r])
        for g in range(2):
            s_ = src.next()
            ld(P, s_[:], i_[("kcT" if kv == 0 else "vcT")][g], s_.r)
            sv = s_[:].rearrange("p (n l) -> p l n", l=16)
            for cc in range(2):
                hp = hps.next()
                for l in range(32):
                    rhs = sv[:, l, 0:511] if l < 16 else sv[:, l - 16, 1:512]
                    P.op("pe", lambda e, hp=hp, cc=cc, l=l, rhs=rhs: e.matmul(hp[:, 0:511], lhsT=w1[:, l, cc * 128:(cc + 1) * 128], rhs=rhs, start=(l == 0), stop=(l == 31)),
                         reads=[w1.r, s_.r], writes=[hp.r])
                P.op("act", lambda e, hp=hp, cc=cc: e.activation(out=hid[:, cc, 0:511], in_=hp[:, 0:511], func=AF.Gelu, bias=bias[:, cc:cc + 1], scale=1.0), reads=[hp.r, bias.r], writes=[hid.r])
            o = og.next()
            if kv == 0:
                op_ = ops_.next()
                for cc in range(2):
                    P.op("pe", lambda e, op_=op_, cc=cc: e.matmul(op_[:, 0:511], lhsT=w2[:, cc, :], rhs=hid[:, cc, 0:511], start=(cc == 0), stop=(cc == 1)), reads=[w2.r, hid.r], writes=[op_.r])
                P.op("dve", lambda e, op_=op_, o=o: e.tensor_copy(out=o[:, 0:511], in_=op_[:, 0:511]), reads=[op_.r], writes=[o.r])
                st(P, o_["kccT"][g, :, 0:511], o[:, 0:511], o.r)
            else:
                for nt in range(4):
                    n1 = min(128, 511 - nt * 128)
                    op_ = ops_.next()
                    for cc in range(2):
                        P.op("pe", lambda e, op_=op_, cc=cc, nt=nt, n1=n1: e.matmul(op_[0:n1, 0:128], lhsT=hid[:, cc, nt * 128:nt * 128 + n1], rhs=w2[:, cc, :], start=(cc == 0), stop=(cc == 1)),
                             reads=[w2.r, hid.r], writes=[op_.r])
                    P.op("dve", lambda e, op_=op_, o=o, nt=nt, n1=n1: e.tensor_copy(out=o[0:n1, nt * 128:(nt + 1) * 128], in_=op_[0:n1, 0:128]), reads=[op_.r], writes=[o.r])
                    st(P, o_["vcc"][g, nt * 128:nt * 128 + n1, :], o[0:n1, nt * 128:(nt + 1) * 128], o.r)


def build_D3a():
    nc = bass.Bass("TRN2", target_bir_lowering=False)
    P = Prog(nc)
    i_ = dict(qT=dram_in(nc, "qT", [16, 128, 2048]), gT=dram_in(nc, "gT", [48, 2048]), kccT=dram_in(nc, "kccT", [2, 128, 512]), vcc=dram_in(nc, "vcc", [2, 512, 128]),
              cmask=dram_in(nc, "cmask", [4, 128, 2048]), ovl=dram_in(nc, "ovl", [4, 128, 128]), m1=dram_in(nc, "m1", [NT, 128, 128]), m2=dram_in(nc, "m2", [NT, 128, 128]),
              ones=dram_in(nc, "ones", [128, 128]), ident=dram_in(nc, "ident", [128, 128]))
    o_ = dict(oc=dram_out(nc, "oc", [16, 128, 2048]), negmT=dram_out(nc, "negmT", [2, 128, 2048], BF16))
    emit_D3a(P, i_, o_)
    P.emit()
    return nc


def emit_D3a(P, i_, o_):
    ones = T(P, "ones", [128, 128]); ld(P, ones[:], i_["ones"], ones.r)
    idn = T(P, "idn", [128, 128]); ld(P, idn[:], i_["ident"], idn.r)
    ovl = T(P, "ovl", [128, 4, 128]); ld(P, ovl[:], i_["ovl"].rearrange("t n j -> n t j"), ovl.r)
    kcc = Pool_(P, "kcc", [128, 512], 2); vcc = Pool_(P, "vcc", [128, 4, 128], 2)
    cm = Pool_(P, "cm", [128, 4, 512], 2)
    qh = Pool_(P, "qh", [128, 512], 3); gb = Pool_(P, "gb", [128, 512], 3)
    ec = Pool_(P, "ec", [128, 4, 512], 2)
    zb = Pool_(P, "zb", [128, 512], 2, psum=True); db = Pool_(P, "db", [128, 512], 1, psum=True); ob = Pool_(P, "ob", [128, 512], 2, psum=True)
    ib = Pool_(P, "ib", [128, 512], 1, psum=True); tb = Pool_(P, "tb", [128, 512], 2, psum=True)
    rden = Pool_(P, "rden", [128, 512], 2); ocs = Pool_(P, "ocs", [128, 512], 2)
    impT = T(P, "impT", [128, 512]); m1 = T(P, "m1", [128, 4, 128]); m2 = T(P, "m2", [128, 4, 128]); score = T(P, "score", [128, 4, 128])
    tv = T(P, "tv", [128, 16]); rep = T(P, "rep", [128, 128]); selm = T(P, "selm", [128, 4, 128]); nmT = Pool_(P, "nmT", [128, 512], 2, dt=BF16)
    NK = [128, 128, 128, 127]
    for g in range(2):
        kc = kcc.next(); vc = vcc.next()
        ld(P, kc[:], i_["kccT"][g], kc.r)
        ld(P, vc[:], i_["vcc"][g].rearrange("(t n) d -> n t d", n=128), vc.r)
        for qg in range(4):
            gc = slice(qg * 512, (qg + 1) * 512)
            cmt = cm.next()
            ld(P, cmt[:], i_["cmask"][:, :, gc].rearrange("t n q -> n t q"), cmt.r)
            ld(P, m1[:], i_["m1"][qg * 4:(qg + 1) * 4].rearrange("t q j -> q t j"), m1.r)
            ld(P, m2[:], i_["m2"][qg * 4:(qg + 1) * 4].rearrange("t q j -> q t j"), m2.r)
            imp = ib.next()
            for r in range(8):
                hd = g * 8 + r
                q = qh.next(); g0 = gb.next(); e_ = ec.next(); d_ = db.next(); o = ob.next(); rd = rden.next(); oc = ocs.next()
                ld(P, q[:], i_["qT"][hd, :, gc], q.r)
                ld(P, g0[:], i_["gT"][hd * 3:hd * 3 + 1, gc].broadcast_to([128, 512]), g0.r)
                for nt in range(4):
                    nk = NK[nt]
                    z = zb.next()
                    P.op("pe", lambda e, z=z, kc=kc, q=q, nt=nt, nk=nk: e.matmul(z[0:nk, :], lhsT=kc[:, nt * 128:nt * 128 + nk], rhs=q[:], start=True, stop=True), reads=[kc.r, q.r], writes=[z.r])
                    P.op("act", lambda e, z=z, e_=e_, nt=nt, nk=nk: e.activation(out=e_[0:nk, nt, :], in_=z[0:nk, :], func=AF.Exp), reads=[z.r], writes=[e_.r])
                    P.op("pool", lambda e, e_=e_, cmt=cmt, nt=nt, nk=nk: e.tensor_tensor(out=e_[0:nk, nt, :], in0=e_[0:nk, nt, :], in1=cmt[0:nk, nt, :], op=ALU.mult), reads=[e_.r, cmt.r], writes=[e_.r])
                    P.op("pe", lambda e, d_=d_, e_=e_, nt=nt, nk=nk: e.matmul(d_[:, :], lhsT=ones[0:nk, :], rhs=e_[0:nk, nt, :], start=(nt == 0), stop=(nt == 3)), reads=[ones.r, e_.r], writes=[d_.r])
                P.op("dve", lambda e, rd=rd, d_=d_: e.tensor_scalar(out=rd[:], in0=d_[:, :], scalar1=1e-30, scalar2=None, op0=ALU.max), reads=[d_.r], writes=[rd.r])
                P.op("dve", lambda e, rd=rd: e.reciprocal(out=rd[:], in_=rd[:]), reads=[rd.r], writes=[rd.r])
                for nt in range(4):
                    nk = NK[nt]
                    P.op("dve", lambda e, e_=e_, rd=rd, nt=nt, nk=nk: e.tensor_tensor(out=e_[0:nk, nt, :], in0=e_[0:nk, nt, :], in1=rd[0:nk, :], op=ALU.mult), reads=[e_.r, rd.r], writes=[e_.r])
                    P.op("pe", lambda e, imp=imp, e_=e_, nt=nt, nk=nk, r=r: e.matmul(imp[:, :], lhsT=ovl[0:nk, nt, :], rhs=e_[0:nk, nt, :], start=(r == 0 and nt == 0), stop=(r == 7 and nt == 3)),
                         reads=[ovl.r, e_.r], writes=[imp.r])
                    P.op("pe", lambda e, o=o, vc=vc, e_=e_, nt=nt, nk=nk: e.matmul(o[:, :], lhsT=vc[0:nk, nt, :], rhs=e_[0:nk, nt, :], start=(nt == 0), stop=(nt == 3)), reads=[vc.r, e_.r], writes=[o.r])
                P.op("dve", lambda e, oc=oc, o=o, g0=g0: e.tensor_tensor(out=oc[:], in0=o[:, :], in1=g0[:], op=ALU.mult), reads=[o.r, g0.r], writes=[oc.r])
                st(P, o_["oc"][hd, :, gc], oc[:], oc.r)
            P.op("act", lambda e, imp=imp: e.copy(out=impT[:], in_=imp[:, :]), reads=[imp.r], writes=[impT.r])
            tp = tb.next()
            for m in range(4):
                P.op("pe", lambda e, tp=tp, m=m: e.transpose(tp[:, m * 128:(m + 1) * 128], impT[:, m * 128:(m + 1) * 128], idn[:]), reads=[impT.r, idn.r], writes=[tp.r])
            P.op("dve", lambda e, tp=tp: e.tensor_tensor(out=score[:], in0=tp[:, :].rearrange("p (m j) -> p m j", j=128), in1=m1[:], op=ALU.mult), reads=[tp.r, m1.r], writes=[score.r])
            P.op("dve", lambda e: e.tensor_tensor(out=score[:], in0=score[:], in1=m2[:], op=ALU.add), reads=[score.r, m2.r], writes=[score.r])
            for m in range(4):
                P.op("dve", lambda e, m=m: e.max(out=tv[:, 0:8], in_=score[:, m, :]), reads=[score.r], writes=[tv.r])
                P.op("dve", lambda e, m=m: e.match_replace(out=rep[:], in_to_replace=tv[:, 0:8], in_values=score[:, m, :], imm_value=-3e30), reads=[score.r, tv.r], writes=[rep.r])
                P.op("dve", lambda e: e.max(out=tv[:, 8:16], in_=rep[:]), reads=[rep.r], writes=[tv.r])
                P.op("dve", lambda e, m=m: e.tensor_scalar(out=selm[:, m, :], in0=score[:, m, :], scalar1=tv[:, 15:16], scalar2=None, op0=ALU.is_ge), reads=[score.r, tv.r], writes=[selm.r])
            P.op("dve", lambda e: e.tensor_scalar(out=selm[:], in0=selm[:], scalar1=-1.0, scalar2=30000.0, op0=ALU.add, op1=ALU.mult), reads=[selm.r], writes=[selm.r])
            tp2 = tb.next(); nm = nmT.next()
            for m in range(4):
                P.op("pe", lambda e, tp2=tp2, m=m: e.transpose(tp2[:, m * 128:(m + 1) * 128], selm[:, m, :], idn[:]), reads=[selm.r, idn.r], writes=[tp2.r])
            P.op("act", lambda e, tp2=tp2, nm=nm: e.copy(out=nm[:], in_=tp2[:, :]), reads=[tp2.r], writes=[nm.r])
            st(P, o_["negmT"][g, :, gc], nm[:], nm.r)


def build_D3b():
    nc = bass.Bass("TRN2", target_bir_lowering=False)
    P = Prog(nc)
    i_ = dict(qT=dram_in(nc, "qT", [16, 128, 2048]), gT=dram_in(nc, "gT", [48, 2048]), oc=dram_in(nc, "oc", [16, 128, 2048]), negmT=dram_in(nc, "negmT", [2, 128, 2048], BF16),
              ksT=dram_in(nc, "ksT", [2, 128, 8192]), vs=dram_in(nc, "vs", [2, 8192, 128]), Ebig=dram_in(nc, "Ebig", [128, 64, 128], BF16),
              masks=dram_in(nc, "masks", [16, 128, 512]), kwwT=dram_in(nc, "kwwT", [2, NT, 128, 640]), vww=dram_in(nc, "vww", [2, NT, 640, 128]),
              wmask=dram_in(nc, "wmask", [4, 5, 128, 512]), ones=dram_in(nc, "ones", [128, 128]))
    o_ = dict(oT=dram_out(nc, "oT", [16, 128, 2048]))
    emit_D3b(P, i_, o_)
    P.emit()
    return nc


def emit_D3b(P, i_, o_):
    MK = Masker(P, i_["masks"])
    on32 = T(P, "on32", [128, 128]); ld(P, on32[:], i_["ones"], on32.r)
    ones = T(P, "ones", [128, 128], ADT)
    P.op("dve", lambda e: e.tensor_copy(out=ones[:], in_=on32[:]), reads=[on32.r], writes=[ones.r])
    Eb = T(P, "Ebig", [128, 64, 128], BF16); ld(P, Eb[:], i_["Ebig"], Eb.r)
    KS = T(P, "KS", [128, 8192], ADT); VS = T(P, "VS", [128, 64, 128], ADT)
    kww = T(P, "kww", [128, 4, 640], ADT); kwin = T(P, "kwin", [128, 20, 128], ADT); vww = T(P, "vwin", [128, 4, 5, 128], ADT); wm = T(P, "wm", [128, 5, 512])
    id32 = T(P, "id32", [128, 128]); ld(P, id32[:], i_["ident"], id32.r)
    idn = T(P, "idn", [128, 128], ADT)
    P.op("dve", lambda e: e.tensor_copy(out=idn[:], in_=id32[:]), reads=[id32.r], writes=[idn.r])
    tpbf = Pool_(P, "tpbf", [128, 512], 1, dt=ADT, psum=True)
    widx = T(P, "widx", [128, 80], I32); ld(P, widx[:], i_["widx"], widx.r)
    nmp = Pool_(P, "nm", [128, 512], 2, dt=BF16)
    qh = Pool_(P, "qh", [128, 512], 3, dt=ADT); g1p = Pool_(P, "g1", [128, 512], 2); g2p = Pool_(P, "g2", [128, 512], 2); ocp = Pool_(P, "occ", [128, 512], 2)
    pt = Pool_(P, "pt", [128, 512], 5, dt=ADT); pw = T(P, "pw", [128, 5, 512], ADT)
    rsp = Pool_(P, "rs", [128, 512], 2); t1p = Pool_(P, "t1", [128, 512], 2); accp = Pool_(P, "oacc", [128, 512], 2)
    zb = Pool_(P, "zb", [128, 512], 3, psum=True); osb = T(P, "osb", [128, 512], psum=True); dsb = T(P, "dsb", [128, 512], psum=True)
    zw = zb; owb = T(P, "owb", [128, 512], psum=True); dwb = T(P, "dwb", [128, 512], psum=True)
    for g in range(2):
        P.dma("sp", [lambda e, g=g, c=c: e.dma_start(out=KS[:, c * 2048:(c + 1) * 2048], in_=i_["ksT"][g, :, c * 2048:(c + 1) * 2048]) for c in range(4)], KS.r, writes=[KS.r])
        vv = i_["vs"][g].rearrange("(k p) d -> p k d", p=128)
        P.dma("sp", [lambda e, vv=vv, c=c: e.dma_start(out=VS[:, c * 16:(c + 1) * 16, :], in_=vv[:, c * 16:(c + 1) * 16, :]) for c in range(4)], VS.r, writes=[VS.r])
        for qg in range(4):
            gc = slice(qg * 512, (qg + 1) * 512)
            nm = nmp.next()
            ld(P, nm[:], i_["negmT"][g, :, gc], nm.r)
            for j in range(20):
                col = (qg * 4 + j // 5) * 5 + j % 5
                P.dma("pool", lambda e, j=j, col=col, g=g: e.indirect_dma_start(out=kwin[:, j, :], out_offset=None, in_=i_["kw"][g], in_offset=bass.IndirectOffsetOnAxis(ap=widx[:, col:col + 1], axis=0)),
                      kwin.r, reads=[widx.r], writes=[kwin.r])
                P.dma("pool", lambda e, j=j, col=col, g=g: e.indirect_dma_start(out=vww[:, j // 5, j % 5, :], out_offset=None, in_=i_["vw"][g], in_offset=bass.IndirectOffsetOnAxis(ap=widx[:, col:col + 1], axis=0)),
                      vww.r, reads=[widx.r], writes=[vww.r])
            for j0 in range(0, 20, 4):
                tpb = tpbf.next()
                for j in range(j0, j0 + 4):
                    P.op("pe", lambda e, tpb=tpb, j=j, j0=j0: e.transpose(tpb[:, (j - j0) * 128:(j - j0 + 1) * 128], kwin[:, j, :], idn[:]), reads=[kwin.r, idn.r], writes=[tpb.r])
                for j in range(j0, j0 + 4):
                    P.op("dve", lambda e, tpb=tpb, j=j, j0=j0: e.tensor_copy(out=kww[:, j // 5, (j % 5) * 128:(j % 5 + 1) * 128], in_=tpb[:, (j - j0) * 128:(j - j0 + 1) * 128]), reads=[tpb.r], writes=[kww.r])
            ld(P, wm[:], i_["wmask"][qg].rearrange("o s q -> s o q"), wm.r)
            kb_max = 16 * qg + 15
            for r in range(8):
                hd = g * 8 + r
                q = qh.next(); g1 = g1p.next(); g2 = g2p.next(); occ = ocp.next()
                ld(P, q[:], i_["qTb"][hd, :, gc], q.r)
                ld(P, g1[:], i_["gT"][hd * 3 + 1:hd * 3 + 2, gc].broadcast_to([128, 512]), g1.r)
                ld(P, g2[:], i_["gT"][hd * 3 + 2:hd * 3 + 3, gc].broadcast_to([128, 512]), g2.r)
                ld(P, occ[:], i_["oc"][hd, :, gc], occ.r)
                kbs = list(range(kb_max, -1, -1))
                pend = []

                def S2(kb, p):
                    P.op("pe", lambda e, p=p, kb=kb, kb_max=kb_max: e.matmul(osb[:, :], lhsT=VS[:, kb, :], rhs=p[:], start=(kb == kb_max), stop=(kb == 0)), reads=[VS.r, p.r], writes=[osb.r])
                    P.op("pe", lambda e, p=p, kb=kb, kb_max=kb_max: e.matmul(dsb[:, :], lhsT=ones[:], rhs=p[:], start=(kb == kb_max), stop=(kb == 0)), reads=[ones.r, p.r], writes=[dsb.r])
                for kb in kbs:
                    ks = slice(kb * 128, (kb + 1) * 128)
                    z = zb.next(); p = pt.next()
                    P.op("pe", lambda e, z=z, q=q, ks=ks: e.matmul(z[:, :], lhsT=KS[:, ks], rhs=q[:], start=True, stop=False), reads=[KS.r, q.r], writes=[z.r])
                    P.op("pe", lambda e, z=z, nm=nm, kb=kb: e.matmul(z[:, :], lhsT=Eb[:, kb, :], rhs=nm[:], start=False, stop=True), reads=[Eb.r, nm.r], writes=[z.r])
                    P.op("act", lambda e, z=z, p=p: e.activation(out=p[:], in_=z[:, :], func=AF.Exp), reads=[z.r], writes=[p.r])
                    if kb >= 16 * qg:
                        MK.apply(p, kb - 16 * qg)
                    pend.append((kb, p))
                    if len(pend) > 2:
                        S2(*pend.pop(0))
                while pend:
                    S2(*pend.pop(0))
                rs = rsp.next(); t1 = t1p.next(); acc = accp.next()
                P.op("dve", lambda e, rs=rs: e.reciprocal(out=rs[:], in_=dsb[:, :]), reads=[dsb.r], writes=[rs.r])
                P.op("dve", lambda e, rs=rs, t1=t1: e.tensor_tensor(out=t1[:], in0=osb[:, :], in1=rs[:], op=ALU.mult), reads=[osb.r, rs.r], writes=[t1.r])
                P.op("pool", lambda e, t1=t1, g1=g1: e.tensor_tensor(out=t1[:], in0=t1[:], in1=g1[:], op=ALU.mult), reads=[t1.r, g1.r], writes=[t1.r])
                P.op("pool", lambda e, t1=t1, acc=acc, occ=occ: e.tensor_tensor(out=acc[:], in0=t1[:], in1=occ[:], op=ALU.add), reads=[t1.r, occ.r], writes=[acc.r])
                for off in range(5):
                    z = zw.next()
                    for m in range(4):
                        P.op("pe", lambda e, z=z, q=q, m=m, off=off: e.matmul(z[:, m * 128:(m + 1) * 128], lhsT=kww[:, m, off * 128:(off + 1) * 128], rhs=q[:, m * 128:(m + 1) * 128], start=True, stop=True),
                             reads=[kww.r, q.r], writes=[z.r])
                    P.op("act", lambda e, z=z, off=off: e.activation(out=pw[:, off, :], in_=z[:, :], func=AF.Exp), reads=[z.r], writes=[pw.r])
                    P.op("pool", lambda e, off=off: e.tensor_tensor(out=pw[:, off, :], in0=pw[:, off, :], in1=wm[:, off, :], op=ALU.mult), reads=[pw.r, wm.r], writes=[pw.r])
                for m in range(4):
                    ms = slice(m * 128, (m + 1) * 128)
                    for off in range(5):
                        P.op("pe", lambda e, m=m, ms=ms, off=off: e.matmul(owb[:, ms], lhsT=vww[:, m, off, :], rhs=pw[:, off, ms], start=(off == 0), stop=(off == 4)), reads=[vww.r, pw.r], writes=[owb.r])
                    for off in range(5):
                        P.op("pe", lambda e, ms=ms, off=off: e.matmul(dwb[:, ms], lhsT=ones[:], rhs=pw[:, off, ms], start=(off == 0), stop=(off == 4)), reads=[ones.r, pw.r], writes=[dwb.r])
                rs2 = rsp.next(); t2 = t1p.next()
                P.op("dve", lambda e, rs2=rs2: e.reciprocal(out=rs2[:], in_=dwb[:, :]), reads=[dwb.r], writes=[rs2.r])
                P.op("dve", lambda e, rs2=rs2, t2=t2: e.tensor_tensor(out=t2[:], in0=owb[:, :], in1=rs2[:], op=ALU.mult), reads=[owb.r, rs2.r], writes=[t2.r])
                P.op("pool", lambda e, t2=t2, g2=g2: e.tensor_tensor(out=t2[:], in0=t2[:], in1=g2[:], op=ALU.mult), reads=[t2.r, g2.r], writes=[t2.r])
                P.op("pool", lambda e, t2=t2, acc=acc: e.tensor_tensor(out=acc[:], in0=acc[:], in1=t2[:], op=ALU.add), reads=[t2.r, acc.r], writes=[acc.r])
                st(P, o_["oT"][hd, :, gc], acc[:], acc.r, eng="act")


import ml_dtypes as _mld

NCORES = 8
TA = 8192
NTA = TA // 128


def build_fused(debug=()):
    nc = bass.Bass("TRN2", target_bir_lowering=False)
    P = Prog(nc)
    I = lambda n, s, dt=F32: dram_in(nc, n, s, dt)

    def mid(name, shape, dt=F32):
        return dram_out(nc, name, shape, dt) if name in debug else dram_tmp(nc, name, shape, dt)
    x = I("x", [TA, 2048]); pos_all = I("pos_all", [128, NTA], I32); pos_own = I("pos_own", [128, NT], I32); c2 = I("c2", [128, 16])
    adaw = [I(f"adaw{m}", [2048, 6144]) for m in range(4)]; adab = [I(f"adab{m}", [1, 6144]) for m in range(4)]; gn = [I(f"g{m}", [1, 2048]) for m in range(4)]
    w_in0 = I("w_in0", [2048, 3904]); qn_g = I("qn_g", [128, 4]); w_uq = I("w_uq", [512, 1536]); kvn_g = I("kvn_g", [128, 2]); w_ukv = I("w_ukv", [256, 2048]); w_out0 = I("w_out0", [2048, 2048])
    wq = [I(f"wq{l}", [2048, 2048]) for l in range(2)]; k1T = [I(f"k1T{l}", [128, 128]) for l in range(2)]; k2T = [I(f"k2T{l}", [128, 128]) for l in range(2)]
    pu = [I(f"pu{l}", [16384, 2048]) for l in range(2)]; pv = [I(f"pv{l}", [16384, 2048]) for l in range(2)]
    w_in1 = I("w_in1", [2048, 3632]); pekT = I("pekT", [128, 32]); pevT = I("pevT", [128, 32]); w1k = I("w1k", [4096, 256]); w2k = I("w2k", [256, 128])
    w1v = I("w1v", [4096, 256]); w2v = I("w2v", [256, 128]); w_out1 = I("w_out1", [2048, 2048]); fin_g = I("fin_g", [1, 2048])
    ident = I("ident", [128, 128]); ones = I("ones", [128, 128]); negT1 = I("negT1", [128, 128]); negOnes = I("negOnes", [128, 128]); iota16 = I("iota16", [1, 16])
    invf32 = I("invf32", [1, 32]); invf16 = I("invf16", [1, 16]); ms4 = I("ms4", [4, 128, 512]); mc4 = I("mc4", [4, 128, 512]); mc16 = I("mc16", [16, 128, 512])
    cmask = I("cmask", [4, 128, 2048]); ovl = I("ovl", [4, 128, 128]); m1 = I("m1", [NT, 128, 128]); m2 = I("m2", [NT, 128, 128]); Ebig = I("Ebig", [128, 64, 128], BF16)
    wmask = I("wmask", [4, 5, 128, 512]); widx = I("widx", [128, 80], I32); own_idx = I("own_idx", [128, NT], I32)
    out = dram_out(nc, "out", [2048, 2048])
    mods = mid("mods", [4, 3, 2048])
    qT_sb = mid("qT_sb", [8, 128, TA], ADT); kT_sb = mid("kT_sb", [8, 128, TA], ADT); v_sb = mid("v_sb", [TA, 1024], ADT); qT_mn = mid("qT_mn", [8, 128, TA], ADT); qT_mr = mid("qT_mr", [4, 128, TA], ADT)
    kT_mn = mid("kT_mn", [8, 128, TA], ADT); kT_r = mid("kT_r", [64, TA], ADT); v_m = mid("v_m", [TA, 1024], ADT); oT0 = mid("oT0", [16, 128, TA], ADT)
    x1 = mid("x1", [TA, 2048]); h0 = mid("h0", [TA, 2048]); ids0 = mid("ids0", [TA, 128], I32); gw0 = mid("gw0", [TA, 128]); x2 = mid("x2", [TA, 2048])
    kcT = mid("kcT", [2, 128, TA]); vcT = mid("vcT", [2, 128, TA]); ksT = mid("ksT", [2, 128, TA], ADT); vs = mid("vs", [TA, 256], ADT); kw = [mid(f"kw{g}", [TA + 128, 128], ADT) for g in range(2)]; vw = [mid(f"vw{g}", [TA + 128, 128], ADT) for g in range(2)]
    qT1 = mid("qT1", [16, 128, 2048]); qT1b = mid("qT1b", [16, 128, 2048], ADT); gT = mid("gT", [48, 2048]); kccT = mid("kccT", [2, 128, 512]); vcc = mid("vcc", [2, 512, 128]); oc = mid("oc", [16, 128, 2048])
    negmT = mid("negmT", [2, 128, 2048], BF16); oT1 = mid("oT1", [16, 128, 2048]); x3 = mid("x3", [2048, 2048]); h1 = mid("h1", [2048, 2048]); ids1 = mid("ids1", [2048, 128], I32); gw1 = mid("gw1", [2048, 128])
    uv = [dram_tmp(nc, f"uv{l}", [16384, 4096], BF16) for l in range(2)]
    stop = [d for d in debug if d.startswith("stop:")]
    stop = stop[0][5:] if stop else None
    sched0 = [(4 * g + 3, 4 * g) for g in range(NTA // 4)]

    def phases():
        with Phase(P, "M"):
            emit_M(P, c2, adaw, adab, gn, mods)
        yield "M"
        with Phase(P, "A"):
            emit_A(P, dict(xload=xload_direct(x), pos=pos_all, mods=mods, w_in=w_in0, qn_g=qn_g, w_uq=w_uq, kvn_g=kvn_g, w_ukv=w_ukv, ident=ident, ones=ones, invf=invf32),
                   dict(qT_sb=qT_sb, kT_sb=kT_sb, v_sb=v_sb, qT_mn=qT_mn, qT_mr=qT_mr, kT_mn=kT_mn, kT_r=kT_r, v_m=v_m), NTA)
        yield "A"
        with Phase(P, "B1"):
            emit_B1(P, dict(qT=qT_sb, kT=kT_sb, v=v_sb.rearrange("t (h d) -> h t d", d=128), negT1=negT1, negOnes=negOnes, masks=ms4), dict(oT=oT0[0:8]), 8, sched0)
        yield "B1"
        with Phase(P, "B2"):
            emit_B2(P, dict(qTn=qT_mn, qTr=qT_mr, kTn=kT_mn, kTr=kT_r, v=v_m.rearrange("t (h d) -> h t d", d=128), ones=ones, masks=mc4), dict(oT=oT0[8:16]), 8, sched0)
        yield "B2"
        with Phase(P, "C1a"):
            emit_C1(P, dict(oT=oT0, w_out=w_out0, xload=xload_direct(x), mods=mods), dict(x1=x1), 0, NTA)
        yield "C1a"
        with Phase(P, "C2a"):
            emit_C2(P, dict(x=x1, mods=mods, w_q=wq[0], k1T=k1T[0], k2T=k2T[0], ident=ident, iota16=iota16), dict(h=h0, ids=ids0, gw=gw0), 1, NTA)
        yield "C2a"
        for l in range(2):
            with Phase(P, f"PCu{l}"):
                emit_PC(P, pu[l], uv[l], 0)
            with Phase(P, f"PCv{l}"):
                emit_PC(P, pv[l], uv[l], 1)
        with Phase(P, "C3a"):
            emit_C3f(P, dict(x=x1, h=h0, ids=ids0, gw=gw0, mods=mods, uv=uv[0], ident=ident), dict(x2=x2), 1, NTA)
        yield "C3a"
        with Phase(P, "D1k"):
            emit_D1(P, dict(xload=xload_direct(x2), pos=pos_all, mods=mods, w_in=w_in1, ident=ident, invf=invf16), dict(kcT=kcT, vcT=vcT, ksT=ksT, vs=vs, kw=kw, vw=vw), 2, NTA, "k")
        yield "D1k"
        with Phase(P, "D1q"):
            emit_D1(P, dict(xload=xload_gather(P, x2, own_idx, NT), pos=pos_own, mods=mods, w_in=w_in1, ident=ident, invf=invf16), dict(qT=qT1, qTb=qT1b, gT=gT), 2, NT, "q")
        yield "D1q"
        with Phase(P, "D2"):
            emit_D2(P, dict(kcT=kcT, vcT=vcT, pekT=pekT, pevT=pevT, w1k=w1k, w2k=w2k, w1v=w1v, w2v=w2v), dict(kccT=kccT, vcc=vcc))
        yield "D2"
        with Phase(P, "D3a"):
            emit_D3a(P, dict(qT=qT1, gT=gT, kccT=kccT, vcc=vcc, cmask=cmask, ovl=ovl, m1=m1, m2=m2, ones=ones, ident=ident), dict(oc=oc, negmT=negmT))
        yield "D3a"
        with Phase(P, "D3b"):
            emit_D3b(P, dict(qTb=qT1b, gT=gT, oc=oc, negmT=negmT, ksT=ksT, vs=vs.rearrange("t (h d) -> h t d", d=128), Ebig=Ebig, masks=mc16, kw=kw, vw=vw, wmask=wmask, ones=ones,
                             ident=ident, widx=widx), dict(oT=oT1))
        yield "D3b"
        with Phase(P, "C1b"):
            emit_C1(P, dict(oT=oT1, w_out=w_out1, xload=xload_gather(P, x2, own_idx, NT), mods=mods), dict(x1=x3), 2, NT)
        yield "C1b"
        with Phase(P, "C2b"):
            emit_C2(P, dict(x=x3, mods=mods, w_q=wq[1], k1T=k1T[1], k2T=k2T[1], ident=ident, iota16=iota16), dict(h=h1, ids=ids1, gw=gw1), 3, NT)
        yield "C2b"
        with Phase(P, "C3b"):
            emit_C3f(P, dict(x=x3, h=h1, ids=ids1, gw=gw1, mods=mods, uv=uv[1], fin_g=fin_g, ident=ident), dict(x2=out), 3, NT)
        yield "C3b"
    for name in phases():
        if name == stop:
            break
    P.emit()
    return nc, P


def _consts(r):
    own = np.arange(8192).reshape(64, 128)[r::4].reshape(-1)
    c = {}
    kbp = np.arange(16)[:, None, None]; p = np.arange(128)[None, :, None]; f = np.arange(512)[None, None, :]
    val = (4 * (f // 128) + r - kbp) * 128 + (f % 128) - p
    c["mc16"] = (val >= 0).astype(np.float32)
    kb4 = np.arange(4)[:, None, None]
    val4 = (f - kb4 * 128 - p) + 0 * kb4
    c["ms4"] = (val4 > 0).astype(np.float32); c["mc4"] = (val4 >= 0).astype(np.float32)
    n = np.arange(512)
    c["cmask"] = ((16 * n[:, None] + 31 <= own[None, :]) & (n[:, None] <= 510)).astype(np.float32).reshape(4, 128, 2048)
    c_s = np.arange(512) * 16; s_s = np.arange(128) * 64
    ovl = np.clip(np.minimum(c_s[:, None] + 32, s_s[None, :] + 64) - np.maximum(c_s[:, None], s_s[None, :]), 0, None).astype(np.float32)
    ovl[511] = 0
    c["ovl"] = ovl.reshape(4, 128, 128)
    j = np.arange(128)[None, :]; t = own[:, None]; cur = t // 64
    forced = (j == 0) | (j == cur) | (j == cur - 1)
    valid = j * 64 <= t
    c["m1"] = (valid & ~forced).astype(np.float32).reshape(16, 128, 128)
    c["m2"] = np.where(forced, 1e9, np.where(valid, 0.0, -1e9)).astype(np.float32).reshape(16, 128, 128)
    jj = np.arange(128)[:, None, None]; kb = np.arange(64)[None, :, None]; s = np.arange(128)[None, None, :]
    c["Ebig"] = (jj == 2 * kb + (s >= 64)).astype(np.float32).astype(_mld.bfloat16)
    wmask = np.zeros((4, 5, 128, 512), np.float32)
    widx = np.zeros((128, 80), np.int32)
    pp = np.arange(128)
    for i in range(16):
        jq = 4 * i + r
        qg, mm = i // 4, i % 4
        tq = jq * 128 + np.arange(128)[None, :]
        for off in range(5):
            kbk = jq - 4 + off
            key = kbk * 128 + np.arange(128)[:, None]
            wmask[qg, off, :, mm * 128:(mm + 1) * 128] = ((key <= tq) & (key > tq - 512) & (key >= 0))
            widx[:, i * 5 + off] = (kbk * 128 + pp) if kbk >= 0 else (8192 + pp)
    c["wmask"] = wmask; c["widx"] = widx
    c["own_idx"] = np.ascontiguousarray(own.reshape(16, 128).T).astype(np.int32)
    return c


_PROG = {}


def make_inputs(inp):
    f32 = lambda a: np.ascontiguousarray(np.asarray(a, dtype=np.float32))
    x = f32(inp["x"]); c = f32(inp["c"]); positions = np.asarray(inp["positions"]).astype(np.int32)
    ident = np.eye(128, dtype=np.float32); ones = np.ones((128, 128), np.float32)
    jj = np.arange(128)[:, None]; ss = np.arange(128)[None, :]
    shared = dict(
        w_in0=f32(inp["sbmla_w_in"][0]), qn_g=np.ascontiguousarray(f32(inp["mla_q_norm"][0]).reshape(4, 128).T), w_uq=f32(inp["mla_w_uq"][0]),
        kvn_g=np.ascontiguousarray(f32(inp["mla_kv_norm"][0]).reshape(2, 128).T), w_ukv=f32(inp["mla_w_ukv"][0]), w_out0=f32(inp["sbmla_w_out"][0]),
        w_in1=f32(inp["nsa_w_in"][0]), pekT=np.ascontiguousarray(f32(inp["nsa_pe_k"][0]).T), pevT=np.ascontiguousarray(f32(inp["nsa_pe_v"][0]).T),
        w1k=f32(inp["nsa_w1_k"][0]), w2k=f32(inp["nsa_w2_k"][0]), w1v=f32(inp["nsa_w1_v"][0]), w2v=f32(inp["nsa_w2_v"][0]), w_out1=f32(inp["nsa_w_out"][0]),
        fin_g=f32(inp["final_norm"])[None, :], ident=ident, ones=ones, negT1=-(jj >= ss).astype(np.float32), negOnes=-ones, iota16=np.arange(16, dtype=np.float32)[None, :],
        invf32=(500000.0 ** (-np.arange(32, dtype=np.float64) / 32) / (2 * np.pi)).astype(np.float32)[None, :],
        invf16=(500000.0 ** (-np.arange(16, dtype=np.float64) / 16) / (2 * np.pi)).astype(np.float32)[None, :])
    mlist = [("ada_mix_w", "ada_mix_b", "norm_mix", 0), ("ada_ffn_w", "ada_ffn_b", "norm_ffn", 0), ("ada_mix_w", "ada_mix_b", "norm_mix", 1), ("ada_ffn_w", "ada_ffn_b", "norm_ffn", 1)]
    for m, (w, bb, g, l) in enumerate(mlist):
        shared[f"adaw{m}"] = f32(inp[w][l]); shared[f"adab{m}"] = f32(inp[bb][l])[None, :]; shared[f"g{m}"] = f32(inp[g][l])[None, :]
    for l in range(2):
        shared[f"wq{l}"] = f32(inp["peer_w_q"][l]); shared[f"k1T{l}"] = np.ascontiguousarray(f32(inp["peer_k1"][l]).T); shared[f"k2T{l}"] = np.ascontiguousarray(f32(inp["peer_k2"][l]).T)
        shared[f"pu{l}"] = f32(inp["peer_u"][l]); shared[f"pv{l}"] = f32(inp["peer_v"][l])
    CS = [_consts(r) for r in range(4)]
    maps = []
    for k in range(NCORES):
        b, r = k // 4, k % 4
        d = dict(shared)
        d.update(CS[r])
        d["x"] = x[b]; d["c2"] = np.ascontiguousarray(c[b].reshape(16, 128).T)
        d["pos_all"] = np.ascontiguousarray(positions[b].reshape(64, 128).T)
        d["pos_own"] = np.ascontiguousarray(positions[b].reshape(64, 128)[r::4].T)
        maps.append(d)
    return maps


def kernel(**inp):
    if "nc" not in _PROG:
        _PROG["nc"] = build_fused()[0]
    maps = make_inputs(inp)
    res = run_bass_kernel_spmd(_PROG["nc"], maps, core_ids=list(range(NCORES)))
    out = np.empty((2, 64, 128, 2048), np.float32)
    for k in range(NCORES):
        b, r = k // 4, k % 4
        out[b, r::4] = res.results[k]["out"].reshape(16, 128, 2048)
    return out.reshape(2, 8192, 2048)
```

```python
import math
import numpy as np
import concourse.bass as bass
import concourse.mybir as mybir
from concourse.alu_op_type import AluOpType as ALU
from concourse.bass_utils import run_bass_kernel_spmd

AF = mybir.ActivationFunctionType
F32 = mybir.dt.float32
BF16 = mybir.dt.bfloat16
I32 = mybir.dt.int32
U32 = mybir.dt.uint32
AX = mybir.AxisListType


class Res:
    __slots__ = ("name", "lw", "rd", "dsem", "dcnt", "dlast", "dkey")

    def __init__(self, name=""):
        self.name = name
        self.lw = None
        self.rd = {}
        self.dsem = None
        self.dcnt = 0
        self.dlast = None
        self.dkey = None


class Prog:
    ENGS = ("pe", "dve", "act", "pool", "sp")

    def __init__(self, nc):
        self.nc = nc
        self.ops = {e: [] for e in self.ENGS}
        self.cnt = {e: 0 for e in self.ENGS}
        self.seen = {e: {} for e in self.ENGS}
        self.esem = {e: nc.alloc_semaphore(name="es_" + e) for e in self.ENGS}
        self.dsems = {}
        self.all_dma = []
        self.nres = 0
        self.prefix = ""
        self.free_dsems = []
        self.nsem = 0

    def sb(self, name, shape, dt=F32):
        return self.nc.alloc_sbuf_tensor("s_" + self.prefix + name, list(shape), dt)

    def ps(self, name, shape=(128, 512), dt=F32):
        return self.nc.alloc_psum_tensor("p_" + self.prefix + name, list(shape), dt)

    def res(self, name=""):
        self.nres += 1
        return Res(name or f"r{self.nres}")

    def _deps(self, reads, writes):
        deps = {}

        def add(tok):
            if tok is None:
                return
            k, v = tok
            if deps.get(k, 0) < v:
                deps[k] = v
        for r in reads:
            add(r.lw)
        for w in writes:
            add(w.lw)
            for k, v in w.rd.items():
                add((k, v))
        return deps

    def _filter(self, eng, deps):
        waits = []
        seen = self.seen[eng]
        for k, v in deps.items():
            if k == ("e", eng) and eng == "pe":
                continue
            if seen.get(k, 0) >= v:
                continue
            seen[k] = v
            waits.append((k, v))
        return waits

    def _commit(self, tok, reads, writes):
        k, v = tok
        for r in reads:
            if r.rd.get(k, 0) < v:
                r.rd[k] = v
        for w in writes:
            w.lw = tok
            w.rd = {}

    def op(self, eng, fn, reads=(), writes=()):
        deps = self._deps(reads, writes)
        waits = self._filter(eng, deps)
        self.cnt[eng] += 1
        tok = (("e", eng), self.cnt[eng])
        self.ops[eng].append((waits, fn, None, 0))
        self._commit(tok, reads, writes)
        return tok

    def dma(self, eng, fns, semres, reads=(), writes=()):
        if not isinstance(fns, (list, tuple)):
            fns = [fns]
        if semres.dsem is None:
            if self.free_dsems:
                semres.dsem, semres.dkey, semres.dcnt = self.free_dsems.pop()
            else:
                self.nsem += 1
                semres.dsem = self.nc.alloc_semaphore(name=f"ds{self.nsem}")
                semres.dkey = ("d", self.nsem)
                semres.dcnt = 0
            self.all_dma.append(semres)
        deps = self._deps(reads, writes)
        if semres.dlast is not None:
            k, v = semres.dlast
            if deps.get(k, 0) < v:
                deps[k] = v
        waits = self._filter(eng, deps)
        key = semres.dkey
        self.dsems[key] = semres.dsem
        tok = None
        for i, fn in enumerate(fns):
            semres.dcnt += 16
            tok = (key, semres.dcnt)
            self.ops[eng].append((waits if i == 0 else [], fn, semres.dsem, 16))
        semres.dlast = tok
        self._commit(tok, reads, writes)
        return tok

    def final_wait(self, eng="sp"):
        deps = {}
        for e in self.ENGS:
            if self.cnt[e] > 0:
                deps[("e", e)] = self.cnt[e]
        for r in self.all_dma:
            deps[r.dkey] = r.dcnt
        waits = self._filter(eng, deps)
        self.ops[eng].append((waits, None, None, 0))

    def barrier(self):
        for e in self.ENGS:
            self.final_wait(e)
        for r in self.all_dma:
            self.free_dsems.append((r.dsem, r.dkey, r.dcnt))
            r.dsem = None
        self.all_dma = []

    def _sem(self, k):
        return self.esem[k[1]] if k[0] == "e" else self.dsems[k]

    def _emit_eng(self, ename, e):
        for waits, fn, dsem, inc in self.ops[ename]:
            for k, v in waits:
                e.wait_ge(self._sem(k), v)
            if fn is None:
                continue
            ins = fn(e)
            if dsem is not None:
                ins.then_inc(dsem, 16)
            else:
                ins.then_inc(self.esem[ename], 1)

    def emit(self):
        for e in self.ENGS:
            self.final_wait(e)
        with self.nc.Block() as block:
            @block.sync
            def _(e):
                self._emit_eng("sp", e)

            @block.tensor
            def _(e):
                self._emit_eng("pe", e)

            @block.vector
            def _(e):
                self._emit_eng("dve", e)

            @block.scalar
            def _(e):
                self._emit_eng("act", e)

            @block.gpsimd
            def _(e):
                self._emit_eng("pool", e)

    def stats(self):
        return {e: len(self.ops[e]) for e in self.ENGS}


class T:
    def __init__(self, P, name, shape, dt=F32, psum=False):
        self.t = P.ps(name, shape, dt) if psum else P.sb(name, shape, dt)
        self.r = P.res(name)

    def __getitem__(self, k):
        return self.t[k]


class Pool_:
    def __init__(self, P, name, shape, n, dt=F32, psum=False):
        self.tiles = [T(P, f"{name}{i}", shape, dt, psum) for i in range(n)]
        self.i = 0

    def next(self):
        t = self.tiles[self.i % len(self.tiles)]
        self.i += 1
        return t


def dram_in(nc, name, shape, dt=F32):
    return nc.dram_tensor(name, list(shape), dt, kind="ExternalInput").ap()


def dram_out(nc, name, shape, dt=F32):
    return nc.dram_tensor(name, list(shape), dt, kind="ExternalOutput").ap()


class Phase:
    def __init__(self, P, name):
        self.P, self.name = P, name

    def __enter__(self):
        nc = self.P.nc
        self.save = (nc.psum_base, nc.psum_top, nc.sbuf_base, nc.sbuf_top)
        self.P.prefix = self.name + "_"
        return self

    def __exit__(self, *a):
        nc = self.P.nc
        self.P.barrier()
        nc.psum_base, nc.psum_top, nc.sbuf_base, nc.sbuf_top = self.save
        self.P.prefix = ""
        return False


def dram_tmp(nc, name, shape, dt=F32):
    return nc.dram_tensor(name, list(shape), dt, kind="Internal").ap()


import math
ADT = BF16

D_MODEL = 2048
NT = 16
EPS = 1e-6
TWO_PI = 2 * math.pi


def ld(P, dst, src, res, eng="sp", reads=()):
    return P.dma(eng, lambda e: e.dma_start(out=dst, in_=src), res, reads=list(reads), writes=[res])


def xload_direct(x_ap):
    def f(P, xt, ti):
        ld(P, xt[:], x_ap[ti * 128:(ti + 1) * 128, :], xt.r)
    return f


def xload_gather(P, x_ap, idx_ap, nt):
    it = T(P, "xidx", [128, nt], I32)
    ld(P, it[:], idx_ap, it.r)
    def f(P, xt, ti):
        P.dma("pool", lambda e: e.indirect_dma_start(out=xt[:], out_offset=None, in_=x_ap, in_offset=bass.IndirectOffsetOnAxis(ap=it[:, ti:ti + 1], axis=0)), xt.r, reads=[it.r], writes=[xt.r])
    return f


class SlabLoader:
    def __init__(self, P, name, lowp, shape=(128, 16, 256)):
        self.P, self.lowp = P, lowp
        self.f = Pool_(P, name, list(shape), 2)
        self.b = Pool_(P, name + "b", list(shape), 2, dt=BF16) if lowp else None
        self.i = 0

    def load(self, src_ap, ncol=256):
        P = self.P
        sl = self.f.next()
        ld(P, sl[:, :, 0:ncol], src_ap, sl.r)
        if not self.lowp:
            return sl
        sb_ = self.b.next()
        eng = ("act", "dve", "pool")[self.i % 3]
        self.i += 1
        if eng == "act":
            P.op("act", lambda e: e.copy(out=sb_[:, :, 0:ncol], in_=sl[:, :, 0:ncol]), reads=[sl.r], writes=[sb_.r])
        else:
            P.op(eng, lambda e: e.tensor_copy(out=sb_[:, :, 0:ncol], in_=sl[:, :, 0:ncol]), reads=[sl.r], writes=[sb_.r])
        return sb_


def st(P, dst, src, res, eng="pool", dram=None):
    return P.dma(eng, lambda e: e.dma_start(out=dst, in_=src), res, reads=[res], writes=[dram] if dram else [])


def build_M():
    nc = bass.Bass("TRN2", target_bir_lowering=False)
    P = Prog(nc)
    c2 = dram_in(nc, "c2", [128, 16])
    Ws = [dram_in(nc, f"adaw{m}", [2048, 6144]) for m in range(4)]
    bs = [dram_in(nc, f"adab{m}", [1, 6144]) for m in range(4)]
    gs = [dram_in(nc, f"g{m}", [1, 2048]) for m in range(4)]
    mods = dram_out(nc, "mods", [4, 3, 2048])
    emit_M(P, c2, Ws, bs, gs, mods)
    P.emit()
    return nc


def emit_M(P, c2, Ws, bs, gs, mods):
    c2t = T(P, "c2t", [128, 16]); sc = T(P, "sc", [128, 16])
    ld(P, c2t[:], c2, c2t.r)
    P.op("act", lambda e: e.activation(out=sc[:], in_=c2t[:], func=AF.Silu), reads=[c2t.r], writes=[sc.r])
    slabs = Pool_(P, "mslab", [128, 16, 256], 2)
    pbank = Pool_(P, "mps", [128, 512], 2, psum=True)
    brow = T(P, "brow", [1, 6144]); grow = T(P, "grow", [1, 2048])
    mrow = T(P, "mrow", [1, 6144]); arow = T(P, "arow", [1, 2048])
    for m in range(4):
        ld(P, brow[:], bs[m], brow.r); ld(P, grow[:], gs[m], grow.r)
        Wv = Ws[m].rearrange("(k p) c -> p k c", p=128)
        for j in range(24):
            sl = slabs.next()
            ld(P, sl[:], Wv[:, :, j * 256:(j + 1) * 256], sl.r)
            pb = pbank.next()
            for k in range(16):
                P.op("pe", lambda e, pb=pb, sl=sl, k=k: e.matmul(pb[0:1, 0:256], lhsT=sc[:, k:k + 1], rhs=sl[:, k, :], start=(k == 0), stop=(k == 15)),
                     reads=[sc.r, sl.r], writes=[pb.r])
            P.op("dve", lambda e, pb=pb, j=j, mrow=mrow, brow=brow: e.tensor_tensor(out=mrow[0:1, j * 256:(j + 1) * 256], in0=pb[0:1, 0:256], in1=brow[0:1, j * 256:(j + 1) * 256], op=ALU.add),
                 reads=[pb.r, brow.r], writes=[mrow.r])
        P.op("dve", lambda e, mrow=mrow, grow=grow, arow=arow: e.scalar_tensor_tensor(out=arow[:], in0=mrow[0:1, 2048:4096], scalar=1.0, in1=grow[:], op0=ALU.add, op1=ALU.mult),
             reads=[mrow.r, grow.r], writes=[arow.r])
        st(P, mods[m, 0:1, :], arow[:], arow.r)
        st(P, mods[m, 1:2, :], mrow[0:1, 0:2048], mrow.r)
        st(P, mods[m, 2:3, :], mrow[0:1, 4096:6144], mrow.r)


class HCtx:
    def __init__(self, P, mods, mi, ident, need_gate=False):
        self.P = P
        self.A = T(P, "Abc", [128, 2048]); self.B = T(P, "Bbc", [128, 2048])
        ld(P, self.A[:], mods[mi, 0:1, :].broadcast_to([128, 2048]), self.A.r)
        ld(P, self.B[:], mods[mi, 1:2, :].broadcast_to([128, 2048]), self.B.r)
        if need_gate:
            self.G = T(P, "Gbc", [128, 2048])
            ld(P, self.G[:], mods[mi, 2:3, :].broadcast_to([128, 2048]), self.G.r)
        self.id = T(P, "ident", [128, 128])
        ld(P, self.id[:], ident, self.id.r)
        self.ss = Pool_(P, "ss", [128, 1], 2)
        self.tp = Pool_(P, "tps", [128, 512], 2, psum=True)
        self.evi = 0

    def norm_mod(self, xt, ht):
        P = self.P
        ss = self.ss.next()
        P.op("act", lambda e: e.activation(out=ht[:], in_=xt[:], func=AF.Square, accum_out=ss[:]), reads=[xt.r], writes=[ht.r, ss.r])
        P.op("dve", lambda e: e.tensor_scalar(out=ss[:], in0=ss[:], scalar1=1.0 / D_MODEL, scalar2=EPS, op0=ALU.mult, op1=ALU.add), reads=[ss.r], writes=[ss.r])
        P.op("act", lambda e: e.activation(out=ss[:], in_=ss[:], func=AF.Sqrt), reads=[ss.r], writes=[ss.r])
        P.op("dve", lambda e: e.reciprocal(out=ss[:], in_=ss[:]), reads=[ss.r], writes=[ss.r])
        P.op("dve", lambda e: e.scalar_tensor_tensor(out=ht[:], in0=xt[:], scalar=ss[:, 0:1], in1=self.A[:], op0=ALU.mult, op1=ALU.mult),
             reads=[xt.r, ss.r, self.A.r], writes=[ht.r])
        P.op("pool", lambda e: e.tensor_tensor(out=ht[:], in0=ht[:], in1=self.B[:], op=ALU.add), reads=[ht.r, self.B.r], writes=[ht.r])

    def evac(self, dst_ap, src_ap, reads, writes, scale=None):
        P = self.P
        self.evi += 1
        if self.evi % 2 == 0:
            if scale is None:
                P.op("act", lambda e: e.copy(out=dst_ap, in_=src_ap), reads=reads, writes=writes)
            else:
                P.op("act", lambda e: e.activation(out=dst_ap, in_=src_ap, func=AF.Copy, scale=scale), reads=reads, writes=writes)
        else:
            if scale is None:
                P.op("dve", lambda e: e.tensor_copy(out=dst_ap, in_=src_ap), reads=reads, writes=writes)
            else:
                P.op("dve", lambda e: e.tensor_scalar(out=dst_ap, in0=src_ap, scalar1=scale, scalar2=None, op0=ALU.mult), reads=reads, writes=writes)

    def transpose_to(self, src, ncols, dst_fn, dst_res):
        P = self.P
        nk = (ncols + 127) // 128
        for k0 in range(0, nk, 4):
            pb = self.tp.next()
            kk = list(range(k0, min(nk, k0 + 4)))
            for k in kk:
                w = min(128, ncols - k * 128)
                P.op("pe", lambda e, pb=pb, k=k, k0=k0, w=w: e.transpose(pb[0:w, (k - k0) * 128:(k - k0 + 1) * 128], src[:, k * 128:k * 128 + w], self.id[:]),
                     reads=[src.r, self.id.r], writes=[pb.r])
            for k in kk:
                w = min(128, ncols - k * 128)
                self.evac(dst_fn(k), pb[0:w, (k - k0) * 128:(k - k0 + 1) * 128], [pb.r], [dst_res])


class Rope:
    def __init__(self, P, invf_ap, half):
        self.P, self.half = P, half
        self.posi = T(P, "posi", [128, 4], I32); self.posf = T(P, "posf", [128, 4])
        self.inv = T(P, "invf", [128, half])
        ld(P, self.inv[:], invf_ap.broadcast_to([128, half]), self.inv.r)
        self.y = T(P, "ropey", [128, 4, half]); self.ki = T(P, "ropeki", [128, 4, half], I32); self.kf = T(P, "ropekf", [128, 4, half])
        self.yp = [T(P, "ypsin", [128, 4, half]), T(P, "ypcos", [128, 4, half])]
        self.tab = [T(P, "tabsin", [128, 4, half]), T(P, "tabcos", [128, 4, half])]
        self.nb = T(P, "negpi", [128, 1])
        P.op("pool", lambda e: e.memset(self.nb[:], -math.pi * 0.999999), writes=[self.nb.r])

    def compute(self, pos_ap):
        P, half = self.P, self.half
        posi, posf, inv, y, ki, kf, nb = self.posi, self.posf, self.inv, self.y, self.ki, self.kf, self.nb
        ld(P, posi[:], pos_ap, posi.r)
        P.op("dve", lambda e: e.tensor_copy(out=posf[:], in_=posi[:]), reads=[posi.r], writes=[posf.r])
        P.op("dve", lambda e: e.tensor_tensor(out=y[:], in0=posf[:].unsqueeze(2).broadcast_to([128, 4, half]), in1=inv[:].unsqueeze(1).broadcast_to([128, 4, half]), op=ALU.mult),
             reads=[posf.r, inv.r], writes=[y.r])
        for yp, tab, off in ((self.yp[0], self.tab[0], 0.5), (self.yp[1], self.tab[1], 0.75)):
            P.op("dve", lambda e, yp=yp, off=off: e.tensor_scalar(out=yp[:], in0=y[:], scalar1=off, scalar2=None, op0=ALU.add), reads=[y.r], writes=[yp.r])
            P.op("dve", lambda e, yp=yp: e.tensor_copy(out=ki[:], in_=yp[:]), reads=[yp.r], writes=[ki.r])
            P.op("dve", lambda e: e.tensor_copy(out=kf[:], in_=ki[:]), reads=[ki.r], writes=[kf.r])
            P.op("dve", lambda e, yp=yp: e.tensor_tensor(out=yp[:], in0=yp[:], in1=kf[:], op=ALU.subtract), reads=[yp.r, kf.r], writes=[yp.r])
            P.op("dve", lambda e, yp=yp: e.tensor_single_scalar(out=kf[:], in_=yp[:], scalar=0.0, op=ALU.is_lt), reads=[yp.r], writes=[kf.r])
            P.op("dve", lambda e, yp=yp: e.tensor_tensor(out=yp[:], in0=yp[:], in1=kf[:], op=ALU.add), reads=[yp.r, kf.r], writes=[yp.r])
            P.op("dve", lambda e, yp=yp: e.tensor_scalar(out=yp[:], in0=yp[:], scalar1=0.0, scalar2=0.999999, op0=ALU.max, op1=ALU.min), reads=[yp.r], writes=[yp.r])
            P.op("act", lambda e, tab=tab, yp=yp: e.activation(out=tab[:], in_=yp[:], func=AF.Sin, bias=nb[:, 0:1], scale=TWO_PI * 0.999999), reads=[yp.r, nb.r], writes=[tab.r])
        return self.tab[0], self.tab[1]


def rope_apply(P, dst, src, sin, cos, ti, nh, half, tmp):
    x1, x2, r1, r2, reads, writes = src
    cb = cos[:, ti, :].unsqueeze(1).broadcast_to([128, nh, half])
    sb_ = sin[:, ti, :].unsqueeze(1).broadcast_to([128, nh, half])
    t1, t2 = tmp
    rd = list(reads) + [sin.r, cos.r]
    P.op("dve", lambda e: e.tensor_tensor(out=t1[:, 0:nh, :], in0=x1, in1=cb, op=ALU.mult), reads=rd, writes=[t1.r])
    P.op("dve", lambda e: e.tensor_tensor(out=t2[:, 0:nh, :], in0=x2, in1=sb_, op=ALU.mult), reads=rd, writes=[t2.r])
    P.op("dve", lambda e: e.tensor_tensor(out=r1, in0=t1[:, 0:nh, :], in1=t2[:, 0:nh, :], op=ALU.subtract), reads=[t1.r, t2.r], writes=writes)
    P.op("dve", lambda e: e.tensor_tensor(out=t1[:, 0:nh, :], in0=x2, in1=cb, op=ALU.mult), reads=rd, writes=[t1.r])
    P.op("dve", lambda e: e.tensor_tensor(out=t2[:, 0:nh, :], in0=x1, in1=sb_, op=ALU.mult), reads=rd, writes=[t2.r])
    P.op("dve", lambda e: e.tensor_tensor(out=r2, in0=t1[:, 0:nh, :], in1=t2[:, 0:nh, :], op=ALU.add), reads=[t1.r, t2.r], writes=writes)


def build_A():
    nc = bass.Bass("TRN2", target_bir_lowering=False)
    P = Prog(nc)
    i_ = dict(
        x=dram_in(nc, "x", [2048, 2048]), pos=dram_in(nc, "pos", [128, NT], I32), mods=dram_in(nc, "mods", [4, 3, 2048]),
        w_in=dram_in(nc, "w_in", [2048, 3904]), qn_g=dram_in(nc, "qn_g", [128, 4]), w_uq=dram_in(nc, "w_uq", [512, 1536]),
        kvn_g=dram_in(nc, "kvn_g", [128, 2]), w_ukv=dram_in(nc, "w_ukv", [256, 2048]), ident=dram_in(nc, "ident", [128, 128]),
        ones=dram_in(nc, "ones", [128, 128]), invf=dram_in(nc, "invf", [1, 32]))
    o_ = dict(
        qT_sb=dram_out(nc, "qT_sb", [8, 128, 2048]), kT_sb=dram_out(nc, "kT_sb", [8, 128, 2048]), v_sb=dram_out(nc, "v_sb", [2048, 1024]),
        qT_mn=dram_out(nc, "qT_mn", [8, 128, 2048]), qT_mr=dram_out(nc, "qT_mr", [4, 128, 2048]), kT_mn=dram_out(nc, "kT_mn", [8, 128, 2048]),
        kT_r=dram_out(nc, "kT_r", [64, 2048]), v_m=dram_out(nc, "v_m", [2048, 1024]))
    emit_A(P, i_, o_)
    P.emit()
    return nc


def emit_A(P, i_, o_, nt=NT):
    H = HCtx(P, i_["mods"], 0, i_["ident"])
    ones = T(P, "ones", [128, 128]); ld(P, ones[:], i_["ones"], ones.r)
    qng = T(P, "qng", [128, 4]); ld(P, qng[:], i_["qn_g"], qng.r)
    kvg = T(P, "kvg", [128, 2]); ld(P, kvg[:], i_["kvn_g"], kvg.r)
    wuq = T(P, "wuq", [128, 4, 1536]); ld(P, wuq[:], i_["w_uq"].rearrange("(k p) c -> p k c", p=128), wuq.r)
    wukv = T(P, "wukv", [128, 2, 2048]); ld(P, wukv[:], i_["w_ukv"].rearrange("(k p) c -> p k c", p=128), wukv.r)
    rope = Rope(P, i_["invf"], 32)
    xs = Pool_(P, "xt", [128, 2048], 2)
    hts = Pool_(P, "ht", [128, 2048], 2)
    hT = T(P, "hT", [128, 16, 512], BF16)
    slabs = SlabLoader(P, "wslab", True)
    acc = Pool_(P, "accps", [128, 512], 4, psum=True)
    stg = Pool_(P, "stg", [128, 512], 3, dt=ADT)
    cqT = T(P, "cqT", [128, 4, 512]); ckvT = T(P, "ckvT", [128, 2, 512])
    sq = T(P, "sqT", [128, 4, 512])
    rstd = T(P, "rstdbc", [128, 512])
    rt1 = T(P, "rt1", [128, 8, 32]); rt2 = T(P, "rt2", [128, 8, 32])
    qr = T(P, "qr", [128, 8, 64]); qrr = T(P, "qrr", [128, 512])
    krt = T(P, "krt", [128, 64]); krr = T(P, "krr", [128, 64])
    Wv = i_["w_in"].rearrange("(k p) c -> p k c", p=128)
    s_sb = 1.0 / math.sqrt(128.0); s_mla = 1.0 / math.sqrt(192.0)
    for g in range(nt // 4):
        gc = slice(g * 512, (g + 1) * 512)
        sin, cos = rope.compute(i_["pos"][:, g * 4:(g + 1) * 4])
        for tl in range(4):
            ti = g * 4 + tl
            xt = xs.next(); ht = hts.next()
            i_["xload"](P, xt, ti)
            H.norm_mod(xt, ht)
            H.transpose_to(ht, 2048, lambda k, tl=tl: hT[:, k, tl * 128:(tl + 1) * 128], hT.r)
        for s in range(16):
            c0 = s * 256
            nc_ = min(256, 3904 - c0)
            sl = slabs.load(Wv[:, :, c0:c0 + nc_], nc_)
            if s < 8 or 12 <= s < 15:
                for half in range(2):
                    pb = acc.next()
                    for k in range(16):
                        P.op("pe", lambda e, pb=pb, sl=sl, k=k, half=half: e.matmul(pb[:, :], lhsT=sl[:, k, half * 128:(half + 1) * 128], rhs=hT[:, k, :], start=(k == 0), stop=(k == 15)),
                             reads=[sl.r, hT.r], writes=[pb.r])
                    if s < 8:
                        hh = (s % 4) * 2 + half
                        sg = stg.next()
                        H.evac(sg[:], pb[:, :], [pb.r], [sg.r], scale=(s_sb if s < 4 else None))
                        dst = o_["qT_sb"] if s < 4 else o_["kT_sb"]
                        st(P, dst[hh, :, gc], sg[:], sg.r)
                    elif s < 14:
                        kc = (s - 12) * 2 + half
                        H.evac(cqT[:, kc, :], pb[:, :], [pb.r], [cqT.r])
                    else:
                        H.evac(ckvT[:, half, :], pb[:, :], [pb.r], [ckvT.r])
            elif s < 12:
                for tl in range(4):
                    ti = g * 4 + tl
                    pb = acc.next()
                    for k in range(16):
                        P.op("pe", lambda e, pb=pb, sl=sl, k=k, tl=tl: e.matmul(pb[:, 0:256], lhsT=hT[:, k, tl * 128:(tl + 1) * 128], rhs=sl[:, k, :], start=(k == 0), stop=(k == 15)),
                             reads=[sl.r, hT.r], writes=[pb.r])
                    sg = stg.next()
                    H.evac(sg[:, 0:256], pb[:, 0:256], [pb.r], [sg.r])
                    st(P, o_["v_sb"][ti * 128:(ti + 1) * 128, (s - 8) * 256:(s - 7) * 256], sg[:, 0:256], sg.r)
            else:
                for tl in range(4):
                    ti = g * 4 + tl
                    pb = acc.next()
                    for k in range(16):
                        P.op("pe", lambda e, pb=pb, sl=sl, k=k, tl=tl: e.matmul(pb[:, 0:64], lhsT=hT[:, k, tl * 128:(tl + 1) * 128], rhs=sl[:, k, 0:64], start=(k == 0), stop=(k == 15)),
                             reads=[sl.r, hT.r], writes=[pb.r])
                    H.evac(krt[:], pb[:, 0:64], [pb.r], [krt.r])
                    rope_apply(P, None, (krt[:, 0:32].unsqueeze(1), krt[:, 32:64].unsqueeze(1), krr[:, 0:32].unsqueeze(1), krr[:, 32:64].unsqueeze(1), [krt.r], [krr.r]),
                               sin, cos, tl, 1, 32, (rt1, rt2))
                    sg = stg.next()
                    pt = H.tp.next()
                    P.op("pe", lambda e, pt=pt: e.transpose(pt[0:64, 0:128], krr[:, 0:64], H.id[:]), reads=[krr.r, H.id.r], writes=[pt.r])
                    H.evac(sg[0:64, 0:128], pt[0:64, 0:128], [pt.r], [sg.r])
                    st(P, o_["kT_r"][:, ti * 128:(ti + 1) * 128], sg[0:64, 0:128], sg.r)
        for (src, nk, gcol, width) in ((cqT, 4, qng, 512.0), (ckvT, 2, kvg, 256.0)):
            P.op("act", lambda e, src=src, nk=nk: e.activation(out=sq[:, 0:nk, :], in_=src[:, 0:nk, :], func=AF.Square), reads=[src.r], writes=[sq.r])
            pb = acc.next()
            for k in range(nk):
                P.op("pe", lambda e, pb=pb, k=k, nk=nk: e.matmul(pb[:, :], lhsT=ones[:], rhs=sq[:, k, :], start=(k == 0), stop=(k == nk - 1)), reads=[ones.r, sq.r], writes=[pb.r])
            P.op("dve", lambda e, pb=pb, width=width: e.tensor_scalar(out=rstd[:], in0=pb[:, :], scalar1=1.0 / width, scalar2=EPS, op0=ALU.mult, op1=ALU.add), reads=[pb.r], writes=[rstd.r])
            P.op("act", lambda e: e.activation(out=rstd[:], in_=rstd[:], func=AF.Sqrt), reads=[rstd.r], writes=[rstd.r])
            P.op("dve", lambda e: e.reciprocal(out=rstd[:], in_=rstd[:]), reads=[rstd.r], writes=[rstd.r])
            for k in range(nk):
                P.op("dve", lambda e, src=src, k=k, gcol=gcol: e.scalar_tensor_tensor(out=src[:, k, :], in0=src[:, k, :], scalar=gcol[:, k:k + 1], in1=rstd[:], op0=ALU.mult, op1=ALU.mult),
                     reads=[src.r, gcol.r, rstd.r], writes=[src.r])
        for hh in range(8):
            pb = acc.next()
            for k in range(4):
                P.op("pe", lambda e, pb=pb, k=k, hh=hh: e.matmul(pb[:, :], lhsT=wuq[:, k, hh * 192:hh * 192 + 128], rhs=cqT[:, k, :], start=(k == 0), stop=(k == 3)),
                     reads=[wuq.r, cqT.r], writes=[pb.r])
            sg = stg.next()
            H.evac(sg[:], pb[:, :], [pb.r], [sg.r], scale=s_mla)
            st(P, o_["qT_mn"][hh, :, gc], sg[:], sg.r)
        for hh in range(8):
            pb = acc.next()
            for k in range(2):
                P.op("pe", lambda e, pb=pb, k=k, hh=hh: e.matmul(pb[:, :], lhsT=wukv[:, k, hh * 256:hh * 256 + 128], rhs=ckvT[:, k, :], start=(k == 0), stop=(k == 1)),
                     reads=[wukv.r, ckvT.r], writes=[pb.r])
            sg = stg.next()
            H.evac(sg[:], pb[:, :], [pb.r], [sg.r])
            st(P, o_["kT_mn"][hh, :, gc], sg[:], sg.r)
        wuq_r = wuq[:].rearrange("p k (h c) -> p k h c", c=192)
        wukv_v = wukv[:].rearrange("p k (h c) -> p k h c", c=256)
        for tl in range(4):
            ti = g * 4 + tl
            tc_ = slice(tl * 128, (tl + 1) * 128)
            pb = acc.next()
            for k in range(4):
                P.op("pe", lambda e, pb=pb, k=k, tc_=tc_: e.matmul(pb[:, :].rearrange("p (h c) -> p h c", c=64), lhsT=cqT[:, k, tc_], rhs=wuq_r[:, k, :, 128:192], start=(k == 0), stop=(k == 3)),
                     reads=[wuq.r, cqT.r], writes=[pb.r])
            H.evac(qr[:], pb[:, :].rearrange("p (h c) -> p h c", c=64), [pb.r], [qr.r], scale=s_mla)
            qrr3 = qrr[:].rearrange("p (h c) -> p h c", c=64)
            rope_apply(P, None, (qr[:, :, 0:32], qr[:, :, 32:64], qrr3[:, :, 0:32], qrr3[:, :, 32:64], [qr.r], [qrr.r]), sin, cos, tl, 8, 32, (rt1, rt2))
            sg = stg.next()
            H.transpose_to(qrr, 512, lambda k, sg=sg: sg[:, k * 128:(k + 1) * 128], sg.r)
            st(P, o_["qT_mr"][:, :, ti * 128:(ti + 1) * 128].rearrange("k p t -> p k t"), sg[:].rearrange("p (k t) -> p k t", t=128), sg.r)
            for vh in range(2):
                pb = acc.next()
                for k in range(2):
                    P.op("pe", lambda e, pb=pb, k=k, tc_=tc_, vh=vh: e.matmul(pb[:, :].rearrange("p (h c) -> p h c", c=128), lhsT=ckvT[:, k, tc_], rhs=wukv_v[:, k, vh * 4:(vh + 1) * 4, 128:256], start=(k == 0), stop=(k == 1)),
                         reads=[wukv.r, ckvT.r], writes=[pb.r])
                sg = stg.next()
                H.evac(sg[:], pb[:, :], [pb.r], [sg.r])
                st(P, o_["v_m"][ti * 128:(ti + 1) * 128, vh * 512:(vh + 1) * 512], sg[:], sg.r)


class Masker:
    def __init__(self, P, masks_ap):
        self.P = P; self.masks = masks_ap
        self.pool = Pool_(P, "mask", [128, 512], 4)
        self.cur = None; self.key = None

    def get(self, kbp):
        if self.key != kbp:
            m = self.pool.next()
            ld(self.P, m[:], self.masks[kbp], m.r)
            self.cur, self.key = m, kbp
        return self.cur

    def apply(self, t, kbp):
        m = self.get(kbp)
        self.P.op("pool", lambda e: e.tensor_tensor(out=t[:], in0=t[:], in1=m[:], op=ALU.mult), reads=[t.r, m.r], writes=[t.r])


def build_B1(heads=8):
    nc = bass.Bass("TRN2", target_bir_lowering=False)
    P = Prog(nc)
    i_ = dict(qT=dram_in(nc, "qT", [8, 128, 2048]), kT=dram_in(nc, "kT", [8, 128, 8192]), v=dram_in(nc, "v", [8, 8192, 128]),
              negT1=dram_in(nc, "negT1", [128, 128]), negOnes=dram_in(nc, "negOnes", [128, 128]), masks=dram_in(nc, "masks", [16, 128, 512]))
    o_ = dict(oT=dram_out(nc, "oT", [8, 128, 2048]))
    emit_B1(P, i_, o_, heads)
    P.emit()
    return nc


def emit_B1(P, i_, o_, heads=8, sched=None):
    if sched is None:
        sched = [(16 * g + 15, 16 * g) for g in range(4)]
    MK = Masker(P, i_["masks"])
    nT1 = T(P, "nT1", [128, 128]); ld(P, nT1[:], i_["negT1"], nT1.r)
    nOn = T(P, "nOn", [128, 128]); ld(P, nOn[:], i_["negOnes"], nOn.r)
    KT = Pool_(P, "KT", [128, 8192], 2, dt=ADT); V = Pool_(P, "V", [128, 64, 128], 2, dt=ADT); QT = Pool_(P, "QT", [128, 512], 3, dt=ADT)
    zb = Pool_(P, "zb", [128, 512], 3, psum=True); lb = Pool_(P, "lb", [128, 512], 3, psum=True); ob = Pool_(P, "ob", [128, 512], 2, psum=True)
    et = Pool_(P, "et", [128, 512], 3); spt = Pool_(P, "spt", [128, 512], 4); At = Pool_(P, "At", [128, 512], 4, dt=ADT)
    sacc = Pool_(P, "sacc", [128, 512], 3); stg = Pool_(P, "ostg", [128, 512], 2, dt=o_["oT"].dtype)
    for h in range(heads):
        kt = KT.next(); v = V.next()
        P.dma("sp", [lambda e, kt=kt, h=h, c=c: e.dma_start(out=kt[:, c * 2048:(c + 1) * 2048], in_=i_["kT"][h, :, c * 2048:(c + 1) * 2048]) for c in range(4)], kt.r, writes=[kt.r])
        vv = i_["v"][h].rearrange("(k p) d -> p k d", p=128)
        P.dma("sp", [lambda e, v=v, vv=vv, c=c: e.dma_start(out=v[:, c * 16:(c + 1) * 16, :], in_=vv[:, c * 16:(c + 1) * 16, :]) for c in range(4)], v.r, writes=[v.r])
        units = []
        for g, (kb_max, span0) in enumerate(sched):
            for kb in range(kb_max, -1, -1):
                units.append(dict(g=g, kb=kb, kb_max=kb_max, span0=span0))
        gstate = {}

        def S1(u):
            g = u["g"]
            if u["kb"] == u["kb_max"]:
                qt = QT.next()
                ld(P, qt[:], i_["qT"][h, :, g * 512:(g + 1) * 512], qt.r)
                gstate[g] = dict(qt=qt, o=ob.next(), sa=None)
            gs_ = gstate[g]; qt = gs_["qt"]
            kblk = kt[:, u["kb"] * 128:(u["kb"] + 1) * 128]
            z = zb.next(); e_ = et.next(); sp = spt.next()
            u.update(kblk=kblk, sp=sp, qt=qt)
            P.op("pe", lambda e, z=z, kblk=kblk, qt=qt: e.matmul(z[:, :], lhsT=kblk, rhs=qt[:], start=True, stop=True), reads=[kt.r, qt.r], writes=[z.r])
            P.op("act", lambda e, z=z, e_=e_: e.activation(out=e_[:], in_=z[:, :], func=AF.Exp), reads=[z.r], writes=[e_.r])
            if u["kb"] >= u["span0"]:
                MK.apply(e_, u["kb"] - u["span0"])
            P.op("act", lambda e, sp=sp, e_=e_: e.activation(out=sp[:], in_=e_[:], func=AF.Ln, bias=1.0, scale=1.0), reads=[e_.r], writes=[sp.r])

        def S2(u):
            gs_ = gstate[u["g"]]; sa = gs_["sa"]; sp = u["sp"]; kblk = u["kblk"]; qt = u["qt"]
            lg = lb.next(); A = At.next()
            u["A"] = A
            P.op("pe", lambda e, lg=lg, kblk=kblk, qt=qt: e.matmul(lg[:, :], lhsT=kblk, rhs=qt[:], start=True, stop=False), reads=[kt.r, qt.r], writes=[lg.r])
            P.op("pe", lambda e, lg=lg, sp=sp, last=(sa is None): e.matmul(lg[:, :], lhsT=nT1[:], rhs=sp[:], start=False, stop=last), reads=[nT1.r, sp.r], writes=[lg.r])
            if sa is not None:
                P.op("pe", lambda e, lg=lg, sa=sa: e.matmul(lg[:, :], lhsT=nOn[:], rhs=sa[:], start=False, stop=True), reads=[nOn.r, sa.r], writes=[lg.r])
            P.op("act", lambda e, lg=lg, A=A: e.activation(out=A[:], in_=lg[:, :], func=AF.Exp), reads=[lg.r], writes=[A.r])
            if u["kb"] >= u["span0"]:
                MK.apply(A, u["kb"] - u["span0"])
            if u["kb"] > 0:
                sn = sacc.next()
                if sa is None:
                    P.op("dve", lambda e, sn=sn, sp=sp: e.tensor_copy(out=sn[:], in_=sp[:]), reads=[sp.r], writes=[sn.r])
                else:
                    P.op("dve", lambda e, sn=sn, sa=sa, sp=sp: e.tensor_tensor(out=sn[:], in0=sa[:], in1=sp[:], op=ALU.add), reads=[sa.r, sp.r], writes=[sn.r])
                gs_["sa"] = sn

        def S3(u):
            gs_ = gstate[u["g"]]; o = gs_["o"]; A = u["A"]; kb = u["kb"]; kb_max = u["kb_max"]; g = u["g"]
            P.op("pe", lambda e, o=o, kb=kb, A=A, kb_max=kb_max, v=v: e.matmul(o[:, :], lhsT=v[:, kb, :], rhs=A[:], start=(kb == kb_max), stop=(kb == 0)), reads=[v.r, A.r], writes=[o.r])
            if kb == 0:
                sg = stg.next()
                P.op("dve", lambda e, sg=sg, o=o: e.tensor_copy(out=sg[:], in_=o[:, :]), reads=[o.r], writes=[sg.r])
                st(P, o_["oT"][h, :, g * 512:(g + 1) * 512], sg[:], sg.r)

        n = len(units)
        for t in range(n + 2):
            if t < n:
                S1(units[t])
            if 0 <= t - 1 < n:
                S2(units[t - 1])
            if 0 <= t - 2 < n:
                S3(units[t - 2])


def build_B2(heads=8):
    nc = bass.Bass("TRN2", target_bir_lowering=False)
    P = Prog(nc)
    i_ = dict(qTn=dram_in(nc, "qTn", [8, 128, 2048]), qTr=dram_in(nc, "qTr", [4, 128, 2048]), kTn=dram_in(nc, "kTn", [8, 128, 8192]),
              kTr=dram_in(nc, "kTr", [64, 8192]), v=dram_in(nc, "v", [8, 8192, 128]), ones=dram_in(nc, "ones", [128, 128]), masks=dram_in(nc, "masks", [16, 128, 512]))
    o_ = dict(oT=dram_out(nc, "oT", [8, 128, 2048]))
    emit_B2(P, i_, o_, heads)
    P.emit()
    return nc


def emit_B2(P, i_, o_, heads=8, sched=None):
    if sched is None:
        sched = [(16 * g + 15, 16 * g) for g in range(4)]
    MK = Masker(P, i_["masks"])
    on32 = T(P, "on32", [128, 128]); ld(P, on32[:], i_["ones"], on32.r)
    on = T(P, "on", [128, 128], ADT)
    P.op("dve", lambda e: e.tensor_copy(out=on[:], in_=on32[:]), reads=[on32.r], writes=[on.r])
    ktr = T(P, "ktr", [64, 8192], ADT); ld(P, ktr[:], i_["kTr"], ktr.r)
    KT = Pool_(P, "KT", [128, 8192], 2, dt=ADT); V = Pool_(P, "V", [128, 64, 128], 2, dt=ADT); QT = Pool_(P, "QT", [128, 512], 3, dt=ADT); QR = Pool_(P, "QR", [64, 512], 3, dt=ADT)
    zb = Pool_(P, "zb", [128, 512], 4, psum=True); ob = Pool_(P, "ob", [128, 512], 2, psum=True); db = Pool_(P, "db", [128, 512], 2, psum=True)
    pt = Pool_(P, "pt", [128, 512], 5, dt=ADT); stg = Pool_(P, "ostg", [128, 512], 2, dt=o_["oT"].dtype); rec = Pool_(P, "rec", [128, 512], 2)
    for h in range(heads):
        kt = KT.next(); v = V.next()
        P.dma("sp", [lambda e, kt=kt, h=h, c=c: e.dma_start(out=kt[:, c * 2048:(c + 1) * 2048], in_=i_["kTn"][h, :, c * 2048:(c + 1) * 2048]) for c in range(4)], kt.r, writes=[kt.r])
        vv = i_["v"][h].rearrange("(k p) d -> p k d", p=128)
        P.dma("sp", [lambda e, v=v, vv=vv, c=c: e.dma_start(out=v[:, c * 16:(c + 1) * 16, :], in_=vv[:, c * 16:(c + 1) * 16, :]) for c in range(4)], v.r, writes=[v.r])
        units = []
        for g, (kb_max, span0) in enumerate(sched):
            for kb in range(kb_max, -1, -1):
                units.append(dict(g=g, kb=kb, kb_max=kb_max, span0=span0))
        gstate = {}

        def S1(u):
            g = u["g"]; gs = slice(g * 512, (g + 1) * 512)
            if u["kb"] == u["kb_max"]:
                qt = QT.next(); qr = QR.next()
                ld(P, qt[:], i_["qTn"][h, :, gs], qt.r)
                ld(P, qr[:], i_["qTr"][h // 2, (h % 2) * 64:(h % 2) * 64 + 64, gs], qr.r)
                gstate[g] = dict(qt=qt, qr=qr, o=ob.next(), d=db.next())
            qt = gstate[g]["qt"]; qr = gstate[g]["qr"]
            ks = slice(u["kb"] * 128, (u["kb"] + 1) * 128)
            z = zb.next(); p = pt.next()
            u["p"] = p
            P.op("pe", lambda e, z=z, qt=qt, ks=ks, kt=kt: e.matmul(z[:, :], lhsT=kt[:, ks], rhs=qt[:], start=True, stop=False), reads=[kt.r, qt.r], writes=[z.r])
            P.op("pe", lambda e, z=z, qr=qr, ks=ks: e.matmul(z[:, :], lhsT=ktr[:, ks], rhs=qr[:], start=False, stop=True), reads=[ktr.r, qr.r], writes=[z.r])
            P.op("act", lambda e, z=z, p=p: e.activation(out=p[:], in_=z[:, :], func=AF.Exp), reads=[z.r], writes=[p.r])
            if u["kb"] >= u["span0"]:
                MK.apply(p, u["kb"] - u["span0"])

        def S2(u):
            g = u["g"]; gs = slice(g * 512, (g + 1) * 512)
            o = gstate[g]["o"]; d = gstate[g]["d"]; p = u["p"]; kb = u["kb"]; kb_max = u["kb_max"]
            P.op("pe", lambda e, o=o, kb=kb, p=p, kb_max=kb_max, v=v: e.matmul(o[:, :], lhsT=v[:, kb, :], rhs=p[:], start=(kb == kb_max), stop=(kb == 0)), reads=[v.r, p.r], writes=[o.r])
            P.op("pe", lambda e, d=d, p=p, kb=kb, kb_max=kb_max: e.matmul(d[:, :], lhsT=on[:], rhs=p[:], start=(kb == kb_max), stop=(kb == 0)), reads=[on.r, p.r], writes=[d.r])
            if kb == 0:
                rc = rec.next(); sg = stg.next()
                P.op("dve", lambda e, rc=rc, d=d: e.reciprocal(out=rc[:], in_=d[:, :]), reads=[d.r], writes=[rc.r])
                P.op("dve", lambda e, sg=sg, o=o, rc=rc: e.tensor_tensor(out=sg[:], in0=o[:, :], in1=rc[:], op=ALU.mult), reads=[o.r, rc.r], writes=[sg.r])
                st(P, o_["oT"][h, :, gs], sg[:], sg.r)

        n = len(units)
        for t in range(n + 2):
            if t < n:
                S1(units[t])
            if 0 <= t - 2 < n:
                S2(units[t - 2])


def build_C1(mi=0):
    nc = bass.Bass("TRN2", target_bir_lowering=False)
    P = Prog(nc)
    i_ = dict(oT=dram_in(nc, "oT", [16, 128, 2048]), w_out=dram_in(nc, "w_out", [2048, 2048]), x=dram_in(nc, "x", [2048, 2048]), mods=dram_in(nc, "mods", [4, 3, 2048]))
    o_ = dict(x1=dram_out(nc, "x1", [2048, 2048]))
    emit_C1(P, i_, o_, mi)
    P.emit()
    return nc


def emit_C1(P, i_, o_, mi, nt=NT):
    lowp = i_["oT"].dtype == BF16
    G = T(P, "Gbc", [128, 2048]); ld(P, G[:], i_["mods"][mi, 2:3, :].broadcast_to([128, 2048]), G.r)
    oT = T(P, "oTg", [128, 16, 512], BF16 if lowp else F32)
    xs = Pool_(P, "xc", [128, 2048], 5)
    slabs = SlabLoader(P, "woslab", lowp)
    acc = Pool_(P, "c1ps", [128, 512], 4, psum=True)
    tmp = Pool_(P, "c1tmp", [128, 256], 2)
    Wv = i_["w_out"].rearrange("(k p) c -> p k c", p=128)
    for g in range(nt // 4):
        P.dma("sp", [lambda e, hu=hu, g=g: e.dma_start(out=oT[:, hu, :], in_=i_["oT"][hu, :, g * 512:(g + 1) * 512]) for hu in range(16)], oT.r, writes=[oT.r])
        xt = []
        for tl in range(4):
            x = xs.next(); ti = g * 4 + tl
            i_["xload"](P, x, ti)
            xt.append(x)
        for s in range(8):
            sl = slabs.load(Wv[:, :, s * 256:(s + 1) * 256])
            cs = slice(s * 256, (s + 1) * 256)
            for tl in range(4):
                pb = acc.next(); x = xt[tl]; tm = tmp.next()
                for k in range(16):
                    P.op("pe", lambda e, pb=pb, sl=sl, k=k, tl=tl: e.matmul(pb[:, 0:256], lhsT=oT[:, k, tl * 128:(tl + 1) * 128], rhs=sl[:, k, :], start=(k == 0), stop=(k == 15)),
                         reads=[oT.r, sl.r], writes=[pb.r])
                P.op("dve", lambda e, pb=pb, tm=tm, cs=cs: e.tensor_tensor(out=tm[:], in0=pb[:, 0:256], in1=G[:, cs], op=ALU.mult), reads=[pb.r, G.r], writes=[tm.r])
                P.op("pool", lambda e, x=x, tm=tm, cs=cs: e.tensor_tensor(out=x[:, cs], in0=x[:, cs], in1=tm[:], op=ALU.add), reads=[x.r, tm.r], writes=[x.r])
        for tl in range(4):
            ti = g * 4 + tl
            st(P, o_["x1"][ti * 128:(ti + 1) * 128, :], xt[tl][:], xt[tl].r, eng="act")


def build_C2(mi=1):
    nc = bass.Bass("TRN2", target_bir_lowering=False)
    P = Prog(nc)
    i_ = dict(x=dram_in(nc, "x", [2048, 2048]), mods=dram_in(nc, "mods", [4, 3, 2048]), w_q=dram_in(nc, "w_q", [2048, 2048]),
              k1T=dram_in(nc, "k1T", [128, 128]), k2T=dram_in(nc, "k2T", [128, 128]), ident=dram_in(nc, "ident", [128, 128]), iota16=dram_in(nc, "iota16", [1, 16]))
    o_ = dict(h=dram_out(nc, "h", [2048, 2048]), ids=dram_out(nc, "ids", [2048, 128], I32), gw=dram_out(nc, "gw", [2048, 128]))
    emit_C2(P, i_, o_, mi)
    P.emit()
    return nc


def emit_C2(P, i_, o_, mi, nt=NT):
    H = HCtx(P, i_["mods"], mi, i_["ident"])
    k1T = T(P, "k1T", [128, 128]); ld(P, k1T[:], i_["k1T"], k1T.r)
    k2T = T(P, "k2T", [128, 128]); ld(P, k2T[:], i_["k2T"], k2T.r)
    io = T(P, "iota16", [128, 16]); ld(P, io[:], i_["iota16"].broadcast_to([128, 16]), io.r)
    xs = Pool_(P, "xt", [128, 2048], 1)
    hts = Pool_(P, "ht", [128, 2048], 2)
    hT = T(P, "hT", [128, 16, 512], BF16)
    slabs = SlabLoader(P, "wqslab", True, shape=(128, 16, 128))
    acc = Pool_(P, "c2acc", [128, 512], 2, psum=True)
    scb = Pool_(P, "c2sc", [128, 512], 2, psum=True)
    qT = Pool_(P, "qTc", [128, 512], 2)
    Sp = Pool_(P, "S", [128, 4, 16, 128], 2)
    Wv = i_["w_q"].rearrange("(k p) c -> p k c", p=128)
    v = T(P, "tv", [128, 16, 16]); ix = T(P, "tix", [128, 16, 16], U32); ixf = T(P, "tixf", [128, 16, 16])
    rep16 = T(P, "trep16", [128, 16, 128]); rep8 = T(P, "trep8", [128, 8, 256]); dmy = T(P, "dmy", [128, 2])
    P.op("dve", lambda e: e.memset(dmy[:], 0.0), writes=[dmy.r])
    vr = [P.res(f"vr{i}") for i in range(16)]; ixr = [P.res(f"ixr{i}") for i in range(16)]; repr_ = [P.res(f"rr{i}") for i in range(16)]
    tr_ = [P.res(f"tr{i}") for i in range(8)]; pr_ = [P.res(f"pr{i}") for i in range(8)]; rr8 = [P.res(f"rr8{i}") for i in range(8)]
    cand = T(P, "cand", [128, 8, 256]); top = T(P, "top", [128, 8, 16]); pos = T(P, "pos", [128, 8, 16], U32)
    au = T(P, "au", [128, 8, 16], U32); bu = T(P, "bu", [128, 8, 16], U32); af = T(P, "af", [128, 8, 16]); bf = T(P, "bf", [128, 8, 16])
    eq = T(P, "eq", [128, 8, 16, 16]); sel1 = T(P, "sel1", [128, 8, 16]); sel2 = T(P, "sel2", [128, 8, 16])
    idf = T(P, "idf", [128, 8, 16]); idi = T(P, "idi", [128, 8, 16], I32)
    gm = T(P, "gm", [128, 8, 16]); gs = T(P, "gs", [128, 8]); gw = T(P, "gw", [128, 8, 16])
    for g in range(nt // 4):
        S = Sp.next()
        for tl in range(4):
            ti = g * 4 + tl
            xt = xs.next(); ht = hts.next()
            ld(P, xt[:], i_["x"][ti * 128:(ti + 1) * 128, :], xt.r)
            H.norm_mod(xt, ht)
            H.transpose_to(ht, 2048, lambda k, tl=tl: hT[:, k, tl * 128:(tl + 1) * 128], hT.r)
            st(P, o_["h"][ti * 128:(ti + 1) * 128, :], ht[:], ht.r)
        for s in range(8):
            for half in range(2):
                sl = slabs.load(Wv[:, :, (s * 2 + half) * 128:(s * 2 + half + 1) * 128], 128)
                pb = acc.next(); q = qT.next()
                for k in range(16):
                    P.op("pe", lambda e, pb=pb, sl=sl, k=k, half=half: e.matmul(pb[:, :], lhsT=sl[:, k, :], rhs=hT[:, k, :], start=(k == 0), stop=(k == 15)),
                         reads=[sl.r, hT.r], writes=[pb.r])
                H.evac(q[:], pb[:, :], [pb.r], [q.r])
                sb_ = scb.next(); kT = k1T if half == 0 else k2T
                for tl in range(4):
                    P.op("pe", lambda e, sb_=sb_, q=q, tl=tl, kT=kT: e.matmul(sb_[:, tl * 128:(tl + 1) * 128], lhsT=q[:, tl * 128:(tl + 1) * 128], rhs=kT[:], start=True, stop=True),
                         reads=[q.r, kT.r], writes=[sb_.r])
                H.evac(S[:, :, s * 2 + half, :], sb_[:, :].rearrange("p (t n) -> p t n", n=128), [sb_.r], [S.r])
        for tl in range(4):
            ti = g * 4 + tl
            for cc in range(16):
                P.op("dve", lambda e, cc=cc, tl=tl, S=S: e.max(out=v[:, cc, 0:8], in_=S[:, tl, cc, :]), reads=[S.r], writes=[vr[cc]])
            for cc in range(16):
                P.op("dve", lambda e, cc=cc, tl=tl, S=S: e.max_index(out=ix[:, cc, 0:8], in_max=v[:, cc, 0:8], in_values=S[:, tl, cc, :]), reads=[S.r, vr[cc]], writes=[ixr[cc]])
            for cc in range(16):
                P.op("dve", lambda e, cc=cc, tl=tl, S=S: e.match_replace(out=rep16[:, cc, :], in_to_replace=v[:, cc, 0:8], in_values=S[:, tl, cc, :], imm_value=-1e30), reads=[S.r, vr[cc]], writes=[repr_[cc]])
            for cc in range(16):
                P.op("dve", lambda e, cc=cc: e.max(out=v[:, cc, 8:16], in_=rep16[:, cc, :]), reads=[repr_[cc]], writes=[vr[cc]])
            for cc in range(16):
                P.op("dve", lambda e, cc=cc: e.max_index(out=ix[:, cc, 8:16], in_max=v[:, cc, 8:16], in_values=rep16[:, cc, :]), reads=[repr_[cc], vr[cc]], writes=[ixr[cc]])
            P.op("dve", lambda e: e.engine_nop() if False else e.tensor_copy(out=ixf[:, 0:1, 0:1], in_=ix[:, 0:1, 0:1]), reads=vr + ixr, writes=[v.r, ix.r])
            P.op("dve", lambda e: e.tensor_copy(out=ixf[:], in_=ix[:]), reads=[ix.r], writes=[ixf.r])
            v4 = v[:].rearrange("p (h j) k -> p h j k", j=2); i4 = ixf[:].rearrange("p (h j) k -> p h j k", j=2)
            v1, v2, i1, i2 = v4[:, :, 0, :], v4[:, :, 1, :], i4[:, :, 0, :], i4[:, :, 1, :]
            c4 = cand[:].rearrange("p h (a b) -> p h a b", b=16)
            P.op("dve", lambda e, v1=v1, v2=v2, c4=c4: e.tensor_tensor(out=c4, in0=v1.unsqueeze(3).broadcast_to([128, 8, 16, 16]), in1=v2.unsqueeze(2).broadcast_to([128, 8, 16, 16]), op=ALU.add),
                 reads=[v.r], writes=[cand.r])
            P.op("dve", lambda e: e.tensor_copy(out=dmy[:, 0:1], in_=dmy[:, 1:2]), reads=[v.r, ix.r, dmy.r], writes=vr + ixr + repr_ + [dmy.r])
            for h in range(8):
                P.op("dve", lambda e, h=h: e.max(out=top[:, h, 0:8], in_=cand[:, h, :]), reads=[cand.r], writes=[tr_[h]])
            for h in range(8):
                P.op("dve", lambda e, h=h: e.max_index(out=pos[:, h, 0:8], in_max=top[:, h, 0:8], in_values=cand[:, h, :]), reads=[cand.r, tr_[h]], writes=[pr_[h]])
            for h in range(8):
                P.op("dve", lambda e, h=h: e.match_replace(out=rep8[:, h, :], in_to_replace=top[:, h, 0:8], in_values=cand[:, h, :], imm_value=-1e30), reads=[cand.r, tr_[h]], writes=[rr8[h]])
            for h in range(8):
                P.op("dve", lambda e, h=h: e.max(out=top[:, h, 8:16], in_=rep8[:, h, :]), reads=[rr8[h]], writes=[tr_[h]])
            for h in range(8):
                P.op("dve", lambda e, h=h: e.max_index(out=pos[:, h, 8:16], in_max=top[:, h, 8:16], in_values=rep8[:, h, :]), reads=[rr8[h], tr_[h]], writes=[pr_[h]])
            P.op("dve", lambda e: e.tensor_copy(out=au[:, 0:1, 0:1], in_=pos[:, 0:1, 0:1]), reads=tr_ + pr_, writes=[top.r, pos.r])
            P.op("dve", lambda e: e.tensor_single_scalar(out=au[:], in_=pos[:], scalar=4, op=ALU.logical_shift_right), reads=[pos.r], writes=[au.r])
            P.op("dve", lambda e: e.tensor_single_scalar(out=bu[:], in_=pos[:], scalar=15, op=ALU.bitwise_and), reads=[pos.r], writes=[bu.r])
            P.op("dve", lambda e: e.tensor_copy(out=af[:], in_=au[:]), reads=[au.r], writes=[af.r])
            P.op("dve", lambda e: e.tensor_copy(out=bf[:], in_=bu[:]), reads=[bu.r], writes=[bf.r])
            iob = io[:].unsqueeze(1).unsqueeze(1).broadcast_to([128, 8, 16, 16])
            for (xf, tab, sel) in ((af, i1, sel1), (bf, i2, sel2)):
                P.op("dve", lambda e, xf=xf: e.tensor_tensor(out=eq[:], in0=xf[:].unsqueeze(3).broadcast_to([128, 8, 16, 16]), in1=iob, op=ALU.is_equal), reads=[xf.r, io.r], writes=[eq.r])
                P.op("dve", lambda e, tab=tab: e.tensor_tensor(out=eq[:], in0=eq[:], in1=tab.unsqueeze(2).broadcast_to([128, 8, 16, 16]), op=ALU.mult), reads=[eq.r, ixf.r], writes=[eq.r])
                P.op("dve", lambda e, sel=sel: e.tensor_reduce(out=sel[:], in_=eq[:], axis=AX.X, op=ALU.add), reads=[eq.r], writes=[sel.r])
            P.op("dve", lambda e: e.scalar_tensor_tensor(out=idf[:], in0=sel1[:], scalar=128.0, in1=sel2[:], op0=ALU.mult, op1=ALU.add), reads=[sel1.r, sel2.r], writes=[idf.r])
            P.op("dve", lambda e: e.tensor_copy(out=idi[:], in_=idf[:]), reads=[idf.r], writes=[idi.r])
            st(P, o_["ids"][ti * 128:(ti + 1) * 128, :], idi[:].rearrange("p h k -> p (h k)"), idi.r)
            P.op("dve", lambda e: e.tensor_tensor(out=gm[:], in0=top[:], in1=top[:, :, 0:1].broadcast_to([128, 8, 16]), op=ALU.subtract), reads=[top.r], writes=[gm.r])
            P.op("act", lambda e: e.activation(out=gm[:], in_=gm[:], func=AF.Exp), reads=[gm.r], writes=[gm.r])
            P.op("dve", lambda e: e.tensor_reduce(out=gs[:], in_=gm[:], axis=AX.X, op=ALU.add), reads=[gm.r], writes=[gs.r])
            P.op("dve", lambda e: e.reciprocal(out=gs[:], in_=gs[:]), reads=[gs.r], writes=[gs.r])
            P.op("dve", lambda e: e.tensor_tensor(out=gw[:], in0=gm[:], in1=gs[:].unsqueeze(2).broadcast_to([128, 8, 16]), op=ALU.mult), reads=[gm.r, gs.r], writes=[gw.r])
            st(P, o_["gw"][ti * 128:(ti + 1) * 128, :], gw[:].rearrange("p h k -> p (h k)"), gw.r)
            P.op("dve", lambda e: e.tensor_copy(out=dmy[:, 0:1], in_=dmy[:, 1:2]), reads=[top.r, pos.r, dmy.r], writes=tr_ + pr_ + rr8 + [dmy.r])


def build_C3(mi=1, ntiles=NT, final=False):
    nc = bass.Bass("TRN2", target_bir_lowering=False)
    P = Prog(nc)
    i_ = dict(x=dram_in(nc, "x", [2048, 2048]), h=dram_in(nc, "h", [2048, 2048]), ids=dram_in(nc, "ids", [2048, 128], I32), gw=dram_in(nc, "gw", [2048, 128]),
              mods=dram_in(nc, "mods", [4, 3, 2048]), u=dram_in(nc, "u", [16384, 2048]), v=dram_in(nc, "v", [16384, 2048]))
    if final:
        i_["fin_g"] = dram_in(nc, "fin_g", [1, 2048])
    o_ = dict(x2=dram_out(nc, "x2", [2048, 2048]))
    emit_C3(P, i_, o_, mi, ntiles)
    P.emit()
    return nc


def emit_PC(P, src, dst, half):
    a = Pool_(P, "pcin", [128, 8192], 2); b = Pool_(P, "pcout", [128, 8192], 2, dt=BF16)
    sv = src.rearrange("(n p r) d -> n p (r d)", p=128, r=4)
    dv = dst[:, half * 2048:(half + 1) * 2048].rearrange("(n p r) d -> n p r d", p=128, r=4)
    for n in range(32):
        t = a.next(); o = b.next()
        ld(P, t[:], sv[n], t.r)
        eng = ("act", "dve", "pool")[n % 3]
        if eng == "act":
            P.op("act", lambda e, t=t, o=o: e.copy(out=o[:], in_=t[:]), reads=[t.r], writes=[o.r])
        else:
            P.op(eng, lambda e, t=t, o=o: e.tensor_copy(out=o[:], in_=t[:]), reads=[t.r], writes=[o.r])
        st(P, dv[n], o[:].rearrange("p (r d) -> p r d", r=4), o.r, eng="act")


def emit_C3f(P, i_, o_, mi, ntiles=NT):
    G = T(P, "Gbc", [128, 2048]); ld(P, G[:], i_["mods"][mi, 2:3, :].broadcast_to([128, 2048]), G.r)
    xs = Pool_(P, "x3", [128, 2048], 2); hs = Pool_(P, "h3", [128, 2048], 2)
    idp = Pool_(P, "id3", [128, 128], 2, dt=I32); gwp = Pool_(P, "gw3", [128, 128], 2)
    gb = Pool_(P, "gath", [128, 4096], 6, dt=BF16)
    junk = T(P, "junk3", [128, 2048])
    acol = Pool_(P, "acol", [128, 1], 8); gcol = Pool_(P, "gcol", [128, 1], 8); wcol = Pool_(P, "wcol", [128, 1], 8)
    accp = Pool_(P, "acc3", [128, 2048], 2)
    idb = T(P, "idb3", [128, 128]); ld(P, idb[:], i_["ident"], idb.r)
    dg = Pool_(P, "diag3", [128, 128], 6, dt=BF16)
    pacc = [T(P, f"pacc{k}", [128, 512], psum=True) for k in range(4)]
    final = "fin_g" in i_
    if final:
        gF = T(P, "gF", [128, 2048]); ld(P, gF[:], i_["fin_g"].broadcast_to([128, 2048]), gF.r)
        fss = Pool_(P, "fss", [128, 1], 2)
    for ti in range(ntiles):
        rs = slice(ti * 128, (ti + 1) * 128)
        x = xs.next(); h = hs.next(); idt = idp.next(); gwt = gwp.next(); acc = accp.next()
        ld(P, h[:], i_["h"][rs, :], h.r); ld(P, idt[:], i_["ids"][rs, :], idt.r); ld(P, gwt[:], i_["gw"][rs, :], gwt.r); ld(P, x[:], i_["x"][rs, :], x.r)
        for s in range(128):
            ug = gb.next(); ac = acol.next(); gc_ = gcol.next(); wc = wcol.next(); d_ = dg.next()
            P.dma("pool", lambda e, ug=ug, idt=idt, s=s: e.indirect_dma_start(out=ug[:], out_offset=None, in_=i_["uv"], in_offset=bass.IndirectOffsetOnAxis(ap=idt[:, s:s + 1], axis=0)),
                  ug.r, reads=[idt.r], writes=[ug.r])
            P.op("dve", lambda e, ug=ug, h=h, ac=ac: e.scalar_tensor_tensor(out=junk[:], in0=ug[:, 0:2048], scalar=1.0, in1=h[:], op0=ALU.mult, op1=ALU.mult, accum_out=ac[:, 0:1]),
                 reads=[ug.r, h.r], writes=[junk.r, ac.r])
            P.op("act", lambda e, ac=ac, gc_=gc_: e.activation(out=gc_[:], in_=ac[:], func=AF.Gelu), reads=[ac.r], writes=[gc_.r])
            P.op("dve", lambda e, gc_=gc_, wc=wc, gwt=gwt, s=s: e.tensor_tensor(out=wc[:], in0=gc_[:], in1=gwt[:, s:s + 1], op=ALU.mult), reads=[gc_.r, gwt.r], writes=[wc.r])
            P.op("act", lambda e, d_=d_, wc=wc: e.activation(out=d_[:], in_=idb[:], func=AF.Copy, scale=wc[:, 0:1]), reads=[idb.r, wc.r], writes=[d_.r])
            for k in range(4):
                P.op("pe", lambda e, d_=d_, ug=ug, k=k, s=s: e.matmul(pacc[k][:, :], lhsT=d_[:], rhs=ug[:, 2048 + k * 512:2048 + (k + 1) * 512], start=(s == 0), stop=(s == 127)),
                     reads=[d_.r, ug.r], writes=[pacc[k].r])
        for k in range(4):
            ks = slice(k * 512, (k + 1) * 512)
            P.op("dve", lambda e, acc=acc, k=k, ks=ks: e.tensor_tensor(out=acc[:, ks], in0=pacc[k][:, :], in1=G[:, ks], op=ALU.mult), reads=[pacc[k].r, G.r], writes=[acc.r])
        P.op("pool", lambda e, acc=acc, x=x: e.tensor_tensor(out=acc[:], in0=acc[:], in1=x[:], op=ALU.add), reads=[acc.r, x.r], writes=[acc.r])
        if final:
            ss = fss.next()
            P.op("act", lambda e, acc=acc, ss=ss: e.activation(out=junk[:], in_=acc[:], func=AF.Square, accum_out=ss[:]), reads=[acc.r], writes=[junk.r, ss.r])
            P.op("dve", lambda e, ss=ss: e.tensor_scalar(out=ss[:], in0=ss[:], scalar1=1.0 / 2048.0, scalar2=1e-6, op0=ALU.mult, op1=ALU.add), reads=[ss.r], writes=[ss.r])
            P.op("act", lambda e, ss=ss: e.activation(out=ss[:], in_=ss[:], func=AF.Sqrt), reads=[ss.r], writes=[ss.r])
            P.op("dve", lambda e, ss=ss: e.reciprocal(out=ss[:], in_=ss[:]), reads=[ss.r], writes=[ss.r])
            P.op("dve", lambda e, acc=acc, ss=ss: e.scalar_tensor_tensor(out=acc[:], in0=acc[:], scalar=ss[:, 0:1], in1=gF[:], op0=ALU.mult, op1=ALU.mult), reads=[acc.r, ss.r, gF.r], writes=[acc.r])
        st(P, o_["x2"][rs, :], acc[:], acc.r, eng="act")


def emit_C3(P, i_, o_, mi, ntiles=NT):
    bf = i_["u"].dtype == BF16
    G = T(P, "Gbc", [128, 2048]); ld(P, G[:], i_["mods"][mi, 2:3, :].broadcast_to([128, 2048]), G.r)
    xs = Pool_(P, "x3", [128, 2048], 2); hs = Pool_(P, "h3", [128, 2048], 2)
    idp = Pool_(P, "id3", [128, 128], 2, dt=I32); gwp = Pool_(P, "gw3", [128, 128], 2)
    gb = Pool_(P, "gath", [128, 2048], 8 if bf else 6, dt=(BF16 if bf else F32))
    junk = T(P, "junk3", [128, 2048])
    actp = Pool_(P, "act3", [128, 128], 2); wp = Pool_(P, "w3", [128, 128], 2)
    accp = Pool_(P, "acc3", [128, 2048], 2)
    if bf:
        idb = T(P, "idb3", [128, 128]); ld(P, idb[:], i_["ident"], idb.r)
        dg = Pool_(P, "diag3", [128, 128], 4, dt=BF16)
        pacc = [T(P, f"pacc{k}", [128, 512], psum=True) for k in range(4)]
    final = "fin_g" in i_
    if final:
        gF = T(P, "gF", [128, 2048]); ld(P, gF[:], i_["fin_g"].broadcast_to([128, 2048]), gF.r)
        fss = Pool_(P, "fss", [128, 1], 2)
    for ti in range(ntiles):
        rs = slice(ti * 128, (ti + 1) * 128)
        x = xs.next(); h = hs.next(); idt = idp.next(); gwt = gwp.next(); act = actp.next(); w = wp.next(); acc = accp.next()
        ld(P, h[:], i_["h"][rs, :], h.r); ld(P, idt[:], i_["ids"][rs, :], idt.r); ld(P, gwt[:], i_["gw"][rs, :], gwt.r); ld(P, x[:], i_["x"][rs, :], x.r)
        for s in range(128):
            ug = gb.next()
            P.dma("pool", lambda e, ug=ug, idt=idt, s=s: e.indirect_dma_start(out=ug[:], out_offset=None, in_=i_["u"], in_offset=bass.IndirectOffsetOnAxis(ap=idt[:, s:s + 1], axis=0)),
                  ug.r, reads=[idt.r], writes=[ug.r])
            P.op("dve", lambda e, ug=ug, h=h, act=act, s=s: e.scalar_tensor_tensor(out=junk[:], in0=ug[:], scalar=1.0, in1=h[:], op0=ALU.mult, op1=ALU.mult, accum_out=act[:, s:s + 1]),
                 reads=[ug.r, h.r], writes=[junk.r, act.r])
        P.op("act", lambda e, act=act: e.activation(out=act[:], in_=act[:], func=AF.Gelu), reads=[act.r], writes=[act.r])
        P.op("dve", lambda e, act=act, w=w, gwt=gwt: e.tensor_tensor(out=w[:], in0=act[:], in1=gwt[:], op=ALU.mult), reads=[act.r, gwt.r], writes=[w.r])
        for s in range(128):
            vg = gb.next()
            P.dma("pool", lambda e, vg=vg, idt=idt, s=s: e.indirect_dma_start(out=vg[:], out_offset=None, in_=i_["v"], in_offset=bass.IndirectOffsetOnAxis(ap=idt[:, s:s + 1], axis=0)),
                  vg.r, reads=[idt.r], writes=[vg.r])
            if bf:
                d_ = dg.next()
                P.op("act", lambda e, d_=d_, w=w, s=s: e.activation(out=d_[:], in_=idb[:], func=AF.Copy, scale=w[:, s:s + 1]), reads=[idb.r, w.r], writes=[d_.r])
                for k in range(4):
                    P.op("pe", lambda e, d_=d_, vg=vg, k=k, s=s: e.matmul(pacc[k][:, :], lhsT=d_[:], rhs=vg[:, k * 512:(k + 1) * 512], start=(s == 0), stop=(s == 127)),
                         reads=[d_.r, vg.r], writes=[pacc[k].r])
            elif s == 0:
                P.op("dve", lambda e, vg=vg, w=w, acc=acc: e.tensor_scalar(out=acc[:], in0=vg[:], scalar1=w[:, 0:1], scalar2=None, op0=ALU.mult), reads=[vg.r, w.r], writes=[acc.r])
            else:
                P.op("dve", lambda e, vg=vg, w=w, acc=acc, s=s: e.scalar_tensor_tensor(out=acc[:], in0=vg[:], scalar=w[:, s:s + 1], in1=acc[:], op0=ALU.mult, op1=ALU.add),
                     reads=[vg.r, w.r, acc.r], writes=[acc.r])
        if bf:
            for k in range(4):
                ks = slice(k * 512, (k + 1) * 512)
                P.op("dve", lambda e, acc=acc, k=k, ks=ks: e.tensor_tensor(out=acc[:, ks], in0=pacc[k][:, :], in1=G[:, ks], op=ALU.mult), reads=[pacc[k].r, G.r], writes=[acc.r])
        else:
            P.op("dve", lambda e, acc=acc: e.tensor_tensor(out=acc[:], in0=acc[:], in1=G[:], op=ALU.mult), reads=[acc.r, G.r], writes=[acc.r])
        P.op("pool", lambda e, acc=acc, x=x: e.tensor_tensor(out=acc[:], in0=acc[:], in1=x[:], op=ALU.add), reads=[acc.r, x.r], writes=[acc.r])
        if final:
            ss = fss.next()
            P.op("act", lambda e, acc=acc, ss=ss: e.activation(out=junk[:], in_=acc[:], func=AF.Square, accum_out=ss[:]), reads=[acc.r], writes=[junk.r, ss.r])
            P.op("dve", lambda e, ss=ss: e.tensor_scalar(out=ss[:], in0=ss[:], scalar1=1.0 / 2048.0, scalar2=1e-6, op0=ALU.mult, op1=ALU.add), reads=[ss.r], writes=[ss.r])
            P.op("act", lambda e, ss=ss: e.activation(out=ss[:], in_=ss[:], func=AF.Sqrt), reads=[ss.r], writes=[ss.r])
            P.op("dve", lambda e, ss=ss: e.reciprocal(out=ss[:], in_=ss[:]), reads=[ss.r], writes=[ss.r])
            P.op("dve", lambda e, acc=acc, ss=ss: e.scalar_tensor_tensor(out=acc[:], in0=acc[:], scalar=ss[:, 0:1], in1=gF[:], op0=ALU.mult, op1=ALU.mult), reads=[acc.r, ss.r, gF.r], writes=[acc.r])
        st(P, o_["x2"][rs, :], acc[:], acc.r, eng="act")


def build_D1():
    nc = bass.Bass("TRN2", target_bir_lowering=False)
    P = Prog(nc)
    i_ = dict(x=dram_in(nc, "x", [2048, 2048]), pos=dram_in(nc, "pos", [128, NT], I32), mods=dram_in(nc, "mods", [4, 3, 2048]),
              w_in=dram_in(nc, "w_in", [2048, 3632]), ident=dram_in(nc, "ident", [128, 128]), invf=dram_in(nc, "invf", [1, 16]))
    o_ = dict(qT=dram_out(nc, "qT", [16, 128, 2048]), kcT=dram_out(nc, "kcT", [2, 128, 2048]), ksT=dram_out(nc, "ksT", [2, 128, 2048]), kwT=dram_out(nc, "kwT", [2, 128, 2048]),
              vc=dram_out(nc, "vc", [2048, 256]), vs=dram_out(nc, "vs", [2048, 256]), vw=dram_out(nc, "vw", [2048, 256]), gT=dram_out(nc, "gT", [48, 2048]))
    emit_D1(P, i_, o_, 2)
    P.emit()
    return nc


def emit_D1(P, i_, o_, mi, nt, part):
    H = HCtx(P, i_["mods"], mi, i_["ident"])
    rope = Rope(P, i_["invf"], 16)
    xs = Pool_(P, "xt", [128, 2048], 2)
    hts = Pool_(P, "ht", [128, 2048], 2)
    lowp = (part == "k")
    hT = T(P, "hT", [128, 16, 512], BF16 if lowp else F32)
    slabs = SlabLoader(P, "wslab", lowp)
    acc = Pool_(P, "accps", [128, 512], 4, psum=True)
    tm = Pool_(P, "tm", [128, 2, 128], 3)
    rr = T(P, "rr", [128, 2, 32]); rt1 = T(P, "rt1", [128, 2, 16]); rt2 = T(P, "rt2", [128, 2, 16])
    stg = Pool_(P, "stg", [128, 2, 512], 2)
    stgb = Pool_(P, "stgb", [128, 2, 512], 2, dt=ADT)
    vst = Pool_(P, "vst", [128, 256], 3, dt=ADT)
    tmb = Pool_(P, "tmb", [128, 2, 128], 2, dt=ADT)
    gst = T(P, "gst", [128, 48]); gTs = T(P, "gTs", [48, 512])
    Wv = i_["w_in"].rearrange("(k p) c -> p k c", p=128)
    s_nsa = 1.0 / math.sqrt(128.0)
    allkinds = ["q"] * 8 + ["kc", "vc", "ks", "vs", "kw", "vw", "gl"]
    sel = [(s_, k) for s_, k in enumerate(allkinds) if (k in ("q", "gl")) == (part == "q")]
    if part == "k":
        zt = T(P, "zpad", [128, 128], ADT)
        P.op("pool", lambda e: e.memset(zt[:], 0.0), writes=[zt.r])
        for nm in ("kw", "vw"):
            for g2 in range(2):
                st(P, o_[nm][g2][nt * 128:nt * 128 + 128, :], zt[:], zt.r)
    for g in range(nt // 4):
        gc = slice(g * 512, (g + 1) * 512)
        sin, cos = rope.compute(i_["pos"][:, g * 4:(g + 1) * 4])
        for tl in range(4):
            ti = g * 4 + tl
            xt = xs.next(); ht = hts.next()
            i_["xload"](P, xt, ti)
            H.norm_mod(xt, ht)
            H.transpose_to(ht, 2048, lambda k, tl=tl: hT[:, k, tl * 128:(tl + 1) * 128], hT.r)
        for s, kind in sel:
            c0 = s * 256
            ncol = min(256, 3632 - c0)
            sl = slabs.load(Wv[:, :, c0:c0 + ncol], ncol)
            sg = (stgb.next() if kind == "ks" else stg.next()) if kind in ("q", "kc", "ks", "vc") else None
            for tl in range(4):
                ti = g * 4 + tl
                pb = acc.next()
                for k in range(16):
                    P.op("pe", lambda e, pb=pb, sl=sl, k=k, tl=tl, ncol=ncol: e.matmul(pb[:, 0:ncol], lhsT=hT[:, k, tl * 128:(tl + 1) * 128], rhs=sl[:, k, 0:ncol], start=(k == 0), stop=(k == 15)),
                         reads=[sl.r, hT.r], writes=[pb.r])
                if kind in ("q", "kc", "ks", "kw", "vc"):
                    t_ = tm.next()
                    H.evac(t_[:], pb[:, 0:256].rearrange("p (h d) -> p h d", d=128), [pb.r], [t_.r], scale=(s_nsa if kind == "q" else None))
                    if kind != "vc":
                        rope_apply(P, None, (t_[:, :, 0:16], t_[:, :, 16:32], rr[:, :, 0:16], rr[:, :, 16:32], [t_.r], [rr.r]), sin, cos, tl, 2, 16, (rt1, rt2))
                        P.op("pool", lambda e, t_=t_: e.tensor_copy(out=t_[:, :, 0:32], in_=rr[:]), reads=[rr.r], writes=[t_.r])
                    if kind == "kw":
                        tb_ = tmb.next()
                        P.op("act", lambda e, tb_=tb_, t_=t_: e.copy(out=tb_[:], in_=t_[:]), reads=[t_.r], writes=[tb_.r])
                        for g2 in range(2):
                            st(P, o_["kw"][g2][ti * 128:(ti + 1) * 128, :], tb_[:, g2, :], tb_.r)
                    else:
                        pt = H.tp.next()
                        for hh in range(2):
                            P.op("pe", lambda e, pt=pt, t_=t_, hh=hh: e.transpose(pt[:, hh * 128:(hh + 1) * 128], t_[:, hh, :], H.id[:]), reads=[t_.r, H.id.r], writes=[pt.r])
                        H.evac(sg[:, :, tl * 128:(tl + 1) * 128], pt[:, 0:256].rearrange("p (h t) -> p h t", t=128), [pt.r], [sg.r])
                elif kind == "gl":
                    P.op("act", lambda e, pb=pb: e.activation(out=gst[:], in_=pb[:, 0:48], func=AF.Sigmoid), reads=[pb.r], writes=[gst.r])
                    pt = H.tp.next()
                    P.op("pe", lambda e, pt=pt: e.transpose(pt[0:48, 0:128], gst[:, 0:48], H.id[:]), reads=[gst.r, H.id.r], writes=[pt.r])
                    H.evac(gTs[:, tl * 128:(tl + 1) * 128], pt[0:48, 0:128], [pt.r], [gTs.r])
                elif kind == "vs":
                    vt = vst.next()
                    H.evac(vt[:], pb[:, 0:256], [pb.r], [vt.r])
                    st(P, o_["vs"][ti * 128:(ti + 1) * 128, :], vt[:], vt.r)
                else:
                    vt = vst.next()
                    H.evac(vt[:], pb[:, 0:256], [pb.r], [vt.r])
                    for g2 in range(2):
                        st(P, o_["vw"][g2][ti * 128:(ti + 1) * 128, :], vt[:, g2 * 128:(g2 + 1) * 128], vt.r)
            if kind == "q":
                st(P, o_["qT"][2 * s:2 * s + 2, :, gc].rearrange("h p t -> p h t"), sg[:], sg.r)
                sgb = stgb.next()
                P.op("pool", lambda e, sgb=sgb, sg=sg: e.tensor_copy(out=sgb[:], in_=sg[:]), reads=[sg.r], writes=[sgb.r])
                st(P, o_["qTb"][2 * s:2 * s + 2, :, gc].rearrange("h p t -> p h t"), sgb[:], sgb.r)
            elif kind in ("kc", "ks", "vc"):
                st(P, o_[kind + "T"][:, :, gc].rearrange("h p t -> p h t"), sg[:], sg.r)
            elif kind == "gl":
                st(P, o_["gT"][:, gc], gTs[:], gTs.r)


def build_D2():
    nc = bass.Bass("TRN2", target_bir_lowering=False)
    P = Prog(nc)
    i_ = dict(kcT=dram_in(nc, "kcT", [2, 128, 8192]), vcT=dram_in(nc, "vcT", [2, 128, 8192]), pekT=dram_in(nc, "pekT", [128, 32]), pevT=dram_in(nc, "pevT", [128, 32]),
              w1k=dram_in(nc, "w1k", [4096, 256]), w2k=dram_in(nc, "w2k", [256, 128]), w1v=dram_in(nc, "w1v", [4096, 256]), w2v=dram_in(nc, "w2v", [256, 128]))
    o_ = dict(kccT=dram_out(nc, "kccT", [2, 128, 512]), vcc=dram_out(nc, "vcc", [2, 512, 128]))
    emit_D2(P, i_, o_)
    P.emit()
    return nc


def emit_D2(P, i_, o_):
    w1 = T(P, "w1", [128, 32, 256]); w2 = T(P, "w2", [128, 2, 128]); pe = T(P, "pe", [128, 32])
    src = Pool_(P, "csrc", [128, 8192], 2)
    hps = Pool_(P, "hps", [128, 512], 2, psum=True); bps = T(P, "bps", [128, 512], psum=True); ops_ = Pool_(P, "ops", [128, 512], 2, psum=True)
    bias = T(P, "cbias", [128, 2]); hid = T(P, "hid", [128, 2, 512]); og = Pool_(P, "og", [128, 512], 2)
    for kv in range(2):
        nm = "k" if kv == 0 else "v"
        ld(P, w1[:], i_["w1" + nm].rearrange("(l p) c -> p l c", p=128), w1.r)
        ld(P, w2[:], i_["w2" + nm].rearrange("(k p) d -> p k d", p=128), w2.r)
        ld(P, pe[:], i_["pe" + nm + "T"], pe.r)
        for cc in range(2):
            for l in range(32):
                P.op("pe", lambda e, cc=cc, l=l: e.matmul(bps[:, cc:cc + 1], lhsT=w1[:, l, cc * 128:(cc + 1) * 128], rhs=pe[:, l:l + 1], start=(l == 0), stop=(l == 31)),
                     reads=[w1.r, pe.r], writes=[bps.r])
        P.op("dve", lambda e: e.tensor_copy(out=bias[:], in_=bps[:, 0:2]), reads=[bps.r], writes=[bias.r])
        for g in range(2):
            s_ = src.next()
            ld(P, s_[:], i_[("kcT" if kv == 0 else "vcT")][g], s_.r)
            sv = s_[:].rearrange("p (n l) -> p l n", l=16)
            for cc in range(2):
                hp = hps.next()
                for l in range(32):
                    rhs = sv[:, l, 0:511] if l < 16 else sv[:, l - 16, 1:512]
                    P.op("pe", lambda e, hp=hp, cc=cc, l=l, rhs=rhs: e.matmul(hp[:, 0:511], lhsT=w1[:, l, cc * 128:(cc + 1) * 128], rhs=rhs, start=(l == 0), stop=(l == 31)),
                         reads=[w1.r, s_.r], writes=[hp.r])
                P.op("act", lambda e, hp=hp, cc=cc: e.activation(out=hid[:, cc, 0:511], in_=hp[:, 0:511], func=AF.Gelu, bias=bias[:, cc:cc + 1], scale=1.0), reads=[hp.r, bias.r], writes=[hid.r])
            o = og.next()
            if kv == 0:
                op_ = ops_.next()
                for cc in range(2):
                    P.op("pe", lambda e, op_=op_, cc=cc: e.matmul(op_[:, 0:511], lhsT=w2[:, cc, :], rhs=hid[:, cc, 0:511], start=(cc == 0), stop=(cc == 1)), reads=[w2.r, hid.r], writes=[op_.r])
                P.op("dve", lambda e, op_=op_, o=o: e.tensor_copy(out=o[:, 0:511], in_=op_[:, 0:511]), reads=[op_.r], writes=[o.r])
                st(P, o_["kccT"][g, :, 0:511], o[:, 0:511], o.r)
            else:
                for nt in range(4):
                    n1 = min(128, 511 - nt * 128)
                    op_ = ops_.next()
                    for cc in range(2):
                        P.op("pe", lambda e, op_=op_, cc=cc, nt=nt, n1=n1: e.matmul(op_[0:n1, 0:128], lhsT=hid[:, cc, nt * 128:nt * 128 + n1], rhs=w2[:, cc, :], start=(cc == 0), stop=(cc == 1)),
                             reads=[w2.r, hid.r], writes=[op_.r])
                    P.op("dve", lambda e, op_=op_, o=o, nt=nt, n1=n1: e.tensor_copy(out=o[0:n1, nt * 128:(nt + 1) * 128], in_=op_[0:n1, 0:128]), reads=[op_.r], writes=[o.r])
                    st(P, o_["vcc"][g, nt * 128:nt * 128 + n1, :], o[0:n1, nt * 128:(nt + 1) * 128], o.r)


def build_D3a():
    nc = bass.Bass("TRN2", target_bir_lowering=False)
    P = Prog(nc)
    i_ = dict(qT=dram_in(nc, "qT", [16, 128, 2048]), gT=dram_in(nc, "gT", [48, 2048]), kccT=dram_in(nc, "kccT", [2, 128, 512]), vcc=dram_in(nc, "vcc", [2, 512, 128]),
              cmask=dram_in(nc, "cmask", [4, 128, 2048]), ovl=dram_in(nc, "ovl", [4, 128, 128]), m1=dram_in(nc, "m1", [NT, 128, 128]), m2=dram_in(nc, "m2", [NT, 128, 128]),
              ones=dram_in(nc, "ones", [128, 128]), ident=dram_in(nc, "ident", [128, 128]))
    o_ = dict(oc=dram_out(nc, "oc", [16, 128, 2048]), negmT=dram_out(nc, "negmT", [2, 128, 2048], BF16))
    emit_D3a(P, i_, o_)
    P.emit()
    return nc


def emit_D3a(P, i_, o_):
    ones = T(P, "ones", [128, 128]); ld(P, ones[:], i_["ones"], ones.r)
    idn = T(P, "idn", [128, 128]); ld(P, idn[:], i_["ident"], idn.r)
    ovl = T(P, "ovl", [128, 4, 128]); ld(P, ovl[:], i_["ovl"].rearrange("t n j -> n t j"), ovl.r)
    kcc = Pool_(P, "kcc", [128, 512], 2); vcc = Pool_(P, "vcc", [128, 4, 128], 2)
    cm = Pool_(P, "cm", [128, 4, 512], 2)
    qh = Pool_(P, "qh", [128, 512], 3); gb = Pool_(P, "gb", [128, 512], 3)
    ec = Pool_(P, "ec", [128, 4, 512], 2)
    zb = Pool_(P, "zb", [128, 512], 2, psum=True); db = Pool_(P, "db", [128, 512], 1, psum=True); ob = Pool_(P, "ob", [128, 512], 2, psum=True)
    ib = Pool_(P, "ib", [128, 512], 1, psum=True); tb = Pool_(P, "tb", [128, 512], 2, psum=True)
    rden = Pool_(P, "rden", [128, 512], 2); ocs = Pool_(P, "ocs", [128, 512], 2)
    impT = T(P, "impT", [128, 512]); m1 = T(P, "m1", [128, 4, 128]); m2 = T(P, "m2", [128, 4, 128]); score = T(P, "score", [128, 4, 128])
    tv = T(P, "tv", [128, 16]); rep = T(P, "rep", [128, 128]); selm = T(P, "selm", [128, 4, 128]); nmT = Pool_(P, "nmT", [128, 512], 2, dt=BF16)
    NK = [128, 128, 128, 127]
    for g in range(2):
        kc = kcc.next(); vc = vcc.next()
        ld(P, kc[:], i_["kccT"][g], kc.r)
        ld(P, vc[:], i_["vcc"][g].rearrange("(t n) d -> n t d", n=128), vc.r)
        for qg in range(4):
            gc = slice(qg * 512, (qg + 1) * 512)
            cmt = cm.next()
            ld(P, cmt[:], i_["cmask"][:, :, gc].rearrange("t n q -> n t q"), cmt.r)
            ld(P, m1[:], i_["m1"][qg * 4:(qg + 1) * 4].rearrange("t q j -> q t j"), m1.r)
            ld(P, m2[:], i_["m2"][qg * 4:(qg + 1) * 4].rearrange("t q j -> q t j"), m2.r)
            imp = ib.next()
            for r in range(8):
                hd = g * 8 + r
                q = qh.next(); g0 = gb.next(); e_ = ec.next(); d_ = db.next(); o = ob.next(); rd = rden.next(); oc = ocs.next()
                ld(P, q[:], i_["qT"][hd, :, gc], q.r)
                ld(P, g0[:], i_["gT"][hd * 3:hd * 3 + 1, gc].broadcast_to([128, 512]), g0.r)
                for nt in range(4):
                    nk = NK[nt]
                    z = zb.next()
                    P.op("pe", lambda e, z=z, kc=kc, q=q, nt=nt, nk=nk: e.matmul(z[0:nk, :], lhsT=kc[:, nt * 128:nt * 128 + nk], rhs=q[:], start=True, stop=True), reads=[kc.r, q.r], writes=[z.r])
                    P.op("act", lambda e, z=z, e_=e_, nt=nt, nk=nk: e.activation(out=e_[0:nk, nt, :], in_=z[0:nk, :], func=AF.Exp), reads=[z.r], writes=[e_.r])
                    P.op("pool", lambda e, e_=e_, cmt=cmt, nt=nt, nk=nk: e.tensor_tensor(out=e_[0:nk, nt, :], in0=e_[0:nk, nt, :], in1=cmt[0:nk, nt, :], op=ALU.mult), reads=[e_.r, cmt.r], writes=[e_.r])
                    P.op("pe", lambda e, d_=d_, e_=e_, nt=nt, nk=nk: e.matmul(d_[:, :], lhsT=ones[0:nk, :], rhs=e_[0:nk, nt, :], start=(nt == 0), stop=(nt == 3)), reads=[ones.r, e_.r], writes=[d_.r])
                P.op("dve", lambda e, rd=rd, d_=d_: e.tensor_scalar(out=rd[:], in0=d_[:, :], scalar1=1e-30, scalar2=None, op0=ALU.max), reads=[d_.r], writes=[rd.r])
                P.op("dve", lambda e, rd=rd: e.reciprocal(out=rd[:], in_=rd[:]), reads=[rd.r], writes=[rd.r])
                for nt in range(4):
                    nk = NK[nt]
                    P.op("dve", lambda e, e_=e_, rd=rd, nt=nt, nk=nk: e.tensor_tensor(out=e_[0:nk, nt, :], in0=e_[0:nk, nt, :], in1=rd[0:nk, :], op=ALU.mult), reads=[e_.r, rd.r], writes=[e_.r])
                    P.op("pe", lambda e, imp=imp, e_=e_, nt=nt, nk=nk, r=r: e.matmul(imp[:, :], lhsT=ovl[0:nk, nt, :], rhs=e_[0:nk, nt, :], start=(r == 0 and nt == 0), stop=(r == 7 and nt == 3)),
                         reads=[ovl.r, e_.r], writes=[imp.r])
                    P.op("pe", lambda e, o=o, vc=vc, e_=e_, nt=nt, nk=nk: e.matmul(o[:, :], lhsT=vc[0:nk, nt, :], rhs=e_[0:nk, nt, :], start=(nt == 0), stop=(nt == 3)), reads=[vc.r, e_.r], writes=[o.r])
                P.op("dve", lambda e, oc=oc, o=o, g0=g0: e.tensor_tensor(out=oc[:], in0=o[:, :], in1=g0[:], op=ALU.mult), reads=[o.r, g0.r], writes=[oc.r])
                st(P, o_["oc"][hd, :, gc], oc[:], oc.r)
            P.op("act", lambda e, imp=imp: e.copy(out=impT[:], in_=imp[:, :]), reads=[imp.r], writes=[impT.r])
            tp = tb.next()
            for m in range(4):
                P.op("pe", lambda e, tp=tp, m=m: e.transpose(tp[:, m * 128:(m + 1) * 128], impT[:, m * 128:(m + 1) * 128], idn[:]), reads=[impT.r, idn.r], writes=[tp.r])
            P.op("dve", lambda e, tp=tp: e.tensor_tensor(out=score[:], in0=tp[:, :].rearrange("p (m j) -> p m j", j=128), in1=m1[:], op=ALU.mult), reads=[tp.r, m1.r], writes=[score.r])
            P.op("dve", lambda e: e.tensor_tensor(out=score[:], in0=score[:], in1=m2[:], op=ALU.add), reads=[score.r, m2.r], writes=[score.r])
            for m in range(4):
                P.op("dve", lambda e, m=m: e.max(out=tv[:, 0:8], in_=score[:, m, :]), reads=[score.r], writes=[tv.r])
                P.op("dve", lambda e, m=m: e.match_replace(out=rep[:], in_to_replace=tv[:, 0:8], in_values=score[:, m, :], imm_value=-3e30), reads=[score.r, tv.r], writes=[rep.r])
                P.op("dve", lambda e: e.max(out=tv[:, 8:16], in_=rep[:]), reads=[rep.r], writes=[tv.r])
                P.op("dve", lambda e, m=m: e.tensor_scalar(out=selm[:, m, :], in0=score[:, m, :], scalar1=tv[:, 15:16], scalar2=None, op0=ALU.is_ge), reads=[score.r, tv.r], writes=[selm.r])
            P.op("dve", lambda e: e.tensor_scalar(out=selm[:], in0=selm[:], scalar1=-1.0, scalar2=30000.0, op0=ALU.add, op1=ALU.mult), reads=[selm.r], writes=[selm.r])
            tp2 = tb.next(); nm = nmT.next()
            for m in range(4):
                P.op("pe", lambda e, tp2=tp2, m=m: e.transpose(tp2[:, m * 128:(m + 1) * 128], selm[:, m, :], idn[:]), reads=[selm.r, idn.r], writes=[tp2.r])
            P.op("act", lambda e, tp2=tp2, nm=nm: e.copy(out=nm[:], in_=tp2[:, :]), reads=[tp2.r], writes=[nm.r])
            st(P, o_["negmT"][g, :, gc], nm[:], nm.r)


def build_D3b():
    nc = bass.Bass("TRN2", target_bir_lowering=False)
    P = Prog(nc)
    i_ = dict(qT=dram_in(nc, "qT", [16, 128, 2048]), gT=dram_in(nc, "gT", [48, 2048]), oc=dram_in(nc, "oc", [16, 128, 2048]), negmT=dram_in(nc, "negmT", [2, 128, 2048], BF16),
              ksT=dram_in(nc, "ksT", [2, 128, 8192]), vs=dram_in(nc, "vs", [2, 8192, 128]), Ebig=dram_in(nc, "Ebig", [128, 64, 128], BF16),
              masks=dram_in(nc, "masks", [16, 128, 512]), kwwT=dram_in(nc, "kwwT", [2, NT, 128, 640]), vww=dram_in(nc, "vww", [2, NT, 640, 128]),
              wmask=dram_in(nc, "wmask", [4, 5, 128, 512]), ones=dram_in(nc, "ones", [128, 128]))
    o_ = dict(oT=dram_out(nc, "oT", [16, 128, 2048]))
    emit_D3b(P, i_, o_)
    P.emit()
    return nc


def emit_D3b(P, i_, o_):
    MK = Masker(P, i_["masks"])
    on32 = T(P, "on32", [128, 128]); ld(P, on32[:], i_["ones"], on32.r)
    ones = T(P, "ones", [128, 128], ADT)
    P.op("dve", lambda e: e.tensor_copy(out=ones[:], in_=on32[:]), reads=[on32.r], writes=[ones.r])
    Eb = T(P, "Ebig", [128, 64, 128], BF16); ld(P, Eb[:], i_["Ebig"], Eb.r)
    KS = T(P, "KS", [128, 8192], ADT); VS = T(P, "VS", [128, 64, 128], ADT)
    kww = T(P, "kww", [128, 4, 640], ADT); kwin = T(P, "kwin", [128, 20, 128], ADT); vww = T(P, "vwin", [128, 4, 5, 128], ADT); wm = T(P, "wm", [128, 5, 512])
    id32 = T(P, "id32", [128, 128]); ld(P, id32[:], i_["ident"], id32.r)
    idn = T(P, "idn", [128, 128], ADT)
    P.op("dve", lambda e: e.tensor_copy(out=idn[:], in_=id32[:]), reads=[id32.r], writes=[idn.r])
    tpbf = Pool_(P, "tpbf", [128, 512], 1, dt=ADT, psum=True)
    widx = T(P, "widx", [128, 80], I32); ld(P, widx[:], i_["widx"], widx.r)
    nmp = Pool_(P, "nm", [128, 512], 2, dt=BF16)
    qh = Pool_(P, "qh", [128, 512], 3, dt=ADT); g1p = Pool_(P, "g1", [128, 512], 2); g2p = Pool_(P, "g2", [128, 512], 2); ocp = Pool_(P, "occ", [128, 512], 2)
    pt = Pool_(P, "pt", [128, 512], 5, dt=ADT); pw = T(P, "pw", [128, 5, 512], ADT)
    rsp = Pool_(P, "rs", [128, 512], 2); t1p = Pool_(P, "t1", [128, 512], 2); accp = Pool_(P, "oacc", [128, 512], 2)
    zb = Pool_(P, "zb", [128, 512], 3, psum=True); osb = T(P, "osb", [128, 512], psum=True); dsb = T(P, "dsb", [128, 512], psum=True)
    zw = zb; owb = T(P, "owb", [128, 512], psum=True); dwb = T(P, "dwb", [128, 512], psum=True)
    for g in range(2):
        P.dma("sp", [lambda e, g=g, c=c: e.dma_start(out=KS[:, c * 2048:(c + 1) * 2048], in_=i_["ksT"][g, :, c * 2048:(c + 1) * 2048]) for c in range(4)], KS.r, writes=[KS.r])
        vv = i_["vs"][g].rearrange("(k p) d -> p k d", p=128)
        P.dma("sp", [lambda e, vv=vv, c=c: e.dma_start(out=VS[:, c * 16:(c + 1) * 16, :], in_=vv[:, c * 16:(c + 1) * 16, :]) for c in range(4)], VS.r, writes=[VS.r])
        for qg in range(4):
            gc = slice(qg * 512, (qg + 1) * 512)
            nm = nmp.next()
            ld(P, nm[:], i_["negmT"][g, :, gc], nm.r)
            for j in range(20):
                col = (qg * 4 + j // 5) * 5 + j % 5
                P.dma("pool", lambda e, j=j, col=col, g=g: e.indirect_dma_start(out=kwin[:, j, :], out_offset=None, in_=i_["kw"][g], in_offset=bass.IndirectOffsetOnAxis(ap=widx[:, col:col + 1], axis=0)),
                      kwin.r, reads=[widx.r], writes=[kwin.r])
                P.dma("pool", lambda e, j=j, col=col, g=g: e.indirect_dma_start(out=vww[:, j // 5, j % 5, :], out_offset=None, in_=i_["vw"][g], in_offset=bass.IndirectOffsetOnAxis(ap=widx[:, col:col + 1], axis=0)),
                      vww.r, reads=[widx.r], writes=[vww.r])
            for j0 in range(0, 20, 4):
                tpb = tpbf.next()
                for j in range(j0, j0 + 4):
                    P.op("pe", lambda e, tpb=tpb, j=j, j0=j0: e.transpose(tpb[:, (j - j0) * 128:(j - j0 + 1) * 128], kwin[:, j, :], idn[:]), reads=[kwin.r, idn.r], writes=[tpb.r])
                for j in range(j0, j0 + 4):
                    P.op("dve", lambda e, tpb=tpb, j=j, j0=j0: e.tensor_copy(out=kww[:, j // 5, (j % 5) * 128:(j % 5 + 1) * 128], in_=tpb[:, (j - j0) * 128:(j - j0 + 1) * 128]), reads=[tpb.r], writes=[kww.r])
            ld(P, wm[:], i_["wmask"][qg].rearrange("o s q -> s o q"), wm.r)
            kb_max = 16 * qg + 15
            for r in range(8):
                hd = g * 8 + r
                q = qh.next(); g1 = g1p.next(); g2 = g2p.next(); occ = ocp.next()
                ld(P, q[:], i_["qTb"][hd, :, gc], q.r)
                ld(P, g1[:], i_["gT"][hd * 3 + 1:hd * 3 + 2, gc].broadcast_to([128, 512]), g1.r)
                ld(P, g2[:], i_["gT"][hd * 3 + 2:hd * 3 + 3, gc].broadcast_to([128, 512]), g2.r)
                ld(P, occ[:], i_["oc"][hd, :, gc], occ.r)
                kbs = list(range(kb_max, -1, -1))
                pend = []

                def S2(kb, p):
                    P.op("pe", lambda e, p=p, kb=kb, kb_max=kb_max: e.matmul(osb[:, :], lhsT=VS[:, kb, :], rhs=p[:], start=(kb == kb_max), stop=(kb == 0)), reads=[VS.r, p.r], writes=[osb.r])
                    P.op("pe", lambda e, p=p, kb=kb, kb_max=kb_max: e.matmul(dsb[:, :], lhsT=ones[:], rhs=p[:], start=(kb == kb_max), stop=(kb == 0)), reads=[ones.r, p.r], writes=[dsb.r])
                for kb in kbs:
                    ks = slice(kb * 128, (kb + 1) * 128)
                    z = zb.next(); p = pt.next()
                    P.op("pe", lambda e, z=z, q=q, ks=ks: e.matmul(z[:, :], lhsT=KS[:, ks], rhs=q[:], start=True, stop=False), reads=[KS.r, q.r], writes=[z.r])
                    P.op("pe", lambda e, z=z, nm=nm, kb=kb: e.matmul(z[:, :], lhsT=Eb[:, kb, :], rhs=nm[:], start=False, stop=True), reads=[Eb.r, nm.r], writes=[z.r])
                    P.op("act", lambda e, z=z, p=p: e.activation(out=p[:], in_=z[:, :], func=AF.Exp), reads=[z.r], writes=[p.r])
                    if kb >= 16 * qg:
                        MK.apply(p, kb - 16 * qg)
                    pend.append((kb, p))
                    if len(pend) > 2:
                        S2(*pend.pop(0))
                while pend:
                    S2(*pend.pop(0))
                rs = rsp.next(); t1 = t1p.next(); acc = accp.next()
                P.op("dve", lambda e, rs=rs: e.reciprocal(out=rs[:], in_=dsb[:, :]), reads=[dsb.r], writes=[rs.r])
                P.op("dve", lambda e, rs=rs, t1=t1: e.tensor_tensor(out=t1[:], in0=osb[:, :], in1=rs[:], op=ALU.mult), reads=[osb.r, rs.r], writes=[t1.r])
                P.op("pool", lambda e, t1=t1, g1=g1: e.tensor_tensor(out=t1[:], in0=t1[:], in1=g1[:], op=ALU.mult), reads=[t1.r, g1.r], writes=[t1.r])
                P.op("pool", lambda e, t1=t1, acc=acc, occ=occ: e.tensor_tensor(out=acc[:], in0=t1[:], in1=occ[:], op=ALU.add), reads=[t1.r, occ.r], writes=[acc.r])
                for off in range(5):
                    z = zw.next()
                    for m in range(4):
                        P.op("pe", lambda e, z=z, q=q, m=m, off=off: e.matmul(z[:, m * 128:(m + 1) * 128], lhsT=kww[:, m, off * 128:(off + 1) * 128], rhs=q[:, m * 128:(m + 1) * 128], start=True, stop=True),
                             reads=[kww.r, q.r], writes=[z.r])
                    P.op("act", lambda e, z=z, off=off: e.activation(out=pw[:, off, :], in_=z[:, :], func=AF.Exp), reads=[z.r], writes=[pw.r])
                    P.op("pool", lambda e, off=off: e.tensor_tensor(out=pw[:, off, :], in0=pw[:, off, :], in1=wm[:, off, :], op=ALU.mult), reads=[pw.r, wm.r], writes=[pw.r])
                for m in range(4):
                    ms = slice(m * 128, (m + 1) * 128)
                    for off in range(5):
                        P.op("pe", lambda e, m=m, ms=ms, off=off: e.matmul(owb[:, ms], lhsT=vww[:, m, off, :], rhs=pw[:, off, ms], start=(off == 0), stop=(off == 4)), reads=[vww.r, pw.r], writes=[owb.r])
                    for off in range(5):
                        P.op("pe", lambda e, ms=ms, off=off: e.matmul(dwb[:, ms], lhsT=ones[:], rhs=pw[:, off, ms], start=(off == 0), stop=(off == 4)), reads=[ones.r, pw.r], writes=[dwb.r])
                rs2 = rsp.next(); t2 = t1p.next()
                P.op("dve", lambda e, rs2=rs2: e.reciprocal(out=rs2[:], in_=dwb[:, :]), reads=[dwb.r], writes=[rs2.r])
                P.op("dve", lambda e, rs2=rs2, t2=t2: e.tensor_tensor(out=t2[:], in0=owb[:, :], in1=rs2[:], op=ALU.mult), reads=[owb.r, rs2.r], writes=[t2.r])
                P.op("pool", lambda e, t2=t2, g2=g2: e.tensor_tensor(out=t2[:], in0=t2[:], in1=g2[:], op=ALU.mult), reads=[t2.r, g2.r], writes=[t2.r])
                P.op("pool", lambda e, t2=t2, acc=acc: e.tensor_tensor(out=acc[:], in0=acc[:], in1=t2[:], op=ALU.add), reads=[t2.r, acc.r], writes=[acc.r])
                st(P, o_["oT"][hd, :, gc], acc[:], acc.r, eng="act")


import ml_dtypes as _mld

NCORES = 8
TA = 8192
NTA = TA // 128


def build_fused(debug=()):
    nc = bass.Bass("TRN2", target_bir_lowering=False)
    P = Prog(nc)
    I = lambda n, s, dt=F32: dram_in(nc, n, s, dt)

    def mid(name, shape, dt=F32):
        return dram_out(nc, name, shape, dt) if name in debug else dram_tmp(nc, name, shape, dt)
    x = I("x", [TA, 2048]); pos_all = I("pos_all", [128, NTA], I32); pos_own = I("pos_own", [128, NT], I32); c2 = I("c2", [128, 16])
    adaw = [I(f"adaw{m}", [2048, 6144]) for m in range(4)]; adab = [I(f"adab{m}", [1, 6144]) for m in range(4)]; gn = [I(f"g{m}", [1, 2048]) for m in range(4)]
    w_in0 = I("w_in0", [2048, 3904]); qn_g = I("qn_g", [128, 4]); w_uq = I("w_uq", [512, 1536]); kvn_g = I("kvn_g", [128, 2]); w_ukv = I("w_ukv", [256, 2048]); w_out0 = I("w_out0", [2048, 2048])
    wq = [I(f"wq{l}", [2048, 2048]) for l in range(2)]; k1T = [I(f"k1T{l}", [128, 128]) for l in range(2)]; k2T = [I(f"k2T{l}", [128, 128]) for l in range(2)]
    pu = [I(f"pu{l}", [16384, 2048]) for l in range(2)]; pv = [I(f"pv{l}", [16384, 2048]) for l in range(2)]
    w_in1 = I("w_in1", [2048, 3632]); pekT = I("pekT", [128, 32]); pevT = I("pevT", [128, 32]); w1k = I("w1k", [4096, 256]); w2k = I("w2k", [256, 128])
    w1v = I("w1v", [4096, 256]); w2v = I("w2v", [256, 128]); w_out1 = I("w_out1", [2048, 2048]); fin_g = I("fin_g", [1, 2048])
    ident = I("ident", [128, 128]); ones = I("ones", [128, 128]); negT1 = I("negT1", [128, 128]); negOnes = I("negOnes", [128, 128]); iota16 = I("iota16", [1, 16])
    invf32 = I("invf32", [1, 32]); invf16 = I("invf16", [1, 16]); ms4 = I("ms4", [4, 128, 512]); mc4 = I("mc4", [4, 128, 512]); mc16 = I("mc16", [16, 128, 512])
    cmask = I("cmask", [4, 128, 2048]); ovl = I("ovl", [4, 128, 128]); m1 = I("m1", [NT, 128, 128]); m2 = I("m2", [NT, 128, 128]); Ebig = I("Ebig", [128, 64, 128], BF16)
    wmask = I("wmask", [4, 5, 128, 512]); widx = I("widx", [128, 80], I32); own_idx = I("own_idx", [128, NT], I32)
    out = dram_out(nc, "out", [2048, 2048])
    mods = mid("mods", [4, 3, 2048])
    qT_sb = mid("qT_sb", [8, 128, TA], ADT); kT_sb = mid("kT_sb", [8, 128, TA], ADT); v_sb = mid("v_sb", [TA, 1024], ADT); qT_mn = mid("qT_mn", [8, 128, TA], ADT); qT_mr = mid("qT_mr", [4, 128, TA], ADT)
    kT_mn = mid("kT_mn", [8, 128, TA], ADT); kT_r = mid("kT_r", [64, TA], ADT); v_m = mid("v_m", [TA, 1024], ADT); oT0 = mid("oT0", [16, 128, TA], ADT)
    x1 = mid("x1", [TA, 2048]); h0 = mid("h0", [TA, 2048]); ids0 = mid("ids0", [TA, 128], I32); gw0 = mid("gw0", [TA, 128]); x2 = mid("x2", [TA, 2048])
    kcT = mid("kcT", [2, 128, TA]); vcT = mid("vcT", [2, 128, TA]); ksT = mid("ksT", [2, 128, TA], ADT); vs = mid("vs", [TA, 256], ADT); kw = [mid(f"kw{g}", [TA + 128, 128], ADT) for g in range(2)]; vw = [mid(f"vw{g}", [TA + 128, 128], ADT) for g in range(2)]
    qT1 = mid("qT1", [16, 128, 2048]); qT1b = mid("qT1b", [16, 128, 2048], ADT); gT = mid("gT", [48, 2048]); kccT = mid("kccT", [2, 128, 512]); vcc = mid("vcc", [2, 512, 128]); oc = mid("oc", [16, 128, 2048])
    negmT = mid("negmT", [2, 128, 2048], BF16); oT1 = mid("oT1", [16, 128, 2048]); x3 = mid("x3", [2048, 2048]); h1 = mid("h1", [2048, 2048]); ids1 = mid("ids1", [2048, 128], I32); gw1 = mid("gw1", [2048, 128])
    uv = [dram_tmp(nc, f"uv{l}", [16384, 4096], BF16) for l in range(2)]
    stop = [d for d in debug if d.startswith("stop:")]
    stop = stop[0][5:] if stop else None
    sched0 = [(4 * g + 3, 4 * g) for g in range(NTA // 4)]

    def phases():
        with Phase(P, "M"):
            emit_M(P, c2, adaw, adab, gn, mods)
        yield "M"
        with Phase(P, "A"):
            emit_A(P, dict(xload=xload_direct(x), pos=pos_all, mods=mods, w_in=w_in0, qn_g=qn_g, w_uq=w_uq, kvn_g=kvn_g, w_ukv=w_ukv, ident=ident, ones=ones, invf=invf32),
                   dict(qT_sb=qT_sb, kT_sb=kT_sb, v_sb=v_sb, qT_mn=qT_mn, qT_mr=qT_mr, kT_mn=kT_mn, kT_r=kT_r, v_m=v_m), NTA)
        yield "A"
        with Phase(P, "B1"):
            emit_B1(P, dict(qT=qT_sb, kT=kT_sb, v=v_sb.rearrange("t (h d) -> h t d", d=128), negT1=negT1, negOnes=negOnes, masks=ms4), dict(oT=oT0[0:8]), 8, sched0)
        yield "B1"
        with Phase(P, "B2"):
            emit_B2(P, dict(qTn=qT_mn, qTr=qT_mr, kTn=kT_mn, kTr=kT_r, v=v_m.rearrange("t (h d) -> h t d", d=128), ones=ones, masks=mc4), dict(oT=oT0[8:16]), 8, sched0)
        yield "B2"
        with Phase(P, "C1a"):
            emit_C1(P, dict(oT=oT0, w_out=w_out0, xload=xload_direct(x), mods=mods), dict(x1=x1), 0, NTA)
        yield "C1a"
        with Phase(P, "C2a"):
            emit_C2(P, dict(x=x1, mods=mods, w_q=wq[0], k1T=k1T[0], k2T=k2T[0], ident=ident, iota16=iota16), dict(h=h0, ids=ids0, gw=gw0), 1, NTA)
        yield "C2a"
        for l in range(2):
            with Phase(P, f"PCu{l}"):
                emit_PC(P, pu[l], uv[l], 0)
            with Phase(P, f"PCv{l}"):
                emit_PC(P, pv[l], uv[l], 1)
        with Phase(P, "C3a"):
            emit_C3f(P, dict(x=x1, h=h0, ids=ids0, gw=gw0, mods=mods, uv=uv[0], ident=ident), dict(x2=x2), 1, NTA)
        yield "C3a"
        with Phase(P, "D1k"):
            emit_D1(P, dict(xload=xload_direct(x2), pos=pos_all, mods=mods, w_in=w_in1, ident=ident, invf=invf16), dict(kcT=kcT, vcT=vcT, ksT=ksT, vs=vs, kw=kw, vw=vw), 2, NTA, "k")
        yield "D1k"
        with Phase(P, "D1q"):
            emit_D1(P, dict(xload=xload_gather(P, x2, own_idx, NT), pos=pos_own, mods=mods, w_in=w_in1, ident=ident, invf=invf16), dict(qT=qT1, qTb=qT1b, gT=gT), 2, NT, "q")
        yield "D1q"
        with Phase(P, "D2"):
            emit_D2(P, dict(kcT=kcT, vcT=vcT, pekT=pekT, pevT=pevT, w1k=w1k, w2k=w2k, w1v=w1v, w2v=w2v), dict(kccT=kccT, vcc=vcc))
        yield "D2"
        with Phase(P, "D3a"):
            emit_D3a(P, dict(qT=qT1, gT=gT, kccT=kccT, vcc=vcc, cmask=cmask, ovl=ovl, m1=m1, m2=m2, ones=ones, ident=ident), dict(oc=oc, negmT=negmT))
        yield "D3a"
        with Phase(P, "D3b"):
            emit_D3b(P, dict(qTb=qT1b, gT=gT, oc=oc, negmT=negmT, ksT=ksT, vs=vs.rearrange("t (h d) -> h t d", d=128), Ebig=Ebig, masks=mc16, kw=kw, vw=vw, wmask=wmask, ones=ones,
                             ident=ident, widx=widx), dict(oT=oT1))
        yield "D3b"
        with Phase(P, "C1b"):
            emit_C1(P, dict(oT=oT1, w_out=w_out1, xload=xload_gather(P, x2, own_idx, NT), mods=mods), dict(x1=x3), 2, NT)
        yield "C1b"
        with Phase(P, "C2b"):
            emit_C2(P, dict(x=x3, mods=mods, w_q=wq[1], k1T=k1T[1], k2T=k2T[1], ident=ident, iota16=iota16), dict(h=h1, ids=ids1, gw=gw1), 3, NT)
        yield "C2b"
        with Phase(P, "C3b"):
            emit_C3f(P, dict(x=x3, h=h1, ids=ids1, gw=gw1, mods=mods, uv=uv[1], fin_g=fin_g, ident=ident), dict(x2=out), 3, NT)
        yield "C3b"
    for name in phases():
        if name == stop:
            break
    P.emit()
    return nc, P


def _consts(r):
    own = np.arange(8192).reshape(64, 128)[r::4].reshape(-1)
    c = {}
    kbp = np.arange(16)[:, None, None]; p = np.arange(128)[None, :, None]; f = np.arange(512)[None, None, :]
    val = (4 * (f // 128) + r - kbp) * 128 + (f % 128) - p
    c["mc16"] = (val >= 0).astype(np.float32)
    kb4 = np.arange(4)[:, None, None]
    val4 = (f - kb4 * 128 - p) + 0 * kb4
    c["ms4"] = (val4 > 0).astype(np.float32); c["mc4"] = (val4 >= 0).astype(np.float32)
    n = np.arange(512)
    c["cmask"] = ((16 * n[:, None] + 31 <= own[None, :]) & (n[:, None] <= 510)).astype(np.float32).reshape(4, 128, 2048)
    c_s = np.arange(512) * 16; s_s = np.arange(128) * 64
    ovl = np.clip(np.minimum(c_s[:, None] + 32, s_s[None, :] + 64) - np.maximum(c_s[:, None], s_s[None, :]), 0, None).astype(np.float32)
    ovl[511] = 0
    c["ovl"] = ovl.reshape(4, 128, 128)
    j = np.arange(128)[None, :]; t = own[:, None]; cur = t // 64
    forced = (j == 0) | (j == cur) | (j == cur - 1)
    valid = j * 64 <= t
    c["m1"] = (valid & ~forced).astype(np.float32).reshape(16, 128, 128)
    c["m2"] = np.where(forced, 1e9, np.where(valid, 0.0, -1e9)).astype(np.float32).reshape(16, 128, 128)
    jj = np.arange(128)[:, None, None]; kb = np.arange(64)[None, :, None]; s = np.arange(128)[None, None, :]
    c["Ebig"] = (jj == 2 * kb + (s >= 64)).astype(np.float32).astype(_mld.bfloat16)
    wmask = np.zeros((4, 5, 128, 512), np.float32)
    widx = np.zeros((128, 80), np.int32)
    pp = np.arange(128)
    for i in range(16):
        jq = 4 * i + r
        qg, mm = i // 4, i % 4
        tq = jq * 128 + np.arange(128)[None, :]
        for off in range(5):
            kbk = jq - 4 + off
            key = kbk * 128 + np.arange(128)[:, None]
            wmask[qg, off, :, mm * 128:(mm + 1) * 128] = ((key <= tq) & (key > tq - 512) & (key >= 0))
            widx[:, i * 5 + off] = (kbk * 128 + pp) if kbk >= 0 else (8192 + pp)
    c["wmask"] = wmask; c["widx"] = widx
    c["own_idx"] = np.ascontiguousarray(own.reshape(16, 128).T).astype(np.int32)
    return c


_PROG = {}


def make_inputs(inp):
    f32 = lambda a: np.ascontiguousarray(np.asarray(a, dtype=np.float32))
    x = f32(inp["x"]); c = f32(inp["c"]); positions = np.asarray(inp["positions"]).astype(np.int32)
    ident = np.eye(128, dtype=np.float32); ones = np.ones((128, 128), np.float32)
    jj = np.arange(128)[:, None]; ss = np.arange(128)[None, :]
    shared = dict(
        w_in0=f32(inp["sbmla_w_in"][0]), qn_g=np.ascontiguousarray(f32(inp["mla_q_norm"][0]).reshape(4, 128).T), w_uq=f32(inp["mla_w_uq"][0]),
        kvn_g=np.ascontiguousarray(f32(inp["mla_kv_norm"][0]).reshape(2, 128).T), w_ukv=f32(inp["mla_w_ukv"][0]), w_out0=f32(inp["sbmla_w_out"][0]),
        w_in1=f32(inp["nsa_w_in"][0]), pekT=np.ascontiguousarray(f32(inp["nsa_pe_k"][0]).T), pevT=np.ascontiguousarray(f32(inp["nsa_pe_v"][0]).T),
        w1k=f32(inp["nsa_w1_k"][0]), w2k=f32(inp["nsa_w2_k"][0]), w1v=f32(inp["nsa_w1_v"][0]), w2v=f32(inp["nsa_w2_v"][0]), w_out1=f32(inp["nsa_w_out"][0]),
        fin_g=f32(inp["final_norm"])[None, :], ident=ident, ones=ones, negT1=-(jj >= ss).astype(np.float32), negOnes=-ones, iota16=np.arange(16, dtype=np.float32)[None, :],
        invf32=(500000.0 ** (-np.arange(32, dtype=np.float64) / 32) / (2 * np.pi)).astype(np.float32)[None, :],
        invf16=(500000.0 ** (-np.arange(16, dtype=np.float64) / 16) / (2 * np.pi)).astype(np.float32)[None, :])
    mlist = [("ada_mix_w", "ada_mix_b", "norm_mix", 0), ("ada_ffn_w", "ada_ffn_b", "norm_ffn", 0), ("ada_mix_w", "ada_mix_b", "norm_mix", 1), ("ada_ffn_w", "ada_ffn_b", "norm_ffn", 1)]
    for m, (w, bb, g, l) in enumerate(mlist):
        shared[f"adaw{m}"] = f32(inp[w][l]); shared[f"adab{m}"] = f32(inp[bb][l])[None, :]; shared[f"g{m}"] = f32(inp[g][l])[None, :]
    for l in range(2):
        shared[f"wq{l}"] = f32(inp["peer_w_q"][l]); shared[f"k1T{l}"] = np.ascontiguousarray(f32(inp["peer_k1"][l]).T); shared[f"k2T{l}"] = np.ascontiguousarray(f32(inp["peer_k2"][l]).T)
        shared[f"pu{l}"] = f32(inp["peer_u"][l]); shared[f"pv{l}"] = f32(inp["peer_v"][l])
    CS = [_consts(r) for r in range(4)]
    maps = []
    for k in range(NCORES):
        b, r = k // 4, k % 4
        d = dict(shared)
        d.update(CS[r])
        d["x"] = x[b]; d["c2"] = np.ascontiguousarray(c[b].reshape(16, 128).T)
        d["pos_all"] = np.ascontiguousarray(positions[b].reshape(64, 128).T)
        d["pos_own"] = np.ascontiguousarray(positions[b].reshape(64, 128)[r::4].T)
        maps.append(d)
    return maps


def kernel(**inp):
    if "nc" not in _PROG:
        _PROG["nc"] = build_fused()[0]
    maps = make_inputs(inp)
    res = run_bass_kernel_spmd(_PROG["nc"], maps, core_ids=list(range(NCORES)))
    out = np.empty((2, 64, 128, 2048), np.float32)
    for k in range(NCORES):
        b, r = k // 4, k % 4
        out[b, r::4] = res.results[k]["out"].reshape(16, 128, 2048)
    return out.reshape(2, 8192, 2048)
```
